# Optimizing a Trainium2 kernel written in Bass

```python
import math
import jax, jax.numpy as jnp
from jax import lax
import numpy as np

D_MODEL = 1024
BATCH = 16
SEQ = 2048
DEPTH = 2

D_MIX = D_MODEL
W_CONV = D_MIX // 4
W_GLA = D_MIX // 4
W_SSD = D_MIX // 4
W_DIFF = D_MIX - W_CONV - W_GLA - W_SSD

CONV_K = 3

GLA_HEADS = 4
GLA_DV = W_GLA // GLA_HEADS
GLA_DK = GLA_DV // 2
GLA_RANK = 16
GLA_TAU = 16.0
GLA_CHUNK = 64

SSD_HEADDIM = 64
SSD_HEADS = W_SSD // SSD_HEADDIM
SSD_GROUPS = 2
SSD_STATE = 128
SSD_CONV_K = 4
SSD_CHUNK = 128
SSD_XBC = W_SSD + 2 * SSD_GROUPS * SSD_STATE

DIFF_HEADS = 4
DIFF_DV = W_DIFF // DIFF_HEADS
DIFF_DQK = DIFF_DV // 2
ATTN_BLOCK = 128

N_GROUPS = 4
EXPERTS_PER_GROUP = 8
N_EXPERTS = N_GROUPS * EXPERTS_PER_GROUP
TOP_K = 2
D_EXPERT = 512
MOE_BLOCK = 128

DEEPNORM_ALPHA = (2 * DEPTH) ** 0.25
DEEPNORM_BETA = (8 * DEPTH) ** -0.25
LN_EPS = 1e-5
RMS_EPS = 1e-6

SPLIT_SIZES = (
    W_CONV, W_CONV, W_CONV,
    GLA_HEADS * GLA_DK, GLA_HEADS * GLA_DK,
    GLA_HEADS * GLA_DV, GLA_HEADS * GLA_DV, GLA_RANK,
    W_SSD, SSD_XBC, SSD_HEADS,
    DIFF_HEADS * 2 * DIFF_DQK, DIFF_HEADS * 2 * DIFF_DQK,
    DIFF_HEADS * DIFF_DV,
)
P_IN = sum(SPLIT_SIZES)

kernel_name = 'hybrid_parallel_heads_hmoe_deepnorm'


def layer_norm(x, g, b):
    xf = x.astype(jnp.float32)
    mu = jnp.mean(xf, axis=-1, keepdims=True)
    var = jnp.mean(jnp.square(xf - mu), axis=-1, keepdims=True)
    return ((xf - mu) * lax.rsqrt(var + LN_EPS) * g + b).astype(x.dtype)


def rms_norm(x, g):
    xf = x.astype(jnp.float32)
    return xf * lax.rsqrt(jnp.mean(jnp.square(xf), axis=-1, keepdims=True) + RMS_EPS) * g


def split_cols(p, sizes):
    outs, off = [], 0
    for sz in sizes:
        outs.append(p[..., off:off + sz])
        off += sz
    return outs


def causal_dwconv(x, w, b=None):
    k_w, s = w.shape[0], x.shape[1]
    xp = jnp.pad(x.astype(jnp.float32), ((0, 0), (k_w - 1, 0), (0, 0)))
    y = sum(xp[:, k:k + s] * w[k] for k in range(k_w))
    return y if b is None else y + b


def short_conv_mixer(u, gate_b, gate_c, conv_w):
    return gate_b.astype(jnp.float32) * causal_dwconv(gate_c * u, conv_w)


def gla_mixer(q, k, v, g, lr, w_lr, b_lr, norm_g):
    bsz, s, _ = q.shape
    h, dk, dv, c = GLA_HEADS, GLA_DK, GLA_DV, GLA_CHUNK
    nc = s // c
    f32 = jnp.float32
    q = q.astype(f32).reshape(bsz, nc, c, h, dk) * dk ** -0.5
    k = k.astype(f32).reshape(bsz, nc, c, h, dk)
    v = v.astype(f32).reshape(bsz, nc, c, h, dv)
    log_a = jax.nn.log_sigmoid(lr.astype(f32) @ w_lr.astype(f32) + b_lr) / GLA_TAU
    cum = jnp.cumsum(log_a.reshape(bsz, nc, c, h, dk), axis=2)
    cum_last = cum[:, :, -1]
    q_dec = q * jnp.exp(cum)
    k_inv = k * jnp.exp(-cum)
    causal = jnp.tril(jnp.ones((c, c), bool))
    att = jnp.where(causal, jnp.einsum('bnihd,bnjhd->bnhij', q_dec, k_inv), 0.0)
    o_intra = jnp.einsum('bnhij,bnjhe->bnihe', att, v)
    k_end = k * jnp.exp(cum_last[:, :, None] - cum)
    d_state = jnp.einsum('bnjhd,bnjhe->bnhde', k_end, v)

    def step(state, inp):
        dec, ds = inp
        return dec[..., None] * state + ds, state

    _, s_prev = lax.scan(step, jnp.zeros((bsz, h, dk, dv), f32),
                         (jnp.moveaxis(jnp.exp(cum_last), 1, 0), jnp.moveaxis(d_state, 1, 0)))
    s_prev = jnp.moveaxis(s_prev, 0, 1)
    o = o_intra + jnp.einsum('bnihd,bnhde->bnihe', q_dec, s_prev)
    o = rms_norm(o.reshape(bsz, s, h, dv), norm_g).reshape(bsz, s, h * dv)
    return o * jax.nn.silu(g.astype(f32))


def ssd_mixer(z, xbc, dt, conv_w, conv_b, a_log, d_skip, dt_bias, norm_g):
    bsz, s, _ = z.shape
    g, r, p, n, c = SSD_GROUPS, SSD_HEADS // SSD_GROUPS, SSD_HEADDIM, SSD_STATE, SSD_CHUNK
    nc = s // c
    f32 = jnp.float32
    xbc = jax.nn.silu(causal_dwconv(xbc, conv_w, conv_b))
    xs, bm, cm = split_cols(xbc, (W_SSD, g * n, g * n))
    x = xs.reshape(bsz, nc, c, g, r, p)
    bm = bm.reshape(bsz, nc, c, g, n)
    cm = cm.reshape(bsz, nc, c, g, n)
    dt = jax.nn.softplus(dt.astype(f32) + dt_bias).reshape(bsz, nc, c, g, r)
    a = -jnp.exp(a_log.astype(f32)).reshape(g, r)
    cum = jnp.cumsum(dt * a, axis=2)
    cum_t = jnp.moveaxis(cum, 2, -1)
    causal = jnp.tril(jnp.ones((c, c), bool))
    decay = jnp.exp(jnp.where(causal, cum_t[..., :, None] - cum_t[..., None, :], -jnp.inf))
    x_dt = x * dt[..., None]
    cb = jnp.einsum('bcign,bcjgn->bcgij', cm, bm)
    y_diag = jnp.einsum('bcgij,bcgrij,bcjgrp->bcigrp', cb, decay, x_dt)
    cum_last = cum[:, :, -1]
    st = jnp.einsum('bcjgn,bcjgr,bcjgrp->bcgrpn', bm, jnp.exp(cum_last[:, :, None] - cum), x_dt)

    def step(state, inp):
        dec, ds = inp
        return dec[..., None, None] * state + ds, state

    _, s_prev = lax.scan(step, jnp.zeros((bsz, g, r, p, n), f32),
                         (jnp.moveaxis(jnp.exp(cum_last), 1, 0), jnp.moveaxis(st, 1, 0)))
    s_prev = jnp.moveaxis(s_prev, 0, 1)
    y_off = jnp.einsum('bcign,bcgrpn,bcigr->bcigrp', cm, s_prev, jnp.exp(cum))
    y = y_diag + y_off + x * d_skip.astype(f32).reshape(g, r)[..., None]
    y = y.reshape(bsz, s, W_SSD) * jax.nn.silu(z.astype(f32))
    y = rms_norm(y.reshape(bsz, s, g, W_SSD // g), norm_g.reshape(g, W_SSD // g))
    return y.reshape(bsz, s, W_SSD)


def diff_attention(q, k, v, lq1, lk1, lq2, lk2, norm_g, layer_idx):
    bsz, s, _ = q.shape
    h, dqk, dv, bq = DIFF_HEADS, DIFF_DQK, DIFF_DV, ATTN_BLOCK
    f32 = jnp.float32
    q = q.astype(f32).reshape(bsz, s, h, 2, dqk) * dqk ** -0.5
    k = k.astype(f32).reshape(bsz, s, h, 2, dqk)
    v = v.astype(f32).reshape(bsz, s, h, dv)
    lam_init = 0.8 - 0.6 * math.exp(-0.3 * layer_idx)
    lam = (jnp.exp(jnp.sum(lq1.astype(f32) * lk1)) - jnp.exp(jnp.sum(lq2.astype(f32) * lk2))
           + lam_init)
    k_pos = jnp.arange(s)

    def block(i):
        qb = lax.dynamic_slice_in_dim(q, i * bq, bq, axis=1)
        q_pos = i * bq + jnp.arange(bq)
        sc = jnp.einsum('bqhcd,bkhcd->bhcqk', qb, k)
        sc = jnp.where(k_pos[None, :] <= q_pos[:, None], sc, -jnp.inf)
        pr = jax.nn.softmax(sc, axis=-1)
        w = pr[:, :, 0] - lam * pr[:, :, 1]
        return jnp.einsum('bhqk,bkhe->bqhe', w, v)

    o = lax.map(block, jnp.arange(s // bq))
    o = jnp.moveaxis(o, 0, 1).reshape(bsz, s, h, dv)
    o = rms_norm(o, norm_g) * (1.0 - lam_init)
    return o.reshape(bsz, s, h * dv)


def expert_dispatch(xf, expert, gate, w_gate, w_up, w_down):
    t, d = xf.shape
    k_sel = expert.shape[1]
    n_assign = t * k_sel
    e_n, blk = N_EXPERTS, MOE_BLOCK
    n_blocks = (n_assign + e_n * (blk - 1) + blk - 1) // blk
    n_slots = n_blocks * blk
    flat_e = expert.reshape(n_assign)
    order = jnp.argsort(flat_e)
    sorted_e = flat_e[order]
    counts = jnp.bincount(flat_e, length=e_n)
    padded = (counts + blk - 1) // blk * blk
    pad_end = jnp.cumsum(padded)
    pad_start = pad_end - padded
    start = jnp.cumsum(counts) - counts
    dest = pad_start[sorted_e] + (jnp.arange(n_assign) - start[sorted_e])
    slot_tok = jnp.full((n_slots,), t, jnp.int32).at[dest].set((order // k_sel).astype(jnp.int32))
    slot_gate = jnp.zeros((n_slots,), gate.dtype).at[dest].set(gate.reshape(n_assign)[order])
    block_e = jnp.minimum(jnp.searchsorted(pad_end, jnp.arange(n_blocks) * blk, side='right'), e_n - 1)
    x_pad = jnp.concatenate([xf, jnp.zeros((1, d), xf.dtype)], axis=0)

    def run_block(args):
        tok, e = args
        xb = x_pad[tok]
        hb = jax.nn.silu(xb @ w_gate[e]) * (xb @ w_up[e])
        return hb @ w_down[e]

    y = lax.map(run_block, (slot_tok.reshape(n_blocks, blk), block_e)).reshape(n_slots, d)
    y = y * slot_gate[:, None].astype(y.dtype)
    return jnp.zeros((t + 1, d), y.dtype).at[slot_tok].add(y)[:t]


def hierarchical_moe(x, router_g, router_e, w_gate, w_up, w_down):
    bsz, s, d = x.shape
    t = bsz * s
    xf = x.reshape(t, d)
    g_prob = jax.nn.softmax((xf @ router_g).astype(jnp.float32), axis=-1)
    p_grp, grp = lax.top_k(g_prob, 1)
    e_logits = jnp.einsum('td,dge->tge', xf, router_e).astype(jnp.float32)
    e_logits = jnp.take_along_axis(
        e_logits, jnp.broadcast_to(grp[:, :, None], (t, 1, EXPERTS_PER_GROUP)), axis=1)[:, 0]
    p_exp, idx = lax.top_k(jax.nn.softmax(e_logits, axis=-1), TOP_K)
    gate = p_grp * p_exp / jnp.sum(p_exp, axis=-1, keepdims=True)
    expert = grp * EXPERTS_PER_GROUP + idx
    y = expert_dispatch(xf, expert, gate, w_gate, w_up, w_down)
    return y.reshape(bsz, s, d)


def setup_inputs(seed: int = 0) -> dict:
    key = jax.random.key(seed)
    ks = jax.random.split(key, 32)
    L, D = DEPTH, D_MODEL

    def nrm(k, shape, scale):
        return jax.random.normal(k, shape, jnp.float32) * scale

    def gain(k, shape):
        return 1.0 + 0.02 * jax.random.normal(k, shape, jnp.float32)

    dt0 = jnp.exp(jax.random.uniform(ks[10], (L, SSD_HEADS), jnp.float32,
                                     minval=math.log(1e-3), maxval=math.log(1e-1)))
    return {
        'x': nrm(ks[0], (BATCH, SEQ, D), 1.0),
        'w_in': nrm(ks[1], (L, D, P_IN), D ** -0.5),
        'conv_w': nrm(ks[2], (L, CONV_K, W_CONV), CONV_K ** -0.5),
        'gla_w_lr': nrm(ks[3], (L, GLA_RANK, GLA_HEADS * GLA_DK), GLA_RANK ** -0.5),
        'gla_b_lr': nrm(ks[4], (L, GLA_HEADS * GLA_DK), 0.1),
        'gla_norm_g': gain(ks[5], (L, GLA_DV)),
        'ssd_conv_w': nrm(ks[6], (L, SSD_CONV_K, SSD_XBC), SSD_CONV_K ** -0.5),
        'ssd_conv_b': nrm(ks[7], (L, SSD_XBC), 0.02),
        'ssd_a_log': jnp.log(jax.random.uniform(ks[8], (L, SSD_HEADS), jnp.float32, minval=1.0, maxval=16.0)),
        'ssd_d': gain(ks[9], (L, SSD_HEADS)),
        'ssd_dt_bias': dt0 + jnp.log(-jnp.expm1(-dt0)),
        'ssd_norm_g': gain(ks[11], (L, W_SSD)),
        'diff_lq1': nrm(ks[12], (L, DIFF_DQK), 0.1),
        'diff_lk1': nrm(ks[13], (L, DIFF_DQK), 0.1),
        'diff_lq2': nrm(ks[14], (L, DIFF_DQK), 0.1),
        'diff_lk2': nrm(ks[15], (L, DIFF_DQK), 0.1),
        'diff_norm_g': gain(ks[16], (L, DIFF_DV)),
        'w_o': nrm(ks[17], (L, D_MIX, D), D_MIX ** -0.5 * DEEPNORM_BETA),
        'ln1_g': gain(ks[18], (L, D)),
        'ln1_b': nrm(ks[19], (L, D), 0.02),
        'router_g': nrm(ks[20], (L, D, N_GROUPS), D ** -0.5),
        'router_e': nrm(ks[21], (L, D, N_GROUPS, EXPERTS_PER_GROUP), D ** -0.5),
        'w_gate': nrm(ks[22], (L, N_EXPERTS, D, D_EXPERT), D ** -0.5),
        'w_up': nrm(ks[23], (L, N_EXPERTS, D, D_EXPERT), D ** -0.5),
        'w_down': nrm(ks[24], (L, N_EXPERTS, D_EXPERT, D), D_EXPERT ** -0.5 * DEEPNORM_BETA),
        'ln2_g': gain(ks[25], (L, D)),
        'ln2_b': nrm(ks[26], (L, D), 0.02),
    }


def reference(x, w_in, conv_w, gla_w_lr, gla_b_lr, gla_norm_g, ssd_conv_w, ssd_conv_b,
              ssd_a_log, ssd_d, ssd_dt_bias, ssd_norm_g, diff_lq1, diff_lk1, diff_lq2,
              diff_lk2, diff_norm_g, w_o, ln1_g, ln1_b, router_g, router_e, w_gate, w_up,
              w_down, ln2_g, ln2_b):
    for l in range(DEPTH):
        proj = x @ w_in[l]
        (cu, cgb, cgc, gq, gk, gv, gg, glr, sz, sxbc, sdt,
         dfq, dfk, dfv) = split_cols(proj, SPLIT_SIZES)
        y_conv = short_conv_mixer(cu, cgb, cgc, conv_w[l])
        y_gla = gla_mixer(gq, gk, gv, gg, glr, gla_w_lr[l], gla_b_lr[l], gla_norm_g[l])
        y_ssd = ssd_mixer(sz, sxbc, sdt, ssd_conv_w[l], ssd_conv_b[l], ssd_a_log[l], ssd_d[l],
                          ssd_dt_bias[l], ssd_norm_g[l])
        y_diff = diff_attention(dfq, dfk, dfv, diff_lq1[l], diff_lk1[l], diff_lq2[l],
                                diff_lk2[l], diff_norm_g[l], l)
        mix = jnp.concatenate([y_conv, y_gla, y_ssd, y_diff], axis=-1).astype(x.dtype) @ w_o[l]
        x = layer_norm(DEEPNORM_ALPHA * x + mix, ln1_g[l], ln1_b[l])
        moe = hierarchical_moe(x, router_g[l], router_e[l], w_gate[l], w_up[l], w_down[l])
        x = layer_norm(DEEPNORM_ALPHA * x + moe, ln2_g[l], ln2_b[l])
    return x
```

```python
import numpy as np
import ml_dtypes
import concourse.bass as bass
import concourse.mybir as mybir
from concourse.bass_utils import run_bass_kernel_spmd

F32 = mybir.dt.float32
BF16 = mybir.dt.bfloat16
I32 = mybir.dt.int32
U8 = mybir.dt.uint8
ALU = mybir.AluOpType
AF = mybir.ActivationFunctionType
AX = mybir.AxisListType
DTSIZE = {F32: 4, BF16: 2, I32: 4, U8: 1}


class Buf:
    __slots__ = ("name", "wr", "rd", "psum", "dram")

    def __init__(self, name="", dram=False):
        self.name = name
        self.wr = []
        self.rd = []
        self.psum = False
        self.dram = dram


class T:
    __slots__ = ("ap", "buf")

    def __init__(self, ap, buf):
        self.ap = ap
        self.buf = buf

    def __getitem__(self, k):
        return T(self.ap[k], self.buf)


class Op:
    __slots__ = ("eng", "fn", "deps", "sig", "isdma", "sem", "val", "prev_val", "cost", "odeps", "idx", "lat")

    def __init__(self, eng, fn, isdma):
        self.eng = eng
        self.fn = fn
        self.isdma = isdma
        self.cost = 300.0
        self.lat = 0.0
        self.odeps = []
        self.idx = 0
        self.deps = []
        self.sig = False
        self.sem = None
        self.val = 0
        self.prev_val = 0


ENGS = ("pe", "act", "dve", "pool", "sp")
N_DMA_SEMS = 32
EPOCH = 30000


class Sched:
    def __init__(self):
        self.streams = {e: [] for e in ENGS}
        self.all_ops = []
        self.dma_count = 0
        self.pending_dma = []

    @staticmethod
    def _acc(x):
        if not isinstance(x, T):
            return x, None
        if x.buf.psum:
            return x.buf, None
        try:
            ap = x.ap
            pairs = [(int(p[0]), int(p[1])) for p in ap.ap]
            off = int(ap.offset)
            esz = DTSIZE.get(ap.dtype, 4)
            if x.buf.dram:
                dims = pairs
                base = off
            else:
                pstep = pairs[0][0]
                dims = pairs[1:]
                base = off % pstep if pstep > 0 else off
            dims = [(s_, c_) for (s_, c_) in dims if c_ > 1 and s_ != 0]
            if not dims:
                return x.buf, [(base * esz, (base + 1) * esz)]
            dims.sort(key=lambda d: -abs(d[0]))
            run = 1
            if dims[-1][0] == 1:
                run = dims[-1][1]
                dims = dims[:-1]
                while dims and dims[-1][0] == run:
                    run *= dims[-1][1]
                    dims = dims[:-1]
            n = 1
            for _, c_ in dims:
                n *= c_
            if n > 96:
                hi = base + sum(abs(s_) * (c_ - 1) for s_, c_ in dims) + run
                return x.buf, [(base * esz, hi * esz)]
            starts = [base]
            for s_, c_ in dims:
                starts = [st + s_ * k for st in starts for k in range(c_)]
            ivs = sorted((st * esz, (st + run) * esz) for st in starts)
            return x.buf, ivs
        except Exception:
            return x.buf, None

    @staticmethod
    def _ovl(a, b):
        if a is None or b is None:
            return True
        i = j = 0
        while i < len(a) and j < len(b):
            if a[i][1] <= b[j][0]:
                i += 1
            elif b[j][1] <= a[i][0]:
                j += 1
            else:
                return True
        return False

    @staticmethod
    def _covers(a, b):
        if a is None:
            return True
        if b is None:
            return False
        i = 0
        for lo, hi in b:
            while i < len(a) and a[i][1] <= lo:
                i += 1
            if i >= len(a) or a[i][0] > lo or a[i][1] < hi:
                return False
        return True

    def op(self, eng, fn, reads=(), writes=(), dma=False, cost=300.0, lat=0.0):
        o = Op(eng, fn, dma)
        o.cost = cost
        o.lat = lat
        o.idx = len(self.all_ops)
        deps = set()
        racc = [self._acc(x) for x in reads]
        wacc = [self._acc(x) for x in writes]
        for b, iv in racc:
            for w, wiv in b.wr:
                if self._ovl(iv, wiv):
                    deps.add(w)
            if b.psum:
                for r, riv in b.rd:
                    if r.eng != eng:
                        deps.add(r)
        for b, iv in wacc:
            for r, riv in b.rd:
                if r is not o and self._ovl(iv, riv):
                    deps.add(r)
            for w, wiv in b.wr:
                if not self._ovl(iv, wiv):
                    continue
                if w.isdma and dma:
                    continue
                if w.isdma or dma or w.eng != eng:
                    deps.add(w)
                else:
                    o.odeps.append(w)
        for b, iv in wacc:
            b.rd = [(r, riv) for (r, riv) in b.rd if not self._covers(iv, riv)]
            b.wr = [(w, wiv) for (w, wiv) in b.wr if (w.isdma and dma) or not self._covers(iv, wiv)]
            if len(b.wr) > 200:
                for w, _ in b.wr:
                    if not (w.isdma and dma):
                        deps.add(w)
                b.wr = [(w, wiv) for (w, wiv) in b.wr if (w.isdma and dma)][-200:]
                iv = None
            b.wr.append((o, iv))
        for b, iv in racc:
            if len(b.rd) > 200:
                for r, _ in b.rd:
                    deps.add(r)
                b.rd = []
                iv = None
            b.rd.append((o, iv))
        for d in deps:
            if d is o:
                continue
            if d.eng == "pe" and eng == "pe" and not d.isdma and not dma:
                o.odeps.append(d)
                continue
            d.sig = True
            o.deps.append(d)
        self.streams[eng].append(o)
        self.all_ops.append(o)
        if dma:
            self.pending_dma.append(o)
        return o

    def barrier(self):
        lasts = []
        for e in ENGS:
            for o in reversed(self.streams[e]):
                if not o.isdma and o.fn is not None:
                    lasts.append(o)
                    break
        pend = list(self.pending_dma)
        self.pending_dma = []
        for e in ENGS:
            o = Op(e, None, False)
            o.idx = len(self.all_ops)
            for d in lasts + pend:
                d.sig = True
                o.deps.append(d)
            self.streams[e].append(o)
            self.all_ops.append(o)

    def reschedule(self):
        import heapq
        new_streams = {e: [] for e in ENGS}
        pos = {e: 0 for e in ENGS}
        done_ids = set()
        while True:
            seg = {}
            more = False
            for e in ENGS:
                st = self.streams[e]
                i = pos[e]
                j = i
                while j < len(st) and st[j].fn is not None:
                    j += 1
                seg[e] = st[i:j]
                if j < len(st):
                    more = True
            ops = [o for e in ENGS for o in seg[e]]
            inseg = set(id(o) for o in ops)
            succ = {id(o): [] for o in ops}
            nun = {}
            for o in ops:
                n = 0
                for d in list(o.deps) + list(o.odeps):
                    if id(d) in inseg:
                        succ[id(d)].append(o)
                        n += 1
                nun[id(o)] = n
            cp = {}
            for o in sorted(ops, key=lambda q: -q.idx):
                m_ = 0.0
                for s_ in succ[id(o)]:
                    v = cp[id(s_)]
                    if v > m_:
                        m_ = v
                cp[id(o)] = o.cost + o.lat + m_
            ready = {e: [] for e in ENGS}
            for o in ops:
                if nun[id(o)] == 0:
                    heapq.heappush(ready[o.eng], (-cp[id(o)] if CP_PRIORITY else o.idx, o.idx, o))
            free = {e: 0.0 for e in ENGS}
            comp = []
            now = 0.0
            nsched = 0
            fabric = 0.0
            while True:
                for e in ENGS:
                    if ready[e] and free[e] <= now:
                        _, _, o = heapq.heappop(ready[e])
                        new_streams[e].append(o)
                        nsched += 1
                        free[e] = now + o.cost
                        if o.isdma:
                            t0_ = max(now + o.cost, fabric)
                            fabric = t0_ + o.lat
                            heapq.heappush(comp, (fabric + 4000.0, o.idx, o))
                        else:
                            heapq.heappush(comp, (now + o.cost + o.lat, o.idx, o))
                nxt = [free[e] for e in ENGS if ready[e] and free[e] > now]
                if not comp and not nxt:
                    break
                tnext = min([comp[0][0]] if comp else []) if comp else None
                cand = nxt + ([comp[0][0]] if comp else [])
                now = min(cand)
                while comp and comp[0][0] <= now:
                    _, _, o = heapq.heappop(comp)
                    for s_ in succ[id(o)]:
                        nun[id(s_)] -= 1
                        if nun[id(s_)] == 0:
                            heapq.heappush(ready[s_.eng], (-cp[id(s_)] if CP_PRIORITY else s_.idx, s_.idx, s_))
            assert nsched == len(ops), (nsched, len(ops))
            for e in ENGS:
                pos[e] += len(seg[e])
                st = self.streams[e]
                if pos[e] < len(st):
                    new_streams[e].append(st[pos[e]])
                    pos[e] += 1
            if not more:
                break
        for e in ENGS:
            assert len(new_streams[e]) == len(self.streams[e]), (e, len(new_streams[e]), len(self.streams[e]))
        self.streams = new_streams

    def emit(self, nc):
        import contextlib
        stack = contextlib.ExitStack()
        with stack:
            counts = {e: 0 for e in ENGS}
            n_epochs = {e: 1 for e in ENGS}
            for o in self.all_ops:
                if o.isdma or not o.sig:
                    continue
                counts[o.eng] += 1
            for e in ENGS:
                n_epochs[e] = max(1, (counts[e] + EPOCH - 1) // EPOCH)
            esems = {e: [stack.enter_context(nc.semaphore(f"s_{e}{i}")) for i in range(n_epochs[e])] for e in ENGS}
            dsems = {e: [stack.enter_context(nc.semaphore(f"s_dma_{e}{i}")) for i in range(N_DMA_SEMS)] for e in ("sp", "pool", "act")}
            for e in ENGS:
                k = 0
                dcnt = 0
                for o in self.streams[e]:
                    if o.isdma:
                        o.sem = dsems[e][dcnt % N_DMA_SEMS]
                        o.prev_val = 16 * (dcnt // N_DMA_SEMS)
                        o.val = o.prev_val + 16
                        dcnt += 1
                    elif o.sig:
                        o.sem = esems[e][k // EPOCH]
                        o.val = (k % EPOCH) + 1
                        k += 1
            block = stack.enter_context(nc.Block())

            def run_stream(ename, eng):
                waited = {}
                pro = getattr(self, "prologue", {}).get(ename)
                if pro is not None:
                    pro(eng)
                for o in self.streams[ename]:
                    for d in o.deps:
                        key = id(d.sem)
                        if waited.get(key, 0) >= d.val:
                            continue
                        eng.wait_ge(d.sem, d.val)
                        waited[key] = d.val
                    if o.isdma and o.prev_val > 0:
                        key = id(o.sem)
                        if waited.get(key, 0) < o.prev_val:
                            eng.wait_ge(o.sem, o.prev_val)
                            waited[key] = o.prev_val
                    if o.fn is None:
                        continue
                    ins = o.fn(eng)
                    if o.isdma:
                        ins.then_inc(o.sem, 16)
                    elif o.sig:
                        ins.then_inc(o.sem, 1)

            @block.tensor
            def _(eng):
                run_stream("pe", eng)

            @block.scalar
            def _(eng):
                run_stream("act", eng)

            @block.vector
            def _(eng):
                run_stream("dve", eng)

            @block.gpsimd
            def _(eng):
                run_stream("pool", eng)

            @block.sync
            def _(eng):
                run_stream("sp", eng)


class Arena:
    def __init__(self, nc, nbytes):
        self.h = nc.alloc_sbuf_tensor("arena", [128, nbytes], U8)
        self.nbytes = nbytes
        self.off = 0

    def alloc(self, shape, dtype, name="", nparts=128):
        n = int(np.prod(shape)) * DTSIZE[dtype]
        off = (self.off + 31) // 32 * 32
        assert off + n <= self.nbytes, (name, off, n, self.nbytes)
        self.off = off + n
        ap = self.h[0:nparts, off:off + n].bitcast(dtype)
        if len(shape) == 2:
            ap = ap.rearrange("p (a b) -> p a b", b=shape[1])
        elif len(shape) == 3:
            ap = ap.rearrange("p (a b c) -> p a b c", b=shape[1], c=shape[2])
        return T(ap, Buf(name))

    def mark(self):
        return self.off

    def release(self, m):
        self.off = m


D = 1024
SEQ = 2048
NT = 16
NTG = 4
P_IN = 3348
ALPHA = 4 ** 0.25
LN_EPS = 1e-5
RMS_EPS = 1e-6
N_EXP = 32
RESCHEDULE = True
CP_PRIORITY = True

C_CONV = 0
C_GLA = 768
C_SSD = 1552
C_DIFF = 2580

PP_OFF = {}
_o = 0
for _n, _w in (("conv_w", 6), ("gla_b", 128), ("gla_g", 256), ("gla_wlr", 128), ("ssd_cw", 24), ("ssd_cb", 6),
               ("ssd_alog", 4), ("ssd_d", 256), ("ssd_dtb", 4), ("ssd_g", 256), ("diff_l", 128), ("diff_g", 256),
               ("ln1_g", 1024), ("ln1_b", 1024), ("ln2_g", 1024), ("ln2_b", 1024)):
    PP_OFF[_n] = (_o, _w)
    _o += _w
NPP = _o
NPPS = PP_OFF["ln1_g"][0]

CO_OFF = {}
_o = 0
for _n, _w in (("ident", 128), ("triU", 128), ("triUn16", 128), ("trisL", 128), ("ones", 128), ("hm", 4), ("triUs", 128), ("thr16", 16), ("bthr", 48), ("pc", 8)):
    CO_OFF[_n] = (_o, _w)
    _o += _w
NCO = _o


def host_consts():
    c = np.zeros((128, NCO), np.float32)
    p = np.arange(128)[:, None]
    j = np.arange(128)[None, :]
    c[:, CO_OFF["ident"][0]:CO_OFF["ident"][0] + 128] = (p == j)
    c[:, CO_OFF["triU"][0]:CO_OFF["triU"][0] + 128] = (p <= j)
    c[:, CO_OFF["triUn16"][0]:CO_OFF["triUn16"][0] + 128] = (p <= j) * np.float32(-1.0 / 16.0)
    c[:, CO_OFF["trisL"][0]:CO_OFF["trisL"][0] + 128] = (p > j)
    c[:, CO_OFF["ones"][0]:CO_OFF["ones"][0] + 128] = 1.0
    c[:, CO_OFF["hm"][0]:CO_OFF["hm"][0] + 4] = ((p // 32) == np.arange(4)[None, :])
    c[:, CO_OFF["triUs"][0]:CO_OFF["triUs"][0] + 128] = (p < j)
    c[:, CO_OFF["thr16"][0]:CO_OFF["thr16"][0] + 16] = 512.0 * np.arange(16)[None, :]
    c[:, CO_OFF["bthr"][0]:CO_OFF["bthr"][0] + 48] = 512.0 * np.arange(48)[None, :]
    c[:, CO_OFF["pc"][0]:CO_OFF["pc"][0] + 8] = 128.0 * np.arange(8)[None, :] + p
    return c


def host_pp(inp, l):
    pp = np.zeros((128, NPP), np.float32)

    def put(name, arr):
        o, w = PP_OFF[name]
        assert arr.shape == (128, w), (name, arr.shape)
        pp[:, o:o + w] = arr

    def row(v):
        return np.broadcast_to(np.asarray(v, np.float32)[None, :], (128, len(v)))

    cw = inp["conv_w"][l]
    put("conv_w", np.stack([cw[k, fc * 128:(fc + 1) * 128] for fc in range(2) for k in range(3)], axis=1))
    put("gla_b", row(inp["gla_b_lr"][l]))
    put("gla_g", row(np.tile(inp["gla_norm_g"][l], 4)))
    wl = np.zeros((128, 128), np.float32)
    wl[:16] = inp["gla_w_lr"][l]
    put("gla_wlr", wl)
    sw = inp["ssd_conv_w"][l]
    put("ssd_cw", np.stack([sw[k, c6 * 128:(c6 + 1) * 128] for c6 in range(6) for k in range(4)], axis=1))
    sb = inp["ssd_conv_b"][l]
    put("ssd_cb", np.stack([sb[c6 * 128:(c6 + 1) * 128] for c6 in range(6)], axis=1))
    put("ssd_alog", row(inp["ssd_a_log"][l]))
    put("ssd_d", row(np.repeat(inp["ssd_d"][l], 64)))
    put("ssd_dtb", row(inp["ssd_dt_bias"][l]))
    put("ssd_g", row(inp["ssd_norm_g"][l]))
    put("diff_l", row(np.concatenate([inp["diff_lq1"][l], inp["diff_lk1"][l], inp["diff_lq2"][l], inp["diff_lk2"][l]])))
    put("diff_g", row(np.tile(inp["diff_norm_g"][l], 4)))
    for n in ("ln1_g", "ln1_b", "ln2_g", "ln2_b"):
        put(n, row(inp[n][l]))
    return pp


class Prog:
    def __init__(self, nseq, n_layers, with_moe, stages="cgsd", dbg=False):
        self.nseq, self.n_layers, self.with_moe, self.stages, self.dbg = nseq, n_layers, with_moe, stages, dbg
        nc = self.nc = bass.Bass("TRN2", target_bir_lowering=False)
        ntok = nseq * SEQ
        dt = nc.dram_tensor
        self.x_d = dt("x", [ntok, D], F32, kind="ExternalInput").ap()
        self.win_d = dt("w_in", [2, D, P_IN], F32, kind="ExternalInput").ap()
        self.wo_d = dt("w_o", [2, D, D], F32, kind="ExternalInput").ap()
        self.pp_d = dt("pp", [2 * 128, NPP], F32, kind="ExternalInput").ap()
        self.co_d = dt("co", [128, NCO], F32, kind="ExternalInput").ap()
        if with_moe:
            self.rt_d = dt("rt", [2, D, 36], F32, kind="ExternalInput").ap()
            self.wg_d = dt("w_gate", [2, N_EXP, D, 512], F32, kind="ExternalInput").ap()
            self.wu_d = dt("w_up", [2, N_EXP, D, 512], F32, kind="ExternalInput").ap()
            self.wd_d = dt("w_down", [2, N_EXP, 512, D], F32, kind="ExternalInput").ap()
        self.y_d = dt("y", [ntok, D], F32, kind="ExternalOutput").ap()
        if dbg:
            self.dbg_d = dt("dbg", [ntok, D], F32, kind="ExternalOutput").ap()
        self.S = Sched()
        self.A = Arena(nc, 207 * 1024)
        self.banks = []
        for i in range(8):
            b = Buf(f"ps{i}")
            b.psum = True
            self.banks.append(T(nc.alloc_psum_tensor(f"ps{i}", [128, 512], F32)[:, :], b))
        self.pool = {"free": list(range(8)), "rr": 0}
        self.bounds = set()
        self.bregs = {}
        self.build()
        self.S.barrier()

        def pool_prologue(eng):
            for bv in sorted(self.bounds):
                r = nc.alloc_register(mybir.EngineType.Pool, f"bnd{bv}")
                eng.reg_mov(r, int(bv))
                self.bregs[bv] = r
        self.S.prologue = {"pool": pool_prologue}
        if RESCHEDULE:
            self.S.reschedule()
        self.S.emit(nc)

    def ps(self):
        pool = self.pool
        i = pool["free"][pool["rr"] % len(pool["free"])]
        pool["rr"] += 1
        return self.banks[i]

    def reserve(self):
        i = self.pool["free"].pop()
        return i, self.banks[i]

    def unreserve(self, i):
        self.pool["free"].append(i)

    def run_gens(self, specs):
        active = [[g, w, {"free": list(banks), "rr": 0}] for g, w, banks in specs]
        save = self.pool
        while active:
            for item in list(active):
                g, w, pool = item
                self.pool = pool
                for _ in range(w):
                    try:
                        next(g)
                    except StopIteration:
                        active.remove(item)
                        break
        self.pool = save

    @staticmethod
    def _a(x):
        return x.ap if isinstance(x, T) else x

    @staticmethod
    def _bufs(*xs):
        return [x for x in xs if isinstance(x, T)]

    @staticmethod
    def _fs(x):
        x = x.ap if isinstance(x, T) else x
        try:
            return float(x.free_size())
        except Exception:
            return 256.0

    def mm(self, out, lhsT, rhs, start=True, stop=True):
        a = self._a
        n = self._fs(rhs)
        c = max(64.0, n) / 2.4 + 40.0 + self._fs(lhsT) / 2.4 * 0.5
        if a(rhs).dtype == F32:
            c *= 4.0
        self.S.op("pe", lambda e: e.matmul(a(out), a(lhsT), a(rhs), start=start, stop=stop),
                  reads=self._bufs(lhsT, rhs), writes=self._bufs(out), cost=c, lat=0.0)

    def tr(self, out, in_):
        a = self._a
        self.S.op("pe", lambda e: e.transpose(a(out), a(in_), a(self.identb)),
                  reads=self._bufs(in_, self.identb), writes=self._bufs(out), cost=120.0, lat=0.0)

    def act(self, out, in_, func=None, bias=None, scale=None, accum=None, eng="act"):
        a = self._a
        kw = {}
        if bias is not None:
            kw["bias"] = a(bias)
        if scale is not None:
            kw["scale"] = a(scale)
        if accum is not None:
            kw["accum_out"] = a(accum)
        f = func if func is not None else AF.Copy
        self.S.op("act", lambda e: e.activation(a(out), a(in_), f, **kw),
                  reads=self._bufs(in_, bias, scale), writes=self._bufs(out, accum), cost=200.0 + 0.85 * self._fs(out), lat=0.0)

    def tt(self, out, in0, in1, op, eng="dve"):
        a = self._a
        self.S.op(eng, lambda e: e.tensor_tensor(a(out), a(in0), a(in1), op),
                  reads=self._bufs(in0, in1), writes=self._bufs(out), cost=self._vc(eng, out), lat=0.0)

    def ts(self, out, in0, s1, op0, s2=None, op1=None, eng="dve"):
        a = self._a
        if op1 is None:
            self.S.op(eng, lambda e: e.tensor_scalar(a(out), a(in0), a(s1), None, op0),
                      reads=self._bufs(in0, s1), writes=self._bufs(out), cost=self._vc(eng, out), lat=0.0)
        else:
            self.S.op(eng, lambda e: e.tensor_scalar(a(out), a(in0), a(s1), a(s2), op0, op1),
                      reads=self._bufs(in0, s1, s2), writes=self._bufs(out), cost=self._vc(eng, out), lat=0.0)

    def stt(self, out, in0, scalar, in1, op0, op1, accum=None):
        a = self._a
        if accum is None:
            self.S.op("dve", lambda e: e.scalar_tensor_tensor(a(out), a(in0), a(scalar), a(in1), op0, op1),
                      reads=self._bufs(in0, scalar, in1), writes=self._bufs(out), cost=self._vc("dve", out), lat=0.0)
        else:
            self.S.op("dve", lambda e: e.scalar_tensor_tensor(a(out), a(in0), a(scalar), a(in1), op0, op1, accum_out=a(accum)),
                      reads=self._bufs(in0, scalar, in1), writes=self._bufs(out, accum), cost=self._vc("dve", out) + 100.0, lat=0.0)

    def silu(self, out, x):
        self.act(out, x, AF.Exp, scale=-1.0)
        self.act(out, out, AF.Ln, bias=self.oner)
        self.act(out, out, AF.Exp, scale=-1.0)
        self.tt(out, out, x, ALU.mult)

    def _vc(self, eng, x):
        n = self._fs(x)
        return (400.0 + 6.0 * n) if eng == "pool" else (160.0 + 1.0 * n)

    def copy(self, out, in_, eng="dve"):
        a = self._a
        if eng == "act":
            self.S.op("act", lambda e: e.copy(a(out), a(in_)), reads=self._bufs(in_), writes=self._bufs(out),
                      cost=200.0 + 0.85 * self._fs(out), lat=0.0)
        else:
            self.S.op(eng, lambda e: e.tensor_copy(a(out), a(in_)), reads=self._bufs(in_), writes=self._bufs(out),
                      cost=self._vc(eng, out), lat=0.0)

    def memset(self, out, v, eng="pool"):
        a = self._a
        self.S.op(eng, lambda e: e.memset(a(out), v), writes=self._bufs(out), cost=self._vc(eng, out), lat=0.0)

    def reduce(self, out, in_, op=ALU.add):
        a = self._a
        self.S.op("dve", lambda e: e.tensor_reduce(a(out), a(in_), AX.X, op), reads=self._bufs(in_), writes=self._bufs(out),
                  cost=self._vc("dve", in_), lat=0.0)

    def recip(self, out, in_):
        a = self._a
        self.S.op("dve", lambda e: e.reciprocal(a(out), a(in_)), reads=self._bufs(in_), writes=self._bufs(out),
                  cost=200.0 + 7.0 * self._fs(out), lat=0.0)

    def dma(self, out, in_, q="sp"):
        a = self._a
        nb = self._fs(out) * 128.0 * 4.0
        self.S.op(q, lambda e: e.dma_start(out=a(out), in_=a(in_)), reads=self._bufs(in_), writes=self._bufs(out), dma=True,
                  cost=(1000.0 if q == "pool" else 150.0), lat=nb / 300.0)

    def co(self, name):
        o, w = CO_OFF[name]
        return self.CO[:, o:o + w]

    def ppv(self, name, a=0, b=None):
        o, w = PP_OFF[name]
        b = w if b is None else b
        return self.PP[:, o + a:o + b]

    def load_w(self, Wt, dram2d, c0, ncols, kchunks=8):
        src = dram2d[:, c0:c0 + ncols].rearrange("(c p) n -> p c n", p=128)
        self.dma(Wt[:, 0:kchunks, 0:ncols], src, q="pool")

    def proj_fm(self, ps, W, c0, ncol, tg):
        for c in range(8):
            self.mm(ps[0:ncol, :], W[:, c, c0:c0 + ncol], self.XT[:, c, tg * 512:(tg + 1) * 512], start=(c == 0), stop=(c == 7))

    def proj_tm(self, ps, W, c0, ncol, t):
        for c in range(8):
            self.mm(ps[:, 0:ncol], self.XT[:, c, t * 128:(t + 1) * 128], W[:, c, c0:c0 + ncol], start=(c == 0), stop=(c == 7))

    def make_XT(self, src, s_):
        A = self.A
        xf = [A.alloc([1024], F32, "xf0")] * 2
        xb = [A.alloc([1024], BF16, f"xb{i}") for i in range(2)]
        for t in range(NT):
            r0 = s_ * SEQ + t * 128
            self.dma(xf[t % 2], src[r0:r0 + 128, :])
            self.copy(xb[t % 2], xf[t % 2], eng="act")
            for hh in range(2):
                ps = self.ps()
                for k in range(4):
                    c = hh * 4 + k
                    self.mm(ps[:, k * 128:(k + 1) * 128], xb[t % 2][:, c * 128:(c + 1) * 128], self.identb)
                self.copy(T(self.XT.ap[:, hh * 4:hh * 4 + 4, t * 128:(t + 1) * 128], self.XT.buf),
                          T(ps.ap.rearrange("p (k j) -> p k j", j=128), ps.buf), eng=("act" if hh == 0 else "dve"))

    def norm_tr(self, o_sb, ng, grow, ytc0, t, tmp, extra=None, post=None):
        gs = 256 // ng
        sq, ss, ybf = tmp["sq"], tmp["ss"], tmp["ybf"]
        self.tt(sq, o_sb, o_sb, ALU.mult)
        self.reduce(ss[:, 0:ng], sq.ap.rearrange("p (g e) -> p g e", e=gs) if False else T(sq.ap.rearrange("p (g e) -> p g e", e=gs), sq.buf))
        self.act(ss[:, 0:ng], ss[:, 0:ng], AF.Ln, bias=self.epsr, scale=1.0 / gs)
        self.act(ss[:, 0:ng], ss[:, 0:ng], AF.Exp, scale=-0.5)
        o3 = T(o_sb.ap.rearrange("p (g e) -> p g e", e=gs), o_sb.buf)
        s3 = T(sq.ap.rearrange("p (g e) -> p g e", e=gs), sq.buf)
        rb = T(ss.ap[:, 0:ng].unsqueeze(2).to_broadcast([128, ng, gs]), ss.buf)
        self.tt(s3, o3, rb, ALU.mult)
        if extra is not None:
            self.tt(sq, sq, extra, ALU.mult)
        if post is not None:
            self.stt(ybf, sq, float(post), grow, ALU.mult, ALU.mult)
        else:
            self.tt(ybf, sq, grow, ALU.mult)
        ps = self.ps()
        for j in range(2):
            self.mm(ps[:, j * 128:(j + 1) * 128], ybf[:, j * 128:(j + 1) * 128], self.identb)
        self.copy(T(self.YT.ap[:, ytc0:ytc0 + 2, t * 128:(t + 1) * 128], self.YT.buf),
                  T(ps.ap[:, 0:256].rearrange("p (j k) -> p j k", k=128), ps.buf), eng="act")

    def conv_mixer(self, l):
        A = self.A
        W = self.Wb[1]
        self.load_w(W, self.win_d[l], C_CONV, 768)
        m = A.mark()
        cu = A.alloc([2050], F32, "cu")
        Bf = A.alloc([2048], BF16, "Bf")
        acc = A.alloc([2048], F32, "acc")
        tmp = [A.alloc([512], F32, "ctmp0")] * 2
        self.memset(cu[:, 0:2], 0.0)
        for fc in range(2):
            for tg in range(NTG):
                pu, pc, pb = self.ps(), self.ps(), self.ps()
                self.proj_fm(pu, W, fc * 128, 128, tg)
                self.proj_fm(pc, W, 512 + fc * 128, 128, tg)
                self.proj_fm(pb, W, 256 + fc * 128, 128, tg)
                tm = tmp[tg % 2]
                self.copy(tm, pu, eng="act")
                self.tt(cu[:, 2 + tg * 512:2 + (tg + 1) * 512], tm, pc, ALU.mult)
                self.copy(Bf[:, tg * 512:(tg + 1) * 512], pb, eng="act")
                yield
            w = [self.ppv("conv_w", fc * 3 + k, fc * 3 + k + 1) for k in range(3)]
            self.act(acc, cu[:, 0:2048], AF.Copy, scale=w[0])
            self.stt(acc, cu[:, 1:2049], w[1], acc, ALU.mult, ALU.add)
            self.stt(acc, cu[:, 2:2050], w[2], acc, ALU.mult, ALU.add)
            self.tt(self.YT[:, fc, :], acc, Bf, ALU.mult)
            yield

    def gla_mixer(self, l):
        A = self.A
        W = self.Wb[0]
        self.load_w(W, self.win_d[l], C_GLA, 784)
        m = A.mark()
        qf = A.alloc([2048], F32, "qf")
        kf = A.alloc([2048], F32, "kf")
        lrT = A.alloc([2048], F32, "lrT")
        cumT = A.alloc([2048], F32, "cumT")
        big = lrT
        qdm = [A.alloc([2048], BF16, f"qdm{h}") for h in range(4)]
        ki = A.alloc([2048], BF16, "ki")
        keT = A.alloc([2048], BF16, "keT")
        ke_tok = A.alloc([16, 128], BF16, "ke_tok")
        dec = A.alloc([16], F32, "dec")
        zb = [A.alloc([128], F32, "zb0")] * 2
        Sf = A.alloc([256], F32, "Sf")
        Sb = A.alloc([256], BF16, "Sb")
        vtok = [A.alloc([256], BF16, f"vtok{i}") for i in range(2)]
        sg = [A.alloc([256], F32, "sg0")] * 2
        attm = [A.alloc([4, 128], BF16, f"attm{i}") for i in range(2)]
        osb = [A.alloc([256], F32, "osb0")] * 2
        tmp = [dict(sq=A.alloc([256], F32, "sq0"), ss=A.alloc([4], F32, "ss0"), ybf=A.alloc([256], BF16, "ybf0"))] * 2
        for tg in range(NTG):
            p1, p2, p3 = self.ps(), self.ps(), self.ps()
            self.proj_fm(p1, W, 0, 128, tg)
            self.proj_fm(p2, W, 128, 128, tg)
            self.proj_fm(p3, W, 768, 16, tg)
            self.copy(qf[:, tg * 512:(tg + 1) * 512], p1, eng="act")
            self.copy(kf[:, tg * 512:(tg + 1) * 512], p2, eng="dve")
            self.copy(lrT[0:16, tg * 512:(tg + 1) * 512], p3[0:16, :], eng="act")
            yield
        wlr = self.ppv("gla_wlr")
        blr = self.ppv("gla_b")
        for tg in range(NTG):
            pci, pc = self.reserve()
            for k in range(4):
                t = tg * 4 + k
                pz = self.ps()
                self.mm(pz[:, 0:128], lrT[0:16, t * 128:(t + 1) * 128], wlr[0:16, :])
                z = zb[t % 2]
                self.tt(z, pz[:, 0:128], blr, ALU.add)
                self.act(z, z, AF.Exp, scale=-1.0)
                self.act(z, z, AF.Ln, bias=self.oner)
                self.mm(pc[:, k * 128:(k + 1) * 128], z, self.co("triUn16"))
            self.copy(cumT[:, tg * 512:(tg + 1) * 512], pc, eng="act")
            self.unreserve(pci)
            yield
        cl = T(cumT.ap.rearrange("p (t k) -> p t k", k=128)[:, :, 127], cumT.buf)
        self.act(dec, cl, AF.Exp)
        self.act(big, cumT, AF.Exp)
        self.tt(qf, qf, big, ALU.mult)
        for h in range(4):
            self.ts(qdm[h], qf, self.co("hm")[:, h:h + 1], ALU.mult, float(32 ** -0.5), ALU.mult)
        self.act(big, cumT, AF.Exp, scale=-1.0)
        self.tt(ki, kf, big, ALU.mult)
        for t in range(NT):
            sl = slice(t * 128, (t + 1) * 128)
            z = zb[t % 2]
            self.act(z, cumT[:, sl], AF.Exp, scale=-1.0, bias=cl[:, t:t + 1])
            self.tt(keT[:, sl], kf[:, sl], z, ALU.mult)
            pt = self.ps()
            self.mm(pt[:, 0:128], keT[:, sl], self.identb)
            self.copy(ke_tok[:, t, :], pt[:, 0:128], eng="act")
            if t % 2 == 1:
                yield
        self.memset(Sf, 0.0)
        self.memset(Sb, 0.0)
        triU3 = T(self.co("triU").ap.unsqueeze(1).to_broadcast([128, 4, 128]), self.CO.buf)
        for t in range(NT):
            sl = slice(t * 128, (t + 1) * 128)
            v, g_, am, ob = vtok[t % 2], sg[t % 2], attm[t % 2], osb[t % 2]
            pv = self.ps()
            self.proj_tm(pv, W, 256, 256, t)
            self.copy(v, pv[:, 0:256], eng="act")
            pg = self.ps()
            self.proj_tm(pg, W, 512, 256, t)
            self.silu(g_, pg[:, 0:256])
            yield
            pa = self.ps()
            for h in range(4):
                self.mm(pa[:, h * 128:(h + 1) * 128], ki[:, sl], qdm[h][:, sl])
            self.tt(am, T(pa.ap.rearrange("p (h i) -> p h i", i=128), pa.buf), triU3, ALU.mult)
            yield
            po = self.ps()
            for h in range(4):
                hs = slice(h * 64, (h + 1) * 64)
                self.mm(po[:, hs], qdm[h][:, sl], Sb[:, hs], start=True, stop=False)
                self.mm(po[:, hs], am[:, h, :], v[:, hs], start=False, stop=True)
            yield
            pd = self.ps()
            self.mm(pd[:, 0:256], ke_tok[:, t, :], v)
            self.stt(Sf, Sf, dec[:, t:t + 1], pd[:, 0:256], ALU.mult, ALU.add)
            self.copy(Sb, Sf, eng="act")
            self.copy(ob, po[:, 0:256], eng="act")
            yield
            self.norm_tr(ob, 4, self.ppv("gla_g"), 2, t, tmp[t % 2], extra=g_)
            yield

    def ssd_mixer(self, l):
        A = self.A
        W = self.Wb[1]
        self.load_w(W, self.win_d[l], C_SSD, 1028)
        m = A.mark()
        pre = [A.alloc([2051], F32, "pre0")] * 2
        acc = A.alloc([2048], F32, "sacc")
        cv = [A.alloc([2048], BF16, f"cv{i}") for i in range(6)]
        dtt = A.alloc([16, 4], F32, "dtt")
        dtA = A.alloc([16, 4], F32, "dtA")
        arow = A.alloc([4], F32, "arow")
        STf = A.alloc([256], F32, "STf")
        STb = A.alloc([256], BF16, "STb")
        xs_tok = [A.alloc([256], F32, f"xs_tok{i}") for i in range(2)]
        B_tok = [A.alloc([256], BF16, f"B_tok{i}") for i in range(2)]
        xdt = [A.alloc([256], F32, f"xdt{i}") for i in range(2)]
        xdtb = [A.alloc([256], BF16, f"xdtb{i}") for i in range(2)]
        xdd = [A.alloc([256], BF16, f"xdd{i}") for i in range(2)]
        cbm = [A.alloc([2, 128], F32, f"cbm{i}") for i in range(2)]
        lhsD = [A.alloc([128], F32, f"lhsD{i}") for i in range(2)]
        eD = [A.alloc([128], F32, f"eD{i}") for i in range(2)]
        MT = [A.alloc([128], BF16, f"MT{i}") for i in range(2)]
        ecum = [A.alloc([4], F32, f"ecum{i}") for i in range(2)]
        decs = [A.alloc([4], F32, f"decs{i}") for i in range(2)]
        t1 = [A.alloc([256], F32, f"t1{i}") for i in range(2)]
        t2 = [A.alloc([256], F32, "t2_0")] * 2
        sz = [A.alloc([256], F32, "sz_0")] * 2
        tmp = [dict(sq=A.alloc([256], F32, f"ssq{i}"), ss=A.alloc([4], F32, f"sss{i}"), ybf=A.alloc([256], BF16, f"sybf{i}")) for i in range(2)]
        self.memset(pre[0][:, 0:3], 0.0)
        for c6 in range(6):
            pr = pre[c6 % 2]
            for tg in range(NTG):
                ps = self.ps()
                self.proj_fm(ps, W, 256 + c6 * 128, 128, tg)
                self.copy(pr[:, 3 + tg * 512:3 + (tg + 1) * 512], ps, eng=("act" if tg % 2 == 0 else "dve"))
            w = [self.ppv("ssd_cw", c6 * 4 + k, c6 * 4 + k + 1) for k in range(4)]
            self.act(acc, pr[:, 0:2048], AF.Copy, scale=w[0])
            for k in range(1, 4):
                self.stt(acc, pr[:, k:k + 2048], w[k], acc, ALU.mult, ALU.add)
            self.ts(acc, acc, self.ppv("ssd_cb", c6, c6 + 1), ALU.add)
            scr = pr[:, 3:2051]
            self.act(scr, acc, AF.Exp, scale=-1.0)
            self.act(scr, scr, AF.Ln, bias=self.oner)
            self.act(scr, scr, AF.Exp, scale=-1.0)
            self.tt(cv[c6], acc, scr, ALU.mult)
            yield
        self.act(arow, self.ppv("ssd_alog"), AF.Exp)
        self.ts(arow, arow, -1.0, ALU.mult)
        for t in range(NT):
            ps = self.ps()
            self.proj_tm(ps, W, 1024, 4, t)
            self.tt(dtt[:, t, :], ps[:, 0:4], self.ppv("ssd_dtb"), ALU.add)
            if t % 4 == 3:
                yield
        self.act(dtt, dtt, AF.Exp)
        self.act(dtt, dtt, AF.Ln, bias=self.oner)
        self.tt(dtA, dtt, T(arow.ap.unsqueeze(1).to_broadcast([128, 16, 4]), arow.buf), ALU.mult)
        self.memset(STf, 0.0)
        self.memset(STb, 0.0)
        triU = self.co("triU")
        triU2 = T(triU.ap.unsqueeze(1).to_broadcast([128, 2, 128]), self.CO.buf)
        for t in range(NT):
            sl = slice(t * 128, (t + 1) * 128)
            i2 = t % 2
            ptx = self.ps()
            for j in range(2):
                self.mm(ptx[:, j * 128:(j + 1) * 128], cv[j][:, sl], self.identb)
                self.mm(ptx[:, 256 + j * 128:256 + (j + 1) * 128], cv[2 + j][:, sl], self.identb)
            self.copy(xs_tok[i2], ptx[:, 0:256], eng="act")
            self.copy(B_tok[i2], ptx[:, 256:512], eng="dve")
            x3 = T(xs_tok[i2].ap.rearrange("p (h e) -> p h e", e=64), xs_tok[i2].buf)
            dtb = T(dtt.ap[:, t, :].unsqueeze(2).to_broadcast([128, 4, 64]), dtt.buf)
            self.tt(T(xdt[i2].ap.rearrange("p (h e) -> p h e", e=64), xdt[i2].buf), x3, dtb, ALU.mult)
            self.copy(xdtb[i2], xdt[i2], eng="act")
            yield
            pcb = self.ps()
            for g in range(2):
                self.mm(pcb[:, g * 128:(g + 1) * 128], cv[2 + g][:, sl], cv[4 + g][:, sl])
            self.tt(cbm[i2], T(pcb.ap[:, 0:256].rearrange("p (g i) -> p g i", i=128), pcb.buf), triU2, ALU.mult)
            pcm = self.ps()
            self.mm(pcm[:, 0:4], triU, dtA[:, t, :])
            self.mm(pcm[:, 4:8], self.co("ones"), dtA[:, t, :])
            self.act(ecum[i2], pcm[:, 0:4], AF.Exp)
            self.act(decs[i2], pcm[:, 4:8], AF.Exp)
            yield
            pyi, py = self.reserve()
            pyoi, pyo = self.reserve()
            for hd in range(4):
                g = hd // 2
                hs = slice(hd * 64, (hd + 1) * 64)
                k2 = hd % 2
                self.ts(lhsD[k2], self.co("trisL"), dtA[:, t, hd:hd + 1], ALU.mult)
                pD = self.ps()
                self.mm(pD[:, 0:128], lhsD[k2], triU)
                self.act(eD[k2], pD[:, 0:128], AF.Exp)
                self.tt(MT[k2], eD[k2], cbm[i2][:, g, :], ALU.mult)
                self.mm(py[:, hs], MT[k2], xdtb[i2][:, hs])
                self.mm(pyo[:, hs], cv[4 + g][:, sl], STb[:, hs])
                self.ts(xdd[i2][:, hs], xdt[i2][:, hs], eD[k2][:, 127:128], ALU.mult)
                yield
            pst = self.ps()
            for g in range(2):
                gs = slice(g * 128, (g + 1) * 128)
                self.mm(pst[:, gs], B_tok[i2][:, gs], xdd[i2][:, gs])
            S3 = T(STf.ap.rearrange("p (h e) -> p h e", e=64), STf.buf)
            self.tt(S3, S3, T(decs[i2].ap.unsqueeze(2).to_broadcast([128, 4, 64]), decs[i2].buf), ALU.mult)
            self.tt(STf, STf, pst[:, 0:256], ALU.add)
            self.copy(STb, STf, eng="act")
            yield
            a3 = T(t1[i2].ap.rearrange("p (h e) -> p h e", e=64), t1[i2].buf)
            self.tt(a3, T(pyo.ap[:, 0:256].rearrange("p (h e) -> p h e", e=64), pyo.buf),
                    T(ecum[i2].ap.unsqueeze(2).to_broadcast([128, 4, 64]), ecum[i2].buf), ALU.mult)
            self.tt(t1[i2], t1[i2], py[:, 0:256], ALU.add)
            self.unreserve(pyoi)
            self.unreserve(pyi)
            self.tt(t2[i2], xs_tok[i2], self.ppv("ssd_d"), ALU.mult, eng="pool")
            self.tt(t1[i2], t1[i2], t2[i2], ALU.add)
            yield
            pz = self.ps()
            self.proj_tm(pz, W, 0, 256, t)
            self.silu(sz[i2], pz[:, 0:256])
            self.tt(t1[i2], t1[i2], sz[i2], ALU.mult)
            yield
            self.norm_tr(t1[i2], 2, self.ppv("ssd_g"), 4, t, tmp[i2])
            yield

    def diff_mixer(self, l):
        A = self.A
        W = self.Wb[0]
        self.load_w(W, self.win_d[l], C_DIFF, 768)
        m = A.mark()
        lam_init = 0.8 - 0.6 * float(np.exp(-0.3 * l))
        kT = A.alloc([2, 2048], BF16, "kT")
        vtok = A.alloc([16, 4, 65], BF16, "dvtok")
        Otok = [A.alloc([4, 256], F32, f"Otok{i}") for i in range(2)]
        qm = [[A.alloc([512], BF16, f"qm{c}{i}") for i in range(2)] for c in range(2)]
        pts = [A.alloc([512], BF16, f"pt{i}") for i in range(3)]
        lt = A.alloc([64], F32, "lt")
        lam = A.alloc([4], F32, "lam")
        rec = [A.alloc([2, 4], F32, f"rec{i}") for i in range(2)]
        o1 = [A.alloc([4, 64], F32, f"o1{i}") for i in range(2)]
        o2 = [A.alloc([4, 64], F32, f"o2{i}") for i in range(2)]
        tmp = [dict(sq=A.alloc([256], F32, f"dsq{i}"), ss=A.alloc([4], F32, f"dss{i}"), ybf=A.alloc([256], BF16, f"dybf{i}")) for i in range(2)]
        dl = self.ppv("diff_l")
        self.tt(lt[:, 0:32], dl[:, 0:32], dl[:, 32:64], ALU.mult)
        self.tt(lt[:, 32:64], dl[:, 64:96], dl[:, 96:128], ALU.mult)
        self.reduce(lam[:, 0:2], T(lt.ap.rearrange("p (a b) -> p a b", b=32), lt.buf))
        self.act(lam[:, 0:2], lam[:, 0:2], AF.Exp)
        self.tt(lam[:, 2:3], lam[:, 0:1], lam[:, 1:2], ALU.subtract)
        self.ts(lam[:, 3:4], lam[:, 2:3], float(lam_init), ALU.add, -1.0, ALU.mult)
        nlam = lam[:, 3:4]
        for kc in range(2):
            for tg in range(NTG):
                ps = self.ps()
                self.proj_fm(ps, W, 256 + kc * 128, 128, tg)
                self.copy(kT[:, kc, tg * 512:(tg + 1) * 512], ps, eng=("act" if tg % 2 else "dve"))
                yield
        self.memset(vtok, 1.0)
        for t in range(NT):
            ps = self.ps()
            self.proj_tm(ps, W, 512, 256, t)
            self.copy(T(vtok.ap[:, t, :, 0:64], vtok.buf), T(ps.ap[:, 0:256].rearrange("p (h e) -> p h e", e=64), ps.buf), eng="act")
            if t % 4 == 3:
                yield
        scale = float(32 ** -0.5)
        ptc = 0
        for qg in range(NTG):
            for h in range(4):
                qc = h // 2
                k2 = h % 2
                pq = self.ps()
                self.proj_fm(pq, W, qc * 128, 128, qg)
                for c in range(2):
                    b = (h % 2) * 2 + c
                    self.ts(qm[c][k2], pq, self.co("hm")[:, b:b + 1], ALU.mult, scale, ALU.mult)
                ib = [self.reserve(), self.reserve()]
                steps = [(c, jt) for c in range(2) for jt in range(4 * qg + 4)]

                def qk(c, jt):
                    n0 = max(0, jt - 4 * qg) * 128
                    ps = self.ps()
                    self.mm(ps[:, n0:512], kT[:, qc, jt * 128:(jt + 1) * 128], qm[c][k2][:, n0:512])
                    return ps
                cur = qk(*steps[0])
                for si, (c, jt) in enumerate(steps):
                    nxt = qk(*steps[si + 1]) if si + 1 < len(steps) else None
                    po = ib[c][1]
                    i0 = max(0, jt - 4 * qg)
                    n0 = i0 * 128
                    pt = pts[ptc % 3]
                    ptc += 1
                    self.act(pt[:, n0:512], cur[:, n0:512], AF.Exp)
                    if jt >= 4 * qg:
                        self.tt(pt[:, n0:n0 + 128], pt[:, n0:n0 + 128], self.triUb, ALU.mult, eng="pool")
                    for it in range(i0, 4):
                        self.mm(po[:, it * 65:(it + 1) * 65], pt[:, it * 128:(it + 1) * 128], vtok[:, jt, h, :],
                                start=(jt == 0 and it == 0), stop=(jt == 4 * qg + it))
                    cur = nxt
                    yield
                r = rec[k2]
                for c in range(2):
                    po = ib[c][1]
                    p3 = T(po.ap[:, 0:260].rearrange("p (i e) -> p i e", e=65), po.buf)
                    self.recip(r[:, c, :], p3[:, :, 64])
                    dst = o1[k2] if c == 0 else o2[k2]
                    self.tt(dst, p3[:, :, 0:64], T(r.ap[:, c, :].unsqueeze(2).to_broadcast([128, 4, 64]), r.buf), ALU.mult)
                self.unreserve(ib[1][0])
                self.unreserve(ib[0][0])
                self.stt(T(Otok[qg % 2].ap[:, :, h * 64:(h + 1) * 64], Otok[qg % 2].buf), o2[k2], nlam, o1[k2], ALU.mult, ALU.add)
                yield
            for k4 in range(4):
                t = 4 * qg + k4
                self.norm_tr(Otok[qg % 2][:, k4, :], 4, self.ppv("diff_g"), 6, t, tmp[t % 2], post=(1.0 - lam_init))
            yield

    def layer_norm(self, xt, grow, brow, st, gb_eng="dve", presum=False):
        junk = self.junk
        self.act(junk, xt, AF.Copy, accum=st[:, 0:1])
        self.act(junk, xt, AF.Square, accum=st[:, 1:2])
        self.ts(st[:, 2:3], st[:, 0:1], 1.0 / D, ALU.mult)
        self.tt(st[:, 3:4], st[:, 2:3], st[:, 2:3], ALU.mult)
        self.stt(st[:, 4:5], st[:, 1:2], 1.0 / D, st[:, 3:4], ALU.mult, ALU.subtract)
        self.act(st[:, 5:6], st[:, 4:5], AF.Ln, bias=self.lnepsr)
        self.act(st[:, 6:7], st[:, 5:6], AF.Exp, scale=-0.5)
        self.stt(st[:, 7:8], st[:, 2:3], -1.0, st[:, 6:7], ALU.mult, ALU.mult)
        self.act(xt, xt, AF.Identity, bias=st[:, 7:8], scale=st[:, 6:7])
        self.tt(xt, xt, grow, ALU.mult, eng=gb_eng)
        self.tt(xt, xt, brow, ALU.add, eng=gb_eng)

    def out_proj_ln1_route(self, s, l):
        A = self.A
        Wo = self.Wb[1]
        self.load_w(Wo, self.wo_d[l], 0, 1024)
        m = A.mark()
        RT = A.alloc([8, 36], BF16, "RT")
        self.dma(RT, self.rt_d[l].rearrange("(c p) n -> p c n", p=128), q="pool")
        xo = [A.alloc([1024], F32, f"xo{i}") for i in range(2)]
        xt_ = [A.alloc([1024], F32, f"xt{i}") for i in range(2)]
        xT = [A.alloc([8, 128], BF16, f"x1T{i}") for i in range(2)]
        xtb = [A.alloc([1024], BF16, f"xtb{i}") for i in range(2)]
        st = [A.alloc([16], F32, f"lnst{i}") for i in range(2)]
        LA = A.alloc([NT, 36], F32, "LA")
        gmx = A.alloc([NT], F32, "gmx")
        ohg = A.alloc([NT, 4], F32, "ohg")
        eg = A.alloc([NT, 4], F32, "eg")
        pg_ = A.alloc([NT], F32, "pg_")
        sel = A.alloc([NT, 4, 8], F32, "sel")
        el = A.alloc([NT, 8], F32, "el")
        el2 = A.alloc([NT, 8], F32, "el2")
        k1 = A.alloc([NT, 8], F32, "k1")
        k2 = A.alloc([NT, 8], F32, "k2")
        m1 = A.alloc([NT], F32, "m1")
        m2 = A.alloc([NT], F32, "m2")
        self.junk = A.alloc([1024], F32, "junk")
        LNR = A.alloc([2048], F32, "LNR")
        o_ = PP_OFF["ln1_g"][0]
        self.dma(LNR, self.pp_d[l * 128:(l + 1) * 128, o_:o_ + 2048])
        src = self.x_d if l == 0 else self.y_d
        ident = self.co("ident")
        for t in range(NT):
            gt = s * NT + t
            r0 = s * SEQ + t * 128
            i2 = t % 2
            self.dma(xo[i2], src[r0:r0 + 128, :])
            self.memset(st[i2][:, 8:10], 0.0, eng="dve")
            xt = xt_[i2]
            for half in range(2):
                ps = self.ps()
                for c in range(8):
                    self.mm(ps, self.YT[:, c, t * 128:(t + 1) * 128], Wo[:, c, half * 512:(half + 1) * 512], start=(c == 0), stop=(c == 7))
                self.stt(xt[:, half * 512:(half + 1) * 512], xo[i2][:, half * 512:(half + 1) * 512], float(ALPHA), ps, ALU.mult, ALU.add,
                         accum=st[i2][:, 8 + half:9 + half])
            self.tt(st[i2][:, 0:1], st[i2][:, 8:9], st[i2][:, 9:10], ALU.add)
            self.layer_norm(xt, LNR[:, 0:1024], LNR[:, 1024:2048], st[i2], presum=True)
            self.dma(self.X1[r0:r0 + 128, :], xt)
            self.copy(xtb[i2], xt, eng="act")
            for hh in range(2):
                ps = self.ps()
                for k in range(4):
                    c = hh * 4 + k
                    self.mm(ps[:, k * 128:(k + 1) * 128], xtb[i2][:, c * 128:(c + 1) * 128], self.identb)
                self.copy(T(xT[i2].ap[:, hh * 4:hh * 4 + 4, :], xT[i2].buf), T(ps.ap.rearrange("p (k j) -> p k j", j=128), ps.buf), eng="act")
            ps = self.ps()
            for c in range(8):
                self.mm(ps[:, 0:36], xT[i2][:, c, :], RT[:, c, :], start=(c == 0), stop=(c == 7))
            self.copy(LA[:, t, :], ps[:, 0:36], eng="act")
        def bc(x, shape, axis):
            return T(x.ap.unsqueeze(axis).to_broadcast(shape), x.buf)
        g0 = s * NT
        Lg = LA[:, :, 0:4]
        Le = T(LA.ap[:, :, 4:36].rearrange("p t (g j) -> p t g j", j=8), LA.buf)
        self.reduce(gmx, Lg, op=ALU.max)
        self.tt(ohg, Lg, bc(gmx, [128, NT, 4], 2), ALU.is_equal)
        self.tt(eg, Lg, bc(gmx, [128, NT, 4], 2), ALU.subtract)
        self.act(eg, eg, AF.Exp)
        self.reduce(pg_, eg)
        self.recip(pg_, pg_)
        self.tt(sel, Le, bc(ohg, [128, NT, 4, 8], 3), ALU.mult)
        self.reduce(el, T(sel.ap.rearrange("p t g j -> p t j g"), sel.buf))
        self.reduce(m1, el, op=ALU.max)
        self.tt(k1, el, bc(m1, [128, NT, 8], 2), ALU.is_equal)
        self.stt(el2, k1, -1e30, el, ALU.mult, ALU.add)
        self.reduce(m2, el2, op=ALU.max)
        self.tt(k2, el2, bc(m2, [128, NT, 8], 2), ALU.is_equal)
        self.tt(m2, m2, m1, ALU.subtract)
        self.act(m2, m2, AF.Exp)
        self.ts(m2, m2, 1.0, ALU.add)
        self.recip(m2, m2)
        self.tt(self.GT[:, g0:g0 + NT, 0], m2, pg_, ALU.mult)
        self.tt(self.GT[:, g0:g0 + NT, 1], pg_, self.GT[:, g0:g0 + NT, 0], ALU.subtract)
        for (OH, kk) in ((self.OH1, k1), (self.OH2, k2)):
            dst = T(OH.ap[:, g0:g0 + NT, :].rearrange("p t (g j) -> p t g j", j=8), OH.buf)
            self.tt(dst, bc(kk, [128, NT, 4, 8], 2), bc(ohg, [128, NT, 4, 8], 3), ALU.mult)
        self.S.barrier()
        A.release(m)

    def moe_sparse(self, l):
        A = self.A
        m = A.mark()
        NTT = self.nseq * NT
        NB = self.NB
        ones = self.co("ones")
        RK = A.alloc([NTT, 32], F32, "RK")
        At = [A.alloc([32], F32, f"At{i}") for i in range(2)]
        Acum = A.alloc([32], F32, "Acum")
        cnt = A.alloc([32], F32, "cnt")
        self.memset(Acum, 0.0)
        for gt in range(NTT):
            a = At[gt % 2]
            self.tt(a, self.OH1[:, gt, :], self.OH2[:, gt, :], ALU.add)
            ps = self.ps()
            self.mm(ps[:, 0:32], ones, Acum, start=True, stop=False)
            self.mm(ps[:, 0:32], self.co("triUs"), a, start=False, stop=True)
            self.copy(RK[:, gt, :], ps[:, 0:32], eng="act")
            self.tt(Acum, Acum, a, ALU.add)
        ps = self.ps()
        self.mm(ps[:, 0:32], ones, Acum)
        self.copy(cnt, ps[:, 0:32], eng="act")
        cmp = A.alloc([32, 16], F32, "cmp")
        pad = A.alloc([32], F32, "pad")
        sc = [A.alloc([32], F32, f"sc{i}") for i in range(2)]
        pstart = A.alloc([32], F32, "pstart")
        thr = self.co("thr16")
        self.tt(cmp, T(cnt.ap.unsqueeze(2).to_broadcast([128, 32, 16]), cnt.buf),
                T(thr.ap.unsqueeze(1).to_broadcast([128, 32, 16]), self.CO.buf), ALU.is_gt)
        self.reduce(pad, cmp)
        self.ts(pad, pad, 512.0, ALU.mult)
        cur = pad
        k = 0
        for sh in (1, 2, 4, 8, 16):
            nx = sc[k % 2]
            k += 1
            self.copy(nx[:, 0:sh], cur[:, 0:sh])
            self.tt(nx[:, sh:32], cur[:, sh:32], cur[:, 0:32 - sh], ALU.add)
            cur = nx
        pend = cur
        self.tt(pstart, pend, pad, ALU.subtract)
        big = A.alloc([NTT, 32], F32, "big")
        dstf = A.alloc([NTT, 2], F32, "dstf")
        DST = A.alloc([NTT, 2], I32, "DST")
        self.tt(RK, RK, T(pstart.ap.unsqueeze(1).to_broadcast([128, NTT, 32]), pstart.buf), ALU.add)
        self.tt(big, RK, self.OH1, ALU.mult)
        self.reduce(dstf[:, :, 0], big)
        self.tt(big, RK, self.OH2, ALU.mult)
        self.reduce(dstf[:, :, 1], big)
        self.copy(DST, dstf)
        cb = A.alloc([NB, 32], F32, "cb")
        be = A.alloc([NB], F32, "be")
        inv = A.alloc([NB], F32, "inv")
        ixf = A.alloc([NB, 8], F32, "ixf")
        IXW = A.alloc([NB, 8], I32, "IXW")
        IXD = A.alloc([NB, 4], I32, "IXD")
        bthr = self.co("bthr")[:, 0:NB]
        self.tt(cb, T(pend.ap.unsqueeze(1).to_broadcast([128, NB, 32]), pend.buf),
                T(bthr.ap.unsqueeze(2).to_broadcast([128, NB, 32]), self.CO.buf), ALU.is_le)
        self.reduce(be, cb)
        self.ts(inv, bthr, pend[:, 31:32], ALU.is_ge, 4.0e6, ALU.mult)
        pc = self.co("pc")
        self.stt(be, be, 1024.0, inv, ALU.mult, ALU.add)
        self.ts(be, be, float(l * N_EXP * 1024), ALU.add)
        self.tt(ixf, T(be.ap.unsqueeze(2).to_broadcast([128, NB, 8]), be.buf),
                T(pc.ap.unsqueeze(1).to_broadcast([128, NB, 8]), self.CO.buf), ALU.add)
        self.copy(IXW, ixf)
        self.stt(be, be, 0.5, inv, ALU.mult, ALU.add)
        self.tt(ixf[:, :, 0:4], T(be.ap.unsqueeze(2).to_broadcast([128, NB, 4]), be.buf),
                T(pc.ap[:, 0:4].unsqueeze(1).to_broadcast([128, NB, 4]), self.CO.buf), ALU.add)
        self.copy(IXD, ixf[:, :, 0:4])
        xl = [A.alloc([1024], F32, f"xl{i}") for i in range(3)]
        nslot = NB * 512
        for gt in range(NTT):
            x_ = xl[gt % 3]
            self.dma(x_, self.X1[gt * 128:(gt + 1) * 128, :])
            for k2 in range(2):
                self.idma(self.XS, DST[:, gt, k2:k2 + 1], x_, scatter=True, bound=nslot - 1)
        Wg = [[A.alloc([512], BF16, f"Wg{i}_{c}") for c in range(8)] for i in range(2)]
        Wu = [[A.alloc([512], BF16, f"Wu{i}_{c}") for c in range(8)] for i in range(2)]
        Wd = [[A.alloc([1024], BF16, f"Wd{i}_{c}") for c in range(4)] for i in range(2)]
        xr = [A.alloc([4, 1024], BF16, f"xr{i}") for i in range(2)]
        xsT = [A.alloc([8, 512], BF16, f"xsT{i}") for i in range(2)]
        hT = [A.alloc([4, 512], BF16, f"hT{i}") for i in range(2)]
        sil = [A.alloc([512], F32, f"sil{i}") for i in range(2)]
        ysb = [A.alloc([1024], F32, f"ysb{i}") for i in range(3)]
        wg2 = T(self.wg_d.rearrange("l e d f -> (l e d) f"), Buf("wg", dram=True))
        wu2 = T(self.wu_d.rearrange("l e d f -> (l e d) f"), Buf("wu", dram=True))
        wd2 = T(self.wd_d.rearrange("l e f d -> (l e f) d"), Buf("wd", dram=True))
        yc = 0
        for b in range(NB):
            i2 = b % 2
            for c in range(8):
                self.idma(Wg[i2][c], IXW[:, b, c:c + 1], wg2, scatter=False, bound=2 * N_EXP * 1024 - 1)
                self.idma(Wu[i2][c], IXW[:, b, c:c + 1], wu2, scatter=False, bound=2 * N_EXP * 1024 - 1)
            for c in range(4):
                self.idma(Wd[i2][c], IXD[:, b, c:c + 1], wd2, scatter=False, bound=2 * N_EXP * 512 - 1)
            self.dma(xr[i2], T(self.XS.ap[b * 512:(b + 1) * 512, :].rearrange("(s p) d -> p s d", p=128), self.XS.buf))
            for c2 in range(4):
                ps = self.ps()
                psb = T(ps.ap.bitcast(BF16), ps.buf)
                for j in range(2):
                    c = 2 * c2 + j
                    for s4 in range(4):
                        self.tr(psb[:, j * 512 + s4 * 128:j * 512 + (s4 + 1) * 128], xr[i2][:, s4, c * 128:(c + 1) * 128])
                self.copy(T(xsT[i2].ap[:, 2 * c2:2 * c2 + 2, :], xsT[i2].buf),
                          T(psb.ap.rearrange("p (j n) -> p j n", n=512), ps.buf), eng=("act" if c2 % 2 == 0 else "dve"))
            for fc in range(4):
                pg, pu = self.ps(), self.ps()
                for c in range(8):
                    self.mm(pg, Wg[i2][c][:, fc * 128:(fc + 1) * 128], xsT[i2][:, c, :], start=(c == 0), stop=(c == 7))
                for c in range(8):
                    self.mm(pu, Wu[i2][c][:, fc * 128:(fc + 1) * 128], xsT[i2][:, c, :], start=(c == 0), stop=(c == 7))
                sl_ = sil[fc % 2]
                self.act(sl_, pg, AF.Silu)
                self.tt(hT[i2][:, fc, :], sl_, pu, ALU.mult)
            for s4 in range(4):
                y_ = ysb[yc % 3]
                yc += 1
                for half in range(2):
                    ps = self.ps()
                    for fc in range(4):
                        self.mm(ps, hT[i2][:, fc, s4 * 128:(s4 + 1) * 128], Wd[i2][fc][:, half * 512:(half + 1) * 512], start=(fc == 0), stop=(fc == 3))
                    self.copy(y_[:, half * 512:(half + 1) * 512], ps, eng=("act" if half == 0 else "dve"))
                r0 = b * 512 + s4 * 128
                self.dma(self.YS[r0:r0 + 128, :], y_)
        ya = [A.alloc([1024], F32, f"ya{i}") for i in range(2)]
        yb = [A.alloc([1024], F32, f"yb{i}") for i in range(2)]
        st = [A.alloc([16], F32, f"lnst{i}") for i in range(2)]
        self.junk = A.alloc([1024], F32, "junk")
        LNR = A.alloc([2048], F32, "LNR")
        o_ = PP_OFF["ln2_g"][0]
        self.dma(LNR, self.pp_d[l * 128:(l + 1) * 128, o_:o_ + 2048])

        def fetch(gt):
            self.dma(xl[gt % 3], self.X1[gt * 128:(gt + 1) * 128, :])
            self.idma(ya[gt % 2], DST[:, gt, 0:1], self.YS, scatter=False, bound=nslot - 1)
            self.idma(yb[gt % 2], DST[:, gt, 1:2], self.YS, scatter=False, bound=nslot - 1)
        fetch(0)
        for gt in range(NTT):
            i2 = gt % 2
            x_ = xl[gt % 3]
            self.ts(x_, x_, float(ALPHA), ALU.mult)
            self.stt(x_, ya[i2], self.GT[:, gt, 0:1], x_, ALU.mult, ALU.add)
            self.memset(st[i2][:, 0:1], 0.0, eng="dve")
            self.stt(x_, yb[i2], self.GT[:, gt, 1:2], x_, ALU.mult, ALU.add, accum=st[i2][:, 0:1])
            if gt + 1 < NTT:
                fetch(gt + 1)
            self.layer_norm(x_, LNR[:, 0:1024], LNR[:, 1024:2048], st[i2], gb_eng="dve", presum=True)
            self.dma(self.y_d[gt * 128:(gt + 1) * 128, :], x_)
        self.S.barrier()
        A.release(m)

    def idma(self, dst, idx, src, scatter, bound):
        a = self._a
        self.bounds.add(bound)
        regs = self.bregs
        bound_key = bound
        bound = None
        if scatter:
            fn = lambda e: e.indirect_dma_start(out=a(dst), out_offset=bass.IndirectOffsetOnAxis(ap=a(idx), axis=0),
                                                in_=a(src), in_offset=None, bounds_check=regs[bound_key], oob_is_err=False)
        else:
            fn = lambda e: e.indirect_dma_start(out=a(dst), out_offset=None, in_=a(src),
                                                in_offset=bass.IndirectOffsetOnAxis(ap=a(idx), axis=0),
                                                bounds_check=regs[bound_key], oob_is_err=False)
        nb = min(self._fs(dst), self._fs(src)) * 128.0 * 4.0
        self.S.op("pool", fn, reads=self._bufs(src, idx), writes=self._bufs(dst), dma=True, cost=900.0, lat=nb / 300.0)

    def next_w(self):
        self.wi += 1
        return self.Wb[self.wi % 2]

    def build(self):
        A = self.A
        nc = self.nc
        ntok = self.nseq * SEQ
        NTT = self.nseq * NT
        self.NB = (2 * ntok + N_EXP * 511) // 512 + 1
        assert self.NB <= 48
        self.X1 = T(nc.dram_tensor("x1_scr", [ntok, D], F32).ap(), Buf("X1", dram=True))
        self.XS = T(nc.dram_tensor("xs_scr", [self.NB * 512, D], BF16).ap(), Buf("XS", dram=True))
        self.YS = T(nc.dram_tensor("ys_scr", [self.NB * 512, D], F32).ap(), Buf("YS", dram=True))
        self.CO = A.alloc([NCO], F32, "CO")
        self.PP = A.alloc([NPPS], F32, "PP")
        self.identb = A.alloc([128], BF16, "identb")
        self.triUb = A.alloc([128], BF16, "triUb")
        cst = A.alloc([4], F32, "cst")
        self.OH1 = A.alloc([NTT, 32], BF16, "OH1")
        self.OH2 = A.alloc([NTT, 32], BF16, "OH2")
        self.GT = A.alloc([NTT, 2], F32, "GT")
        self.xtmark = A.mark()
        self.XT = A.alloc([8, 2048], BF16, "XT")
        self.ytmark = A.mark()
        self.YT = A.alloc([8, 2048], BF16, "YT")
        self.dma(self.CO, self.co_d)
        self.copy(self.identb, self.co("ident"))
        self.copy(self.triUb, self.co("triU"))
        self.memset(cst[:, 0:1], RMS_EPS)
        self.memset(cst[:, 1:2], 1.0)
        self.memset(cst[:, 2:3], LN_EPS)
        self.epsr, self.oner, self.lnepsr = cst[:, 0:1], cst[:, 1:2], cst[:, 2:3]
        self.Wb = [A.alloc([8, 784], BF16, "Wb0"), A.alloc([8, 1040], BF16, "Wb1")]
        self.wi = 0
        base = A.mark()
        self.xbase = base
        for l in range(self.n_layers):
            self.dma(self.PP, self.pp_d[l * 128:(l + 1) * 128, 0:NPPS])
            src = self.x_d if l == 0 else self.y_d
            for s in range(self.nseq):
                A.release(base)
                self.make_XT(src, s)
                self.run_gens([(self.conv_mixer(l), 1, [0, 1, 2, 3]), (self.gla_mixer(l), 3, [4, 5, 6, 7])])
                self.S.barrier()
                A.release(base)
                gens = [(self.ssd_mixer(l), 1, [0, 1, 2, 3]), (self.diff_mixer(l), 2, [4, 5, 6, 7])]
                self.run_gens(gens)
                self.S.barrier()
                A.release(base)
                self.out_proj_ln1_route(s, l)
            A.release(self.xtmark)
            self.moe_sparse(l)
            A.release(base)


_PROG_CACHE = {}


def get_prog(nseq, n_layers, with_moe, stages="cgsd", dbg=False):
    key = (nseq, n_layers, with_moe, stages, dbg)
    if key not in _PROG_CACHE:
        _PROG_CACHE[key] = Prog(*key)
    return _PROG_CACHE[key]


def kernel(**inp):
    inp = {k: np.asarray(v) for k, v in inp.items()}
    n_cores = 8
    nseq = 2
    prog = get_prog(nseq, 2, True)
    x = np.ascontiguousarray(inp["x"], dtype=np.float32).reshape(16 * SEQ, D)
    pp = np.concatenate([host_pp(inp, l) for l in range(2)], axis=0)
    co = host_consts()
    rt = np.ascontiguousarray(np.concatenate([inp["router_g"], inp["router_e"].reshape(2, D, 32)], axis=2), dtype=np.float32)
    shared = {"w_in": inp["w_in"], "w_o": inp["w_o"], "pp": pp, "co": co, "rt": rt,
              "w_gate": inp["w_gate"], "w_up": inp["w_up"], "w_down": inp["w_down"]}
    in_maps = []
    for c in range(n_cores):
        mcore = dict(shared)
        mcore["x"] = x[c * nseq * SEQ:(c + 1) * nseq * SEQ]
        in_maps.append(mcore)
    res = run_bass_kernel_spmd(prog.nc, in_maps, core_ids=list(range(n_cores)))
    y = np.concatenate([res.results[c]["y"] for c in range(n_cores)], axis=0)
    return y.reshape(16, SEQ, D).astype(np.float32)
```

```python
import numpy as np
import ml_dtypes
import concourse.bass as bass
import concourse.mybir as mybir
from concourse.bass_utils import run_bass_kernel_spmd

F32 = mybir.dt.float32
BF16 = mybir.dt.bfloat16
I32 = mybir.dt.int32
U8 = mybir.dt.uint8
ALU = mybir.AluOpType
AF = mybir.ActivationFunctionType
AX = mybir.AxisListType
DTSIZE = {F32: 4, BF16: 2, I32: 4, U8: 1}


class Buf:
    __slots__ = ("name", "wr", "rd", "psum", "dram")

    def __init__(self, name="", dram=False):
        self.name = name
        self.wr = []
        self.rd = []
        self.psum = False
        self.dram = dram


class T:
    __slots__ = ("ap", "buf")

    def __init__(self, ap, buf):
        self.ap = ap
        self.buf = buf

    def __getitem__(self, k):
        return T(self.ap[k], self.buf)


class Op:
    __slots__ = ("eng", "fn", "deps", "sig", "isdma", "sem", "val", "prev_val", "cost", "odeps", "idx", "lat")

    def __init__(self, eng, fn, isdma):
        self.eng = eng
        self.fn = fn
        self.isdma = isdma
        self.cost = 300.0
        self.lat = 0.0
        self.odeps = []
        self.idx = 0
        self.deps = []
        self.sig = False
        self.sem = None
        self.val = 0
        self.prev_val = 0


ENGS = ("pe", "act", "dve", "pool", "sp")
N_DMA_SEMS = 32
EPOCH = 30000


class Sched:
    def __init__(self):
        self.streams = {e: [] for e in ENGS}
        self.all_ops = []
        self.dma_count = 0
        self.pending_dma = []

    @staticmethod
    def _acc(x):
        if not isinstance(x, T):
            return x, None
        if x.buf.psum:
            return x.buf, None
        try:
            ap = x.ap
            pairs = [(int(p[0]), int(p[1])) for p in ap.ap]
            off = int(ap.offset)
            esz = DTSIZE.get(ap.dtype, 4)
            if x.buf.dram:
                dims = pairs
                base = off
            else:
                pstep = pairs[0][0]
                dims = pairs[1:]
                base = off % pstep if pstep > 0 else off
            dims = [(s_, c_) for (s_, c_) in dims if c_ > 1 and s_ != 0]
            if not dims:
                return x.buf, [(base * esz, (base + 1) * esz)]
            dims.sort(key=lambda d: -abs(d[0]))
            run = 1
            if dims[-1][0] == 1:
                run = dims[-1][1]
                dims = dims[:-1]
                while dims and dims[-1][0] == run:
                    run *= dims[-1][1]
                    dims = dims[:-1]
            n = 1
            for _, c_ in dims:
                n *= c_
            if n > 96:
                hi = base + sum(abs(s_) * (c_ - 1) for s_, c_ in dims) + run
                return x.buf, [(base * esz, hi * esz)]
            starts = [base]
            for s_, c_ in dims:
                starts = [st + s_ * k for st in starts for k in range(c_)]
            ivs = sorted((st * esz, (st + run) * esz) for st in starts)
            return x.buf, ivs
        except Exception:
            return x.buf, None

    @staticmethod
    def _ovl(a, b):
        if a is None or b is None:
            return True
        i = j = 0
        while i < len(a) and j < len(b):
            if a[i][1] <= b[j][0]:
                i += 1
            elif b[j][1] <= a[i][0]:
                j += 1
            else:
                return True
        return False

    @staticmethod
    def _covers(a, b):
        if a is None:
            return True
        if b is None:
            return False
        i = 0
        for lo, hi in b:
            while i < len(a) and a[i][1] <= lo:
                i += 1
            if i >= len(a) or a[i][0] > lo or a[i][1] < hi:
                return False
        return True

    def op(self, eng, fn, reads=(), writes=(), dma=False, cost=300.0, lat=0.0):
        o = Op(eng, fn, dma)
        o.cost = cost
        o.lat = lat
        o.idx = len(self.all_ops)
        deps = set()
        racc = [self._acc(x) for x in reads]
        wacc = [self._acc(x) for x in writes]
        for b, iv in racc:
            for w, wiv in b.wr:
                if self._ovl(iv, wiv):
                    deps.add(w)
            if b.psum:
                for r, riv in b.rd:
                    if r.eng != eng:
                        deps.add(r)
        for b, iv in wacc:
            for r, riv in b.rd:
                if r is not o and self._ovl(iv, riv):
                    deps.add(r)
            for w, wiv in b.wr:
                if not self._ovl(iv, wiv):
                    continue
                if w.isdma and dma:
                    continue
                if w.isdma or dma or w.eng != eng:
                    deps.add(w)
                else:
                    o.odeps.append(w)
        for b, iv in wacc:
            b.rd = [(r, riv) for (r, riv) in b.rd if not self._covers(iv, riv)]
            b.wr = [(w, wiv) for (w, wiv) in b.wr if (w.isdma and dma) or not self._covers(iv, wiv)]
            if len(b.wr) > 200:
                for w, _ in b.wr:
                    if not (w.isdma and dma):
                        deps.add(w)
                b.wr = [(w, wiv) for (w, wiv) in b.wr if (w.isdma and dma)][-200:]
                iv = None
            b.wr.append((o, iv))
        for b, iv in racc:
            if len(b.rd) > 200:
                for r, _ in b.rd:
                    deps.add(r)
                b.rd = []
                iv = None
            b.rd.append((o, iv))
        for d in deps:
            if d is o:
                continue
            if d.eng == "pe" and eng == "pe" and not d.isdma and not dma:
                o.odeps.append(d)
                continue
            d.sig = True
            o.deps.append(d)
        self.streams[eng].append(o)
        self.all_ops.append(o)
        if dma:
            self.pending_dma.append(o)
        return o

    def barrier(self):
        lasts = []
        for e in ENGS:
            for o in reversed(self.streams[e]):
                if not o.isdma and o.fn is not None:
                    lasts.append(o)
                    break
        pend = list(self.pending_dma)
        self.pending_dma = []
        for e in ENGS:
            o = Op(e, None, False)
            o.idx = len(self.all_ops)
            for d in lasts + pend:
                d.sig = True
                o.deps.append(d)
            self.streams[e].append(o)
            self.all_ops.append(o)

    def reschedule(self):
        import heapq
        new_streams = {e: [] for e in ENGS}
        pos = {e: 0 for e in ENGS}
        done_ids = set()
        while True:
            seg = {}
            more = False
            for e in ENGS:
                st = self.streams[e]
                i = pos[e]
                j = i
                while j < len(st) and st[j].fn is not None:
                    j += 1
                seg[e] = st[i:j]
                if j < len(st):
                    more = True
            ops = [o for e in ENGS for o in seg[e]]
            inseg = set(id(o) for o in ops)
            succ = {id(o): [] for o in ops}
            nun = {}
            for o in ops:
                n = 0
                for d in list(o.deps) + list(o.odeps):
                    if id(d) in inseg:
                        succ[id(d)].append(o)
                        n += 1
                nun[id(o)] = n
            cp = {}
            for o in sorted(ops, key=lambda q: -q.idx):
                m_ = 0.0
                for s_ in succ[id(o)]:
                    v = cp[id(s_)]
                    if v > m_:
                        m_ = v
                cp[id(o)] = o.cost + o.lat + m_
            ready = {e: [] for e in ENGS}
            for o in ops:
                if nun[id(o)] == 0:
                    heapq.heappush(ready[o.eng], (-cp[id(o)] if CP_PRIORITY else o.idx, o.idx, o))
            free = {e: 0.0 for e in ENGS}
            comp = []
            now = 0.0
            nsched = 0
            fabric = 0.0
            while True:
                for e in ENGS:
                    if ready[e] and free[e] <= now:
                        _, _, o = heapq.heappop(ready[e])
                        new_streams[e].append(o)
                        nsched += 1
                        free[e] = now + o.cost
                        if o.isdma:
                            t0_ = max(now + o.cost, fabric)
                            fabric = t0_ + o.lat
                            heapq.heappush(comp, (fabric + 2000.0, o.idx, o))
                        else:
                            heapq.heappush(comp, (now + o.cost + o.lat, o.idx, o))
                nxt = [free[e] for e in ENGS if ready[e] and free[e] > now]
                if not comp and not nxt:
                    break
                tnext = min([comp[0][0]] if comp else []) if comp else None
                cand = nxt + ([comp[0][0]] if comp else [])
                now = min(cand)
                while comp and comp[0][0] <= now:
                    _, _, o = heapq.heappop(comp)
                    for s_ in succ[id(o)]:
                        nun[id(s_)] -= 1
                        if nun[id(s_)] == 0:
                            heapq.heappush(ready[s_.eng], (-cp[id(s_)] if CP_PRIORITY else s_.idx, s_.idx, s_))
            assert nsched == len(ops), (nsched, len(ops))
            for e in ENGS:
                pos[e] += len(seg[e])
                st = self.streams[e]
                if pos[e] < len(st):
                    new_streams[e].append(st[pos[e]])
                    pos[e] += 1
            if not more:
                break
        for e in ENGS:
            assert len(new_streams[e]) == len(self.streams[e]), (e, len(new_streams[e]), len(self.streams[e]))
        self.streams = new_streams

    def emit(self, nc):
        import contextlib
        stack = contextlib.ExitStack()
        with stack:
            counts = {e: 0 for e in ENGS}
            n_epochs = {e: 1 for e in ENGS}
            for o in self.all_ops:
                if o.isdma or not o.sig:
                    continue
                counts[o.eng] += 1
            for e in ENGS:
                n_epochs[e] = max(1, (counts[e] + EPOCH - 1) // EPOCH)
            esems = {e: [stack.enter_context(nc.semaphore(f"s_{e}{i}")) for i in range(n_epochs[e])] for e in ENGS}
            dsems = {e: [stack.enter_context(nc.semaphore(f"s_dma_{e}{i}")) for i in range(N_DMA_SEMS)] for e in ("sp", "pool", "act")}
            for e in ENGS:
                k = 0
                dcnt = 0
                for o in self.streams[e]:
                    if o.isdma:
                        o.sem = dsems[e][dcnt % N_DMA_SEMS]
                        o.prev_val = 16 * (dcnt // N_DMA_SEMS)
                        o.val = o.prev_val + 16
                        dcnt += 1
                    elif o.sig:
                        o.sem = esems[e][k // EPOCH]
                        o.val = (k % EPOCH) + 1
                        k += 1
            block = stack.enter_context(nc.Block())

            def run_stream(ename, eng):
                waited = {}
                pro = getattr(self, "prologue", {}).get(ename)
                if pro is not None:
                    pro(eng)
                for o in self.streams[ename]:
                    for d in o.deps:
                        key = id(d.sem)
                        if waited.get(key, 0) >= d.val:
                            continue
                        eng.wait_ge(d.sem, d.val)
                        waited[key] = d.val
                    if o.isdma and o.prev_val > 0:
                        key = id(o.sem)
                        if waited.get(key, 0) < o.prev_val:
                            eng.wait_ge(o.sem, o.prev_val)
                            waited[key] = o.prev_val
                    if o.fn is None:
                        continue
                    ins = o.fn(eng)
                    if o.isdma:
                        ins.then_inc(o.sem, 16)
                    elif o.sig:
                        ins.then_inc(o.sem, 1)

            @block.tensor
            def _(eng):
                run_stream("pe", eng)

            @block.scalar
            def _(eng):
                run_stream("act", eng)

            @block.vector
            def _(eng):
                run_stream("dve", eng)

            @block.gpsimd
            def _(eng):
                run_stream("pool", eng)

            @block.sync
            def _(eng):
                run_stream("sp", eng)


class Arena:
    def __init__(self, nc, nbytes):
        self.h = nc.alloc_sbuf_tensor("arena", [128, nbytes], U8)
        self.nbytes = nbytes
        self.off = 0

    def alloc(self, shape, dtype, name="", nparts=128):
        n = int(np.prod(shape)) * DTSIZE[dtype]
        off = (self.off + 31) // 32 * 32
        assert off + n <= self.nbytes, (name, off, n, self.nbytes)
        self.off = off + n
        ap = self.h[0:nparts, off:off + n].bitcast(dtype)
        if len(shape) == 2:
            ap = ap.rearrange("p (a b) -> p a b", b=shape[1])
        elif len(shape) == 3:
            ap = ap.rearrange("p (a b c) -> p a b c", b=shape[1], c=shape[2])
        return T(ap, Buf(name))

    def mark(self):
        return self.off

    def release(self, m):
        self.off = m


D = 1024
SEQ = 2048
NT = 16
NTG = 4
P_IN = 3348
ALPHA = 4 ** 0.25
LN_EPS = 1e-5
RMS_EPS = 1e-6
N_EXP = 32
RESCHEDULE = True
CP_PRIORITY = True

C_CONV = 0
C_GLA = 768
C_SSD = 1552
C_DIFF = 2580

PP_OFF = {}
_o = 0
for _n, _w in (("conv_w", 6), ("gla_b", 128), ("gla_g", 256), ("gla_wlr", 128), ("ssd_cw", 24), ("ssd_cb", 6),
               ("ssd_alog", 4), ("ssd_d", 256), ("ssd_dtb", 4), ("ssd_g", 256), ("diff_l", 128), ("diff_g", 256),
               ("ln1_g", 1024), ("ln1_b", 1024), ("ln2_g", 1024), ("ln2_b", 1024)):
    PP_OFF[_n] = (_o, _w)
    _o += _w
NPP = _o
NPPS = PP_OFF["ln1_g"][0]

CO_OFF = {}
_o = 0
for _n, _w in (("ident", 128), ("triU", 128), ("triUn16", 128), ("trisL", 128), ("ones", 128), ("hm", 4), ("triUs", 128), ("thr16", 16), ("bthr", 48), ("pc", 8)):
    CO_OFF[_n] = (_o, _w)
    _o += _w
NCO = _o


def host_consts():
    c = np.zeros((128, NCO), np.float32)
    p = np.arange(128)[:, None]
    j = np.arange(128)[None, :]
    c[:, CO_OFF["ident"][0]:CO_OFF["ident"][0] + 128] = (p == j)
    c[:, CO_OFF["triU"][0]:CO_OFF["triU"][0] + 128] = (p <= j)
    c[:, CO_OFF["triUn16"][0]:CO_OFF["triUn16"][0] + 128] = (p <= j) * np.float32(-1.0 / 16.0)
    c[:, CO_OFF["trisL"][0]:CO_OFF["trisL"][0] + 128] = (p > j)
    c[:, CO_OFF["ones"][0]:CO_OFF["ones"][0] + 128] = 1.0
    c[:, CO_OFF["hm"][0]:CO_OFF["hm"][0] + 4] = ((p // 32) == np.arange(4)[None, :])
    c[:, CO_OFF["triUs"][0]:CO_OFF["triUs"][0] + 128] = (p < j)
    c[:, CO_OFF["thr16"][0]:CO_OFF["thr16"][0] + 16] = 512.0 * np.arange(16)[None, :]
    c[:, CO_OFF["bthr"][0]:CO_OFF["bthr"][0] + 48] = 512.0 * np.arange(48)[None, :]
    c[:, CO_OFF["pc"][0]:CO_OFF["pc"][0] + 8] = 128.0 * np.arange(8)[None, :] + p
    return c


def host_pp(inp, l):
    pp = np.zeros((128, NPP), np.float32)

    def put(name, arr):
        o, w = PP_OFF[name]
        assert arr.shape == (128, w), (name, arr.shape)
        pp[:, o:o + w] = arr

    def row(v):
        return np.broadcast_to(np.asarray(v, np.float32)[None, :], (128, len(v)))

    cw = inp["conv_w"][l]
    put("conv_w", np.stack([cw[k, fc * 128:(fc + 1) * 128] for fc in range(2) for k in range(3)], axis=1))
    put("gla_b", row(inp["gla_b_lr"][l]))
    put("gla_g", row(np.tile(inp["gla_norm_g"][l], 4)))
    wl = np.zeros((128, 128), np.float32)
    wl[:16] = inp["gla_w_lr"][l]
    put("gla_wlr", wl)
    sw = inp["ssd_conv_w"][l]
    put("ssd_cw", np.stack([sw[k, c6 * 128:(c6 + 1) * 128] for c6 in range(6) for k in range(4)], axis=1))
    sb = inp["ssd_conv_b"][l]
    put("ssd_cb", np.stack([sb[c6 * 128:(c6 + 1) * 128] for c6 in range(6)], axis=1))
    put("ssd_alog", row(inp["ssd_a_log"][l]))
    put("ssd_d", row(np.repeat(inp["ssd_d"][l], 64)))
    put("ssd_dtb", row(inp["ssd_dt_bias"][l]))
    put("ssd_g", row(inp["ssd_norm_g"][l]))
    put("diff_l", row(np.concatenate([inp["diff_lq1"][l], inp["diff_lk1"][l], inp["diff_lq2"][l], inp["diff_lk2"][l]])))
    put("diff_g", row(np.tile(inp["diff_norm_g"][l], 4)))
    for n in ("ln1_g", "ln1_b", "ln2_g", "ln2_b"):
        put(n, row(inp[n][l]))
    return pp


class Prog:
    def __init__(self, nseq, n_layers, with_moe, stages="cgsd", dbg=False):
        self.nseq, self.n_layers, self.with_moe, self.stages, self.dbg = nseq, n_layers, with_moe, stages, dbg
        nc = self.nc = bass.Bass("TRN2", target_bir_lowering=False)
        ntok = nseq * SEQ
        dt = nc.dram_tensor
        self.x_d = dt("x", [ntok, D], F32, kind="ExternalInput").ap()
        self.win_d = dt("w_in", [2, D, P_IN], F32, kind="ExternalInput").ap()
        self.wo_d = dt("w_o", [2, D, D], F32, kind="ExternalInput").ap()
        self.pp_d = dt("pp", [2 * 128, NPP], F32, kind="ExternalInput").ap()
        self.co_d = dt("co", [128, NCO], F32, kind="ExternalInput").ap()
        if with_moe:
            self.rt_d = dt("rt", [2, D, 36], F32, kind="ExternalInput").ap()
            self.wg_d = dt("w_gate", [2, N_EXP, D, 512], F32, kind="ExternalInput").ap()
            self.wu_d = dt("w_up", [2, N_EXP, D, 512], F32, kind="ExternalInput").ap()
            self.wd_d = dt("w_down", [2, N_EXP, 512, D], F32, kind="ExternalInput").ap()
        self.y_d = dt("y", [ntok, D], F32, kind="ExternalOutput").ap()
        if dbg:
            self.dbg_d = dt("dbg", [ntok, D], F32, kind="ExternalOutput").ap()
        self.S = Sched()
        self.A = Arena(nc, 207 * 1024)
        self.banks = []
        for i in range(8):
            b = Buf(f"ps{i}")
            b.psum = True
            self.banks.append(T(nc.alloc_psum_tensor(f"ps{i}", [128, 512], F32)[:, :], b))
        self.pool = {"free": list(range(8)), "rr": 0}
        self.bounds = set()
        self.bregs = {}
        self.build()
        self.S.barrier()

        def pool_prologue(eng):
            for bv in sorted(self.bounds):
                r = nc.alloc_register(mybir.EngineType.Pool, f"bnd{bv}")
                eng.reg_mov(r, int(bv))
                self.bregs[bv] = r
        self.S.prologue = {"pool": pool_prologue}
        if RESCHEDULE:
            self.S.reschedule()
        self.S.emit(nc)

    def ps(self):
        pool = self.pool
        i = pool["free"][pool["rr"] % len(pool["free"])]
        pool["rr"] += 1
        return self.banks[i]

    def reserve(self):
        i = self.pool["free"].pop()
        return i, self.banks[i]

    def unreserve(self, i):
        self.pool["free"].append(i)

    def run_gens(self, specs):
        active = [[g, w, {"free": list(banks), "rr": 0}] for g, w, banks in specs]
        save = self.pool
        while active:
            for item in list(active):
                g, w, pool = item
                self.pool = pool
                for _ in range(w):
                    try:
                        next(g)
                    except StopIteration:
                        active.remove(item)
                        break
        self.pool = save

    @staticmethod
    def _a(x):
        return x.ap if isinstance(x, T) else x

    @staticmethod
    def _bufs(*xs):
        return [x for x in xs if isinstance(x, T)]

    @staticmethod
    def _fs(x):
        x = x.ap if isinstance(x, T) else x
        try:
            return float(x.free_size())
        except Exception:
            return 256.0

    def mm(self, out, lhsT, rhs, start=True, stop=True):
        a = self._a
        n = self._fs(rhs)
        c = max(64.0, n) / 2.4 + 40.0 + self._fs(lhsT) / 2.4 * 0.5
        if a(rhs).dtype == F32:
            c *= 4.0
        self.S.op("pe", lambda e: e.matmul(a(out), a(lhsT), a(rhs), start=start, stop=stop),
                  reads=self._bufs(lhsT, rhs), writes=self._bufs(out), cost=c, lat=0.0)

    def tr(self, out, in_):
        a = self._a
        self.S.op("pe", lambda e: e.transpose(a(out), a(in_), a(self.identb)),
                  reads=self._bufs(in_, self.identb), writes=self._bufs(out), cost=120.0, lat=0.0)

    def act(self, out, in_, func=None, bias=None, scale=None, accum=None, eng="act"):
        a = self._a
        kw = {}
        if bias is not None:
            kw["bias"] = a(bias)
        if scale is not None:
            kw["scale"] = a(scale)
        if accum is not None:
            kw["accum_out"] = a(accum)
        f = func if func is not None else AF.Copy
        self.S.op("act", lambda e: e.activation(a(out), a(in_), f, **kw),
                  reads=self._bufs(in_, bias, scale), writes=self._bufs(out, accum), cost=200.0 + 0.85 * self._fs(out), lat=0.0)

    def tt(self, out, in0, in1, op, eng="dve"):
        a = self._a
        self.S.op(eng, lambda e: e.tensor_tensor(a(out), a(in0), a(in1), op),
                  reads=self._bufs(in0, in1), writes=self._bufs(out), cost=self._vc(eng, out), lat=0.0)

    def ts(self, out, in0, s1, op0, s2=None, op1=None, eng="dve"):
        a = self._a
        if op1 is None:
            self.S.op(eng, lambda e: e.tensor_scalar(a(out), a(in0), a(s1), None, op0),
                      reads=self._bufs(in0, s1), writes=self._bufs(out), cost=self._vc(eng, out), lat=0.0)
        else:
            self.S.op(eng, lambda e: e.tensor_scalar(a(out), a(in0), a(s1), a(s2), op0, op1),
                      reads=self._bufs(in0, s1, s2), writes=self._bufs(out), cost=self._vc(eng, out), lat=0.0)

    def stt(self, out, in0, scalar, in1, op0, op1, accum=None):
        a = self._a
        if accum is None:
            self.S.op("dve", lambda e: e.scalar_tensor_tensor(a(out), a(in0), a(scalar), a(in1), op0, op1),
                      reads=self._bufs(in0, scalar, in1), writes=self._bufs(out), cost=self._vc("dve", out), lat=0.0)
        else:
            self.S.op("dve", lambda e: e.scalar_tensor_tensor(a(out), a(in0), a(scalar), a(in1), op0, op1, accum_out=a(accum)),
                      reads=self._bufs(in0, scalar, in1), writes=self._bufs(out, accum), cost=self._vc("dve", out) + 100.0, lat=0.0)

    def silu(self, out, x):
        self.act(out, x, AF.Exp, scale=-1.0)
        self.act(out, out, AF.Ln, bias=self.oner)
        self.act(out, out, AF.Exp, scale=-1.0)
        self.tt(out, out, x, ALU.mult)

    def _vc(self, eng, x):
        n = self._fs(x)
        return (400.0 + 6.0 * n) if eng == "pool" else (160.0 + 1.0 * n)

    def copy(self, out, in_, eng="dve"):
        a = self._a
        if eng == "act":
            self.S.op("act", lambda e: e.copy(a(out), a(in_)), reads=self._bufs(in_), writes=self._bufs(out),
                      cost=200.0 + 0.85 * self._fs(out), lat=0.0)
        else:
            self.S.op(eng, lambda e: e.tensor_copy(a(out), a(in_)), reads=self._bufs(in_), writes=self._bufs(out),
                      cost=self._vc(eng, out), lat=0.0)

    def memset(self, out, v, eng="pool"):
        a = self._a
        self.S.op(eng, lambda e: e.memset(a(out), v), writes=self._bufs(out), cost=self._vc(eng, out), lat=0.0)

    def reduce(self, out, in_, op=ALU.add):
        a = self._a
        self.S.op("dve", lambda e: e.tensor_reduce(a(out), a(in_), AX.X, op), reads=self._bufs(in_), writes=self._bufs(out),
                  cost=self._vc("dve", in_), lat=0.0)

    def recip(self, out, in_):
        a = self._a
        self.S.op("dve", lambda e: e.reciprocal(a(out), a(in_)), reads=self._bufs(in_), writes=self._bufs(out),
                  cost=200.0 + 7.0 * self._fs(out), lat=0.0)

    def dma(self, out, in_, q="sp"):
        a = self._a
        nb = self._fs(out) * 128.0 * 4.0
        self.S.op(q, lambda e: e.dma_start(out=a(out), in_=a(in_)), reads=self._bufs(in_), writes=self._bufs(out), dma=True,
                  cost=(1000.0 if q == "pool" else 150.0), lat=nb / 300.0)

    def co(self, name):
        o, w = CO_OFF[name]
        return self.CO[:, o:o + w]

    def ppv(self, name, a=0, b=None):
        o, w = PP_OFF[name]
        b = w if b is None else b
        return self.PP[:, o + a:o + b]

    def load_w(self, Wt, dram2d, c0, ncols, kchunks=8):
        src = dram2d[:, c0:c0 + ncols].rearrange("(c p) n -> p c n", p=128)
        self.dma(Wt[:, 0:kchunks, 0:ncols], src, q="pool")

    def proj_fm(self, ps, W, c0, ncol, tg):
        for c in range(8):
            self.mm(ps[0:ncol, :], W[:, c, c0:c0 + ncol], self.XT[:, c, tg * 512:(tg + 1) * 512], start=(c == 0), stop=(c == 7))

    def proj_tm(self, ps, W, c0, ncol, t):
        for c in range(8):
            self.mm(ps[:, 0:ncol], self.XT[:, c, t * 128:(t + 1) * 128], W[:, c, c0:c0 + ncol], start=(c == 0), stop=(c == 7))

    def make_XT(self, src, s_):
        A = self.A
        xf = [A.alloc([1024], F32, "xf0")] * 2
        xb = [A.alloc([1024], BF16, f"xb{i}") for i in range(2)]
        for t in range(NT):
            r0 = s_ * SEQ + t * 128
            self.dma(xf[t % 2], src[r0:r0 + 128, :])
            self.copy(xb[t % 2], xf[t % 2], eng="act")
            for hh in range(2):
                ps = self.ps()
                for k in range(4):
                    c = hh * 4 + k
                    self.mm(ps[:, k * 128:(k + 1) * 128], xb[t % 2][:, c * 128:(c + 1) * 128], self.identb)
                self.copy(T(self.XT.ap[:, hh * 4:hh * 4 + 4, t * 128:(t + 1) * 128], self.XT.buf),
                          T(ps.ap.rearrange("p (k j) -> p k j", j=128), ps.buf), eng=("act" if hh == 0 else "dve"))

    def norm_tr(self, o_sb, ng, grow, ytc0, t, tmp, extra=None, post=None):
        gs = 256 // ng
        sq, ss, ybf = tmp["sq"], tmp["ss"], tmp["ybf"]
        self.tt(sq, o_sb, o_sb, ALU.mult)
        self.reduce(ss[:, 0:ng], sq.ap.rearrange("p (g e) -> p g e", e=gs) if False else T(sq.ap.rearrange("p (g e) -> p g e", e=gs), sq.buf))
        self.act(ss[:, 0:ng], ss[:, 0:ng], AF.Ln, bias=self.epsr, scale=1.0 / gs)
        self.act(ss[:, 0:ng], ss[:, 0:ng], AF.Exp, scale=-0.5)
        o3 = T(o_sb.ap.rearrange("p (g e) -> p g e", e=gs), o_sb.buf)
        s3 = T(sq.ap.rearrange("p (g e) -> p g e", e=gs), sq.buf)
        rb = T(ss.ap[:, 0:ng].unsqueeze(2).to_broadcast([128, ng, gs]), ss.buf)
        self.tt(s3, o3, rb, ALU.mult)
        if extra is not None:
            self.tt(sq, sq, extra, ALU.mult)
        if post is not None:
            self.stt(ybf, sq, float(post), grow, ALU.mult, ALU.mult)
        else:
            self.tt(ybf, sq, grow, ALU.mult)
        ps = self.ps()
        for j in range(2):
            self.mm(ps[:, j * 128:(j + 1) * 128], ybf[:, j * 128:(j + 1) * 128], self.identb)
        self.copy(T(self.YT.ap[:, ytc0:ytc0 + 2, t * 128:(t + 1) * 128], self.YT.buf),
                  T(ps.ap[:, 0:256].rearrange("p (j k) -> p j k", k=128), ps.buf), eng="act")

    def conv_mixer(self, l):
        A = self.A
        W = self.Wb[1]
        self.load_w(W, self.win_d[l], C_CONV, 768)
        m = A.mark()
        cu = A.alloc([2050], F32, "cu")
        Bf = A.alloc([2048], BF16, "Bf")
        acc = A.alloc([2048], F32, "acc")
        tmp = [A.alloc([512], F32, "ctmp0")] * 2
        self.memset(cu[:, 0:2], 0.0)
        for fc in range(2):
            for tg in range(NTG):
                pu, pc, pb = self.ps(), self.ps(), self.ps()
                self.proj_fm(pu, W, fc * 128, 128, tg)
                self.proj_fm(pc, W, 512 + fc * 128, 128, tg)
                self.proj_fm(pb, W, 256 + fc * 128, 128, tg)
                tm = tmp[tg % 2]
                self.copy(tm, pu, eng="act")
                self.tt(cu[:, 2 + tg * 512:2 + (tg + 1) * 512], tm, pc, ALU.mult)
                self.copy(Bf[:, tg * 512:(tg + 1) * 512], pb, eng="act")
                yield
            w = [self.ppv("conv_w", fc * 3 + k, fc * 3 + k + 1) for k in range(3)]
            self.act(acc, cu[:, 0:2048], AF.Copy, scale=w[0])
            self.stt(acc, cu[:, 1:2049], w[1], acc, ALU.mult, ALU.add)
            self.stt(acc, cu[:, 2:2050], w[2], acc, ALU.mult, ALU.add)
            self.tt(self.YT[:, fc, :], acc, Bf, ALU.mult)
            yield

    def gla_mixer(self, l):
        A = self.A
        W = self.Wb[0]
        self.load_w(W, self.win_d[l], C_GLA, 784)
        m = A.mark()
        qf = A.alloc([2048], F32, "qf")
        kf = A.alloc([2048], F32, "kf")
        lrT = A.alloc([2048], F32, "lrT")
        cumT = A.alloc([2048], F32, "cumT")
        big = lrT
        qdm = [A.alloc([2048], BF16, f"qdm{h}") for h in range(4)]
        ki = A.alloc([2048], BF16, "ki")
        keT = A.alloc([2048], BF16, "keT")
        ke_tok = A.alloc([16, 128], BF16, "ke_tok")
        dec = A.alloc([16], F32, "dec")
        zb = [A.alloc([128], F32, "zb0")] * 2
        Sf = A.alloc([256], F32, "Sf")
        Sb = A.alloc([256], BF16, "Sb")
        vtok = [A.alloc([256], BF16, f"vtok{i}") for i in range(2)]
        sg = [A.alloc([256], F32, "sg0")] * 2
        attm = [A.alloc([4, 128], BF16, f"attm{i}") for i in range(2)]
        osb = [A.alloc([256], F32, "osb0")] * 2
        tmp = [dict(sq=A.alloc([256], F32, "sq0"), ss=A.alloc([4], F32, "ss0"), ybf=A.alloc([256], BF16, "ybf0"))] * 2
        for tg in range(NTG):
            p1, p2, p3 = self.ps(), self.ps(), self.ps()
            self.proj_fm(p1, W, 0, 128, tg)
            self.proj_fm(p2, W, 128, 128, tg)
            self.proj_fm(p3, W, 768, 16, tg)
            self.copy(qf[:, tg * 512:(tg + 1) * 512], p1, eng="act")
            self.copy(kf[:, tg * 512:(tg + 1) * 512], p2, eng="dve")
            self.copy(lrT[0:16, tg * 512:(tg + 1) * 512], p3[0:16, :], eng="act")
            yield
        wlr = self.ppv("gla_wlr")
        blr = self.ppv("gla_b")
        for tg in range(NTG):
            pci, pc = self.reserve()
            for k in range(4):
                t = tg * 4 + k
                pz = self.ps()
                self.mm(pz[:, 0:128], lrT[0:16, t * 128:(t + 1) * 128], wlr[0:16, :])
                z = zb[t % 2]
                self.tt(z, pz[:, 0:128], blr, ALU.add)
                self.act(z, z, AF.Exp, scale=-1.0)
                self.act(z, z, AF.Ln, bias=self.oner)
                self.mm(pc[:, k * 128:(k + 1) * 128], z, self.co("triUn16"))
            self.copy(cumT[:, tg * 512:(tg + 1) * 512], pc, eng="act")
            self.unreserve(pci)
            yield
        cl = T(cumT.ap.rearrange("p (t k) -> p t k", k=128)[:, :, 127], cumT.buf)
        self.act(dec, cl, AF.Exp)
        self.act(big, cumT, AF.Exp)
        self.tt(qf, qf, big, ALU.mult)
        for h in range(4):
            self.ts(qdm[h], qf, self.co("hm")[:, h:h + 1], ALU.mult, float(32 ** -0.5), ALU.mult)
        self.act(big, cumT, AF.Exp, scale=-1.0)
        self.tt(ki, kf, big, ALU.mult)
        for t in range(NT):
            sl = slice(t * 128, (t + 1) * 128)
            z = zb[t % 2]
            self.act(z, cumT[:, sl], AF.Exp, scale=-1.0, bias=cl[:, t:t + 1])
            self.tt(keT[:, sl], kf[:, sl], z, ALU.mult)
            pt = self.ps()
            self.mm(pt[:, 0:128], keT[:, sl], self.identb)
            self.copy(ke_tok[:, t, :], pt[:, 0:128], eng="act")
            if t % 2 == 1:
                yield
        self.memset(Sf, 0.0)
        self.memset(Sb, 0.0)
        triU3 = T(self.co("triU").ap.unsqueeze(1).to_broadcast([128, 4, 128]), self.CO.buf)
        for t in range(NT):
            sl = slice(t * 128, (t + 1) * 128)
            v, g_, am, ob = vtok[t % 2], sg[t % 2], attm[t % 2], osb[t % 2]
            pv = self.ps()
            self.proj_tm(pv, W, 256, 256, t)
            self.copy(v, pv[:, 0:256], eng="act")
            pg = self.ps()
            self.proj_tm(pg, W, 512, 256, t)
            self.silu(g_, pg[:, 0:256])
            yield
            pa = self.ps()
            for h in range(4):
                self.mm(pa[:, h * 128:(h + 1) * 128], ki[:, sl], qdm[h][:, sl])
            self.tt(am, T(pa.ap.rearrange("p (h i) -> p h i", i=128), pa.buf), triU3, ALU.mult)
            yield
            po = self.ps()
            for h in range(4):
                hs = slice(h * 64, (h + 1) * 64)
                self.mm(po[:, hs], qdm[h][:, sl], Sb[:, hs], start=True, stop=False)
                self.mm(po[:, hs], am[:, h, :], v[:, hs], start=False, stop=True)
            yield
            pd = self.ps()
            self.mm(pd[:, 0:256], ke_tok[:, t, :], v)
            self.stt(Sf, Sf, dec[:, t:t + 1], pd[:, 0:256], ALU.mult, ALU.add)
            self.copy(Sb, Sf, eng="act")
            self.copy(ob, po[:, 0:256], eng="act")
            yield
            self.norm_tr(ob, 4, self.ppv("gla_g"), 2, t, tmp[t % 2], extra=g_)
            yield

    def ssd_mixer(self, l):
        A = self.A
        W = self.Wb[1]
        self.load_w(W, self.win_d[l], C_SSD, 1028)
        m = A.mark()
        pre = [A.alloc([2051], F32, "pre0")] * 2
        acc = A.alloc([2048], F32, "sacc")
        cv = [A.alloc([2048], BF16, f"cv{i}") for i in range(6)]
        dtt = A.alloc([16, 4], F32, "dtt")
        dtA = A.alloc([16, 4], F32, "dtA")
        arow = A.alloc([4], F32, "arow")
        STf = A.alloc([256], F32, "STf")
        STb = A.alloc([256], BF16, "STb")
        xs_tok = [A.alloc([256], F32, f"xs_tok{i}") for i in range(2)]
        B_tok = [A.alloc([256], BF16, f"B_tok{i}") for i in range(2)]
        xdt = [A.alloc([256], F32, f"xdt{i}") for i in range(2)]
        xdtb = [A.alloc([256], BF16, f"xdtb{i}") for i in range(2)]
        xdd = [A.alloc([256], BF16, f"xdd{i}") for i in range(2)]
        cbm = [A.alloc([2, 128], F32, f"cbm{i}") for i in range(2)]
        lhsD = [A.alloc([128], F32, f"lhsD{i}") for i in range(2)]
        eD = [A.alloc([128], F32, f"eD{i}") for i in range(2)]
        MT = [A.alloc([128], BF16, f"MT{i}") for i in range(2)]
        ecum = [A.alloc([4], F32, f"ecum{i}") for i in range(2)]
        decs = [A.alloc([4], F32, f"decs{i}") for i in range(2)]
        t1 = [A.alloc([256], F32, f"t1{i}") for i in range(2)]
        t2 = [A.alloc([256], F32, "t2_0")] * 2
        sz = [A.alloc([256], F32, "sz_0")] * 2
        tmp = [dict(sq=A.alloc([256], F32, f"ssq{i}"), ss=A.alloc([4], F32, f"sss{i}"), ybf=A.alloc([256], BF16, f"sybf{i}")) for i in range(2)]
        self.memset(pre[0][:, 0:3], 0.0)
        for c6 in range(6):
            pr = pre[c6 % 2]
            for tg in range(NTG):
                ps = self.ps()
                self.proj_fm(ps, W, 256 + c6 * 128, 128, tg)
                self.copy(pr[:, 3 + tg * 512:3 + (tg + 1) * 512], ps, eng=("act" if tg % 2 == 0 else "dve"))
            w = [self.ppv("ssd_cw", c6 * 4 + k, c6 * 4 + k + 1) for k in range(4)]
            self.act(acc, pr[:, 0:2048], AF.Copy, scale=w[0])
            for k in range(1, 4):
                self.stt(acc, pr[:, k:k + 2048], w[k], acc, ALU.mult, ALU.add)
            self.ts(acc, acc, self.ppv("ssd_cb", c6, c6 + 1), ALU.add)
            scr = pr[:, 3:2051]
            self.act(scr, acc, AF.Exp, scale=-1.0)
            self.act(scr, scr, AF.Ln, bias=self.oner)
            self.act(scr, scr, AF.Exp, scale=-1.0)
            self.tt(cv[c6], acc, scr, ALU.mult)
            yield
        self.act(arow, self.ppv("ssd_alog"), AF.Exp)
        self.ts(arow, arow, -1.0, ALU.mult)
        for t in range(NT):
            ps = self.ps()
            self.proj_tm(ps, W, 1024, 4, t)
            self.tt(dtt[:, t, :], ps[:, 0:4], self.ppv("ssd_dtb"), ALU.add)
            if t % 4 == 3:
                yield
        self.act(dtt, dtt, AF.Exp)
        self.act(dtt, dtt, AF.Ln, bias=self.oner)
        self.tt(dtA, dtt, T(arow.ap.unsqueeze(1).to_broadcast([128, 16, 4]), arow.buf), ALU.mult)
        self.memset(STf, 0.0)
        self.memset(STb, 0.0)
        triU = self.co("triU")
        triU2 = T(triU.ap.unsqueeze(1).to_broadcast([128, 2, 128]), self.CO.buf)
        for t in range(NT):
            sl = slice(t * 128, (t + 1) * 128)
            i2 = t % 2
            ptx = self.ps()
            for j in range(2):
                self.mm(ptx[:, j * 128:(j + 1) * 128], cv[j][:, sl], self.identb)
                self.mm(ptx[:, 256 + j * 128:256 + (j + 1) * 128], cv[2 + j][:, sl], self.identb)
            self.copy(xs_tok[i2], ptx[:, 0:256], eng="act")
            self.copy(B_tok[i2], ptx[:, 256:512], eng="dve")
            x3 = T(xs_tok[i2].ap.rearrange("p (h e) -> p h e", e=64), xs_tok[i2].buf)
            dtb = T(dtt.ap[:, t, :].unsqueeze(2).to_broadcast([128, 4, 64]), dtt.buf)
            self.tt(T(xdt[i2].ap.rearrange("p (h e) -> p h e", e=64), xdt[i2].buf), x3, dtb, ALU.mult)
            self.copy(xdtb[i2], xdt[i2], eng="act")
            yield
            pcb = self.ps()
            for g in range(2):
                self.mm(pcb[:, g * 128:(g + 1) * 128], cv[2 + g][:, sl], cv[4 + g][:, sl])
            self.tt(cbm[i2], T(pcb.ap[:, 0:256].rearrange("p (g i) -> p g i", i=128), pcb.buf), triU2, ALU.mult)
            pcm = self.ps()
            self.mm(pcm[:, 0:4], triU, dtA[:, t, :])
            self.mm(pcm[:, 4:8], self.co("ones"), dtA[:, t, :])
            self.act(ecum[i2], pcm[:, 0:4], AF.Exp)
            self.act(decs[i2], pcm[:, 4:8], AF.Exp)
            yield
            pyi, py = self.reserve()
            pyoi, pyo = self.reserve()
            for hd in range(4):
                g = hd // 2
                hs = slice(hd * 64, (hd + 1) * 64)
                k2 = hd % 2
                self.ts(lhsD[k2], self.co("trisL"), dtA[:, t, hd:hd + 1], ALU.mult)
                pD = self.ps()
                self.mm(pD[:, 0:128], lhsD[k2], triU)
                self.act(eD[k2], pD[:, 0:128], AF.Exp)
                self.tt(MT[k2], eD[k2], cbm[i2][:, g, :], ALU.mult)
                self.mm(py[:, hs], MT[k2], xdtb[i2][:, hs])
                self.mm(pyo[:, hs], cv[4 + g][:, sl], STb[:, hs])
                self.ts(xdd[i2][:, hs], xdt[i2][:, hs], eD[k2][:, 127:128], ALU.mult)
                yield
            pst = self.ps()
            for g in range(2):
                gs = slice(g * 128, (g + 1) * 128)
                self.mm(pst[:, gs], B_tok[i2][:, gs], xdd[i2][:, gs])
            S3 = T(STf.ap.rearrange("p (h e) -> p h e", e=64), STf.buf)
            self.tt(S3, S3, T(decs[i2].ap.unsqueeze(2).to_broadcast([128, 4, 64]), decs[i2].buf), ALU.mult)
            self.tt(STf, STf, pst[:, 0:256], ALU.add)
            self.copy(STb, STf, eng="act")
            yield
            a3 = T(t1[i2].ap.rearrange("p (h e) -> p h e", e=64), t1[i2].buf)
            self.tt(a3, T(pyo.ap[:, 0:256].rearrange("p (h e) -> p h e", e=64), pyo.buf),
                    T(ecum[i2].ap.unsqueeze(2).to_broadcast([128, 4, 64]), ecum[i2].buf), ALU.mult)
            self.tt(t1[i2], t1[i2], py[:, 0:256], ALU.add)
            self.unreserve(pyoi)
            self.unreserve(pyi)
            self.tt(t2[i2], xs_tok[i2], self.ppv("ssd_d"), ALU.mult, eng="pool")
            self.tt(t1[i2], t1[i2], t2[i2], ALU.add)
            yield
            pz = self.ps()
            self.proj_tm(pz, W, 0, 256, t)
            self.silu(sz[i2], pz[:, 0:256])
            self.tt(t1[i2], t1[i2], sz[i2], ALU.mult)
            yield
            self.norm_tr(t1[i2], 2, self.ppv("ssd_g"), 4, t, tmp[i2])
            yield

    def diff_mixer(self, l):
        A = self.A
        W = self.Wb[0]
        self.load_w(W, self.win_d[l], C_DIFF, 768)
        m = A.mark()
        lam_init = 0.8 - 0.6 * float(np.exp(-0.3 * l))
        kT = A.alloc([2, 2048], BF16, "kT")
        vtok = A.alloc([16, 4, 65], BF16, "dvtok")
        Otok = [A.alloc([4, 256], F32, f"Otok{i}") for i in range(2)]
        qm = [[A.alloc([512], BF16, f"qm{c}{i}") for i in range(2)] for c in range(2)]
        pts = [A.alloc([512], BF16, f"pt{i}") for i in range(3)]
        lt = A.alloc([64], F32, "lt")
        lam = A.alloc([4], F32, "lam")
        rec = [A.alloc([2, 4], F32, f"rec{i}") for i in range(2)]
        o1 = [A.alloc([4, 64], F32, f"o1{i}") for i in range(2)]
        o2 = [A.alloc([4, 64], F32, f"o2{i}") for i in range(2)]
        tmp = [dict(sq=A.alloc([256], F32, f"dsq{i}"), ss=A.alloc([4], F32, f"dss{i}"), ybf=A.alloc([256], BF16, f"dybf{i}")) for i in range(2)]
        dl = self.ppv("diff_l")
        self.tt(lt[:, 0:32], dl[:, 0:32], dl[:, 32:64], ALU.mult)
        self.tt(lt[:, 32:64], dl[:, 64:96], dl[:, 96:128], ALU.mult)
        self.reduce(lam[:, 0:2], T(lt.ap.rearrange("p (a b) -> p a b", b=32), lt.buf))
        self.act(lam[:, 0:2], lam[:, 0:2], AF.Exp)
        self.tt(lam[:, 2:3], lam[:, 0:1], lam[:, 1:2], ALU.subtract)
        self.ts(lam[:, 3:4], lam[:, 2:3], float(lam_init), ALU.add, -1.0, ALU.mult)
        nlam = lam[:, 3:4]
        for kc in range(2):
            for tg in range(NTG):
                ps = self.ps()
                self.proj_fm(ps, W, 256 + kc * 128, 128, tg)
                self.copy(kT[:, kc, tg * 512:(tg + 1) * 512], ps, eng=("act" if tg % 2 else "dve"))
                yield
        self.memset(vtok, 1.0)
        for t in range(NT):
            ps = self.ps()
            self.proj_tm(ps, W, 512, 256, t)
            self.copy(T(vtok.ap[:, t, :, 0:64], vtok.buf), T(ps.ap[:, 0:256].rearrange("p (h e) -> p h e", e=64), ps.buf), eng="act")
            if t % 4 == 3:
                yield
        scale = float(32 ** -0.5)
        ptc = 0
        for qg in range(NTG):
            for h in range(4):
                qc = h // 2
                k2 = h % 2
                pq = self.ps()
                self.proj_fm(pq, W, qc * 128, 128, qg)
                for c in range(2):
                    b = (h % 2) * 2 + c
                    self.ts(qm[c][k2], pq, self.co("hm")[:, b:b + 1], ALU.mult, scale, ALU.mult)
                ib = [self.reserve(), self.reserve()]
                steps = [(c, jt) for c in range(2) for jt in range(4 * qg + 4)]

                def qk(c, jt):
                    n0 = max(0, jt - 4 * qg) * 128
                    ps = self.ps()
                    self.mm(ps[:, n0:512], kT[:, qc, jt * 128:(jt + 1) * 128], qm[c][k2][:, n0:512])
                    return ps
                cur = qk(*steps[0])
                for si, (c, jt) in enumerate(steps):
                    nxt = qk(*steps[si + 1]) if si + 1 < len(steps) else None
                    po = ib[c][1]
                    i0 = max(0, jt - 4 * qg)
                    n0 = i0 * 128
                    pt = pts[ptc % 3]
                    ptc += 1
                    self.act(pt[:, n0:512], cur[:, n0:512], AF.Exp)
                    if jt >= 4 * qg:
                        self.tt(pt[:, n0:n0 + 128], pt[:, n0:n0 + 128], self.triUb, ALU.mult, eng="dve")
                    for it in range(i0, 4):
                        self.mm(po[:, it * 65:(it + 1) * 65], pt[:, it * 128:(it + 1) * 128], vtok[:, jt, h, :],
                                start=(jt == 0 and it == 0), stop=(jt == 4 * qg + it))
                    cur = nxt
                    yield
                r = rec[k2]
                for c in range(2):
                    po = ib[c][1]
                    p3 = T(po.ap[:, 0:260].rearrange("p (i e) -> p i e", e=65), po.buf)
                    self.recip(r[:, c, :], p3[:, :, 64])
                    dst = o1[k2] if c == 0 else o2[k2]
                    self.tt(dst, p3[:, :, 0:64], T(r.ap[:, c, :].unsqueeze(2).to_broadcast([128, 4, 64]), r.buf), ALU.mult)
                self.unreserve(ib[1][0])
                self.unreserve(ib[0][0])
                self.stt(T(Otok[qg % 2].ap[:, :, h * 64:(h + 1) * 64], Otok[qg % 2].buf), o2[k2], nlam, o1[k2], ALU.mult, ALU.add)
                yield
            for k4 in range(4):
                t = 4 * qg + k4
                self.norm_tr(Otok[qg % 2][:, k4, :], 4, self.ppv("diff_g"), 6, t, tmp[t % 2], post=(1.0 - lam_init))
            yield

    def layer_norm(self, xt, grow, brow, st, gb_eng="dve", presum=False):
        junk = self.junk
        self.act(junk, xt, AF.Copy, accum=st[:, 0:1])
        self.act(junk, xt, AF.Square, accum=st[:, 1:2])
        self.ts(st[:, 2:3], st[:, 0:1], 1.0 / D, ALU.mult)
        self.tt(st[:, 3:4], st[:, 2:3], st[:, 2:3], ALU.mult)
        self.stt(st[:, 4:5], st[:, 1:2], 1.0 / D, st[:, 3:4], ALU.mult, ALU.subtract)
        self.act(st[:, 5:6], st[:, 4:5], AF.Ln, bias=self.lnepsr)
        self.act(st[:, 6:7], st[:, 5:6], AF.Exp, scale=-0.5)
        self.stt(st[:, 7:8], st[:, 2:3], -1.0, st[:, 6:7], ALU.mult, ALU.mult)
        self.act(xt, xt, AF.Identity, bias=st[:, 7:8], scale=st[:, 6:7])
        self.tt(xt, xt, grow, ALU.mult, eng=gb_eng)
        self.tt(xt, xt, brow, ALU.add, eng=gb_eng)

    def out_proj_ln1_route(self, s, l):
        A = self.A
        Wo = self.Wb[1]
        self.load_w(Wo, self.wo_d[l], 0, 1024)
        m = A.mark()
        RT = A.alloc([8, 36], BF16, "RT")
        self.dma(RT, self.rt_d[l].rearrange("(c p) n -> p c n", p=128), q="pool")
        xo = [A.alloc([1024], F32, f"xo{i}") for i in range(2)]
        xt_ = [A.alloc([1024], F32, f"xt{i}") for i in range(2)]
        xT = [A.alloc([8, 128], BF16, f"x1T{i}") for i in range(2)]
        xtb = [A.alloc([1024], BF16, f"xtb{i}") for i in range(2)]
        st = [A.alloc([16], F32, f"lnst{i}") for i in range(2)]
        LA = A.alloc([NT, 36], F32, "LA")
        gmx = A.alloc([NT], F32, "gmx")
        ohg = A.alloc([NT, 4], F32, "ohg")
        eg = A.alloc([NT, 4], F32, "eg")
        pg_ = A.alloc([NT], F32, "pg_")
        sel = A.alloc([NT, 4, 8], F32, "sel")
        el = A.alloc([NT, 8], F32, "el")
        el2 = A.alloc([NT, 8], F32, "el2")
        k1 = A.alloc([NT, 8], F32, "k1")
        k2 = A.alloc([NT, 8], F32, "k2")
        m1 = A.alloc([NT], F32, "m1")
        m2 = A.alloc([NT], F32, "m2")
        self.junk = A.alloc([1024], F32, "junk")
        LNR = A.alloc([2048], F32, "LNR")
        o_ = PP_OFF["ln1_g"][0]
        self.dma(LNR, self.pp_d[l * 128:(l + 1) * 128, o_:o_ + 2048])
        src = self.x_d if l == 0 else self.y_d
        ident = self.co("ident")
        for t in range(NT):
            gt = s * NT + t
            r0 = s * SEQ + t * 128
            i2 = t % 2
            self.dma(xo[i2], src[r0:r0 + 128, :])
            self.memset(st[i2][:, 8:10], 0.0, eng="dve")
            xt = xt_[i2]
            for half in range(2):
                ps = self.ps()
                for c in range(8):
                    self.mm(ps, self.YT[:, c, t * 128:(t + 1) * 128], Wo[:, c, half * 512:(half + 1) * 512], start=(c == 0), stop=(c == 7))
                self.stt(xt[:, half * 512:(half + 1) * 512], xo[i2][:, half * 512:(half + 1) * 512], float(ALPHA), ps, ALU.mult, ALU.add,
                         accum=st[i2][:, 8 + half:9 + half])
            self.tt(st[i2][:, 0:1], st[i2][:, 8:9], st[i2][:, 9:10], ALU.add)
            self.layer_norm(xt, LNR[:, 0:1024], LNR[:, 1024:2048], st[i2], presum=True)
            self.dma(self.X1[r0:r0 + 128, :], xt)
            self.copy(xtb[i2], xt, eng="act")
            for hh in range(2):
                ps = self.ps()
                for k in range(4):
                    c = hh * 4 + k
                    self.mm(ps[:, k * 128:(k + 1) * 128], xtb[i2][:, c * 128:(c + 1) * 128], self.identb)
                self.copy(T(xT[i2].ap[:, hh * 4:hh * 4 + 4, :], xT[i2].buf), T(ps.ap.rearrange("p (k j) -> p k j", j=128), ps.buf), eng="act")
            ps = self.ps()
            for c in range(8):
                self.mm(ps[:, 0:36], xT[i2][:, c, :], RT[:, c, :], start=(c == 0), stop=(c == 7))
            self.copy(LA[:, t, :], ps[:, 0:36], eng="act")
        def bc(x, shape, axis):
            return T(x.ap.unsqueeze(axis).to_broadcast(shape), x.buf)
        g0 = s * NT
        Lg = LA[:, :, 0:4]
        Le = T(LA.ap[:, :, 4:36].rearrange("p t (g j) -> p t g j", j=8), LA.buf)
        self.reduce(gmx, Lg, op=ALU.max)
        self.tt(ohg, Lg, bc(gmx, [128, NT, 4], 2), ALU.is_equal)
        self.tt(eg, Lg, bc(gmx, [128, NT, 4], 2), ALU.subtract)
        self.act(eg, eg, AF.Exp)
        self.reduce(pg_, eg)
        self.recip(pg_, pg_)
        self.tt(sel, Le, bc(ohg, [128, NT, 4, 8], 3), ALU.mult)
        self.reduce(el, T(sel.ap.rearrange("p t g j -> p t j g"), sel.buf))
        self.reduce(m1, el, op=ALU.max)
        self.tt(k1, el, bc(m1, [128, NT, 8], 2), ALU.is_equal)
        self.stt(el2, k1, -1e30, el, ALU.mult, ALU.add)
        self.reduce(m2, el2, op=ALU.max)
        self.tt(k2, el2, bc(m2, [128, NT, 8], 2), ALU.is_equal)
        self.tt(m2, m2, m1, ALU.subtract)
        self.act(m2, m2, AF.Exp)
        self.ts(m2, m2, 1.0, ALU.add)
        self.recip(m2, m2)
        self.tt(self.GT[:, g0:g0 + NT, 0], m2, pg_, ALU.mult)
        self.tt(self.GT[:, g0:g0 + NT, 1], pg_, self.GT[:, g0:g0 + NT, 0], ALU.subtract)
        for (OH, kk) in ((self.OH1, k1), (self.OH2, k2)):
            dst = T(OH.ap[:, g0:g0 + NT, :].rearrange("p t (g j) -> p t g j", j=8), OH.buf)
            self.tt(dst, bc(kk, [128, NT, 4, 8], 2), bc(ohg, [128, NT, 4, 8], 3), ALU.mult)
        self.S.barrier()
        A.release(m)

    def moe_sparse(self, l):
        A = self.A
        m = A.mark()
        NTT = self.nseq * NT
        NB = self.NB
        ones = self.co("ones")
        RK = A.alloc([NTT, 32], F32, "RK")
        At = [A.alloc([32], F32, f"At{i}") for i in range(2)]
        Acum = A.alloc([32], F32, "Acum")
        cnt = A.alloc([32], F32, "cnt")
        self.memset(Acum, 0.0)
        for gt in range(NTT):
            a = At[gt % 2]
            self.tt(a, self.OH1[:, gt, :], self.OH2[:, gt, :], ALU.add)
            ps = self.ps()
            self.mm(ps[:, 0:32], ones, Acum, start=True, stop=False)
            self.mm(ps[:, 0:32], self.co("triUs"), a, start=False, stop=True)
            self.copy(RK[:, gt, :], ps[:, 0:32], eng="act")
            self.tt(Acum, Acum, a, ALU.add)
        ps = self.ps()
        self.mm(ps[:, 0:32], ones, Acum)
        self.copy(cnt, ps[:, 0:32], eng="act")
        cmp = A.alloc([32, 16], F32, "cmp")
        pad = A.alloc([32], F32, "pad")
        sc = [A.alloc([32], F32, f"sc{i}") for i in range(2)]
        pstart = A.alloc([32], F32, "pstart")
        thr = self.co("thr16")
        self.tt(cmp, T(cnt.ap.unsqueeze(2).to_broadcast([128, 32, 16]), cnt.buf),
                T(thr.ap.unsqueeze(1).to_broadcast([128, 32, 16]), self.CO.buf), ALU.is_gt)
        self.reduce(pad, cmp)
        self.ts(pad, pad, 512.0, ALU.mult)
        cur = pad
        k = 0
        for sh in (1, 2, 4, 8, 16):
            nx = sc[k % 2]
            k += 1
            self.copy(nx[:, 0:sh], cur[:, 0:sh])
            self.tt(nx[:, sh:32], cur[:, sh:32], cur[:, 0:32 - sh], ALU.add)
            cur = nx
        pend = cur
        self.tt(pstart, pend, pad, ALU.subtract)
        big = A.alloc([NTT, 32], F32, "big")
        dstf = A.alloc([NTT, 2], F32, "dstf")
        DST = A.alloc([NTT, 2], I32, "DST")
        self.tt(RK, RK, T(pstart.ap.unsqueeze(1).to_broadcast([128, NTT, 32]), pstart.buf), ALU.add)
        self.tt(big, RK, self.OH1, ALU.mult)
        self.reduce(dstf[:, :, 0], big)
        self.tt(big, RK, self.OH2, ALU.mult)
        self.reduce(dstf[:, :, 1], big)
        self.copy(DST, dstf)
        cb = A.alloc([NB, 32], F32, "cb")
        be = A.alloc([NB], F32, "be")
        inv = A.alloc([NB], F32, "inv")
        ixf = A.alloc([NB, 8], F32, "ixf")
        IXW = A.alloc([NB, 8], I32, "IXW")
        IXD = A.alloc([NB, 4], I32, "IXD")
        bthr = self.co("bthr")[:, 0:NB]
        self.tt(cb, T(pend.ap.unsqueeze(1).to_broadcast([128, NB, 32]), pend.buf),
                T(bthr.ap.unsqueeze(2).to_broadcast([128, NB, 32]), self.CO.buf), ALU.is_le)
        self.reduce(be, cb)
        self.ts(inv, bthr, pend[:, 31:32], ALU.is_ge, 4.0e6, ALU.mult)
        pc = self.co("pc")
        self.stt(be, be, 1024.0, inv, ALU.mult, ALU.add)
        self.ts(be, be, float(l * N_EXP * 1024), ALU.add)
        self.tt(ixf, T(be.ap.unsqueeze(2).to_broadcast([128, NB, 8]), be.buf),
                T(pc.ap.unsqueeze(1).to_broadcast([128, NB, 8]), self.CO.buf), ALU.add)
        self.copy(IXW, ixf)
        self.stt(be, be, 0.5, inv, ALU.mult, ALU.add)
        self.tt(ixf[:, :, 0:4], T(be.ap.unsqueeze(2).to_broadcast([128, NB, 4]), be.buf),
                T(pc.ap[:, 0:4].unsqueeze(1).to_broadcast([128, NB, 4]), self.CO.buf), ALU.add)
        self.copy(IXD, ixf[:, :, 0:4])
        xl = [A.alloc([1024], F32, f"xl{i}") for i in range(3)]
        nslot = NB * 512
        for gt in range(NTT):
            x_ = xl[gt % 3]
            self.dma(x_, self.X1[gt * 128:(gt + 1) * 128, :])
            for k2 in range(2):
                self.idma(self.XS, DST[:, gt, k2:k2 + 1], x_, scatter=True, bound=nslot - 1)
        Wg = [[A.alloc([512], BF16, f"Wg{i}_{c}") for c in range(8)] for i in range(2)]
        Wu = [[A.alloc([512], BF16, f"Wu{i}_{c}") for c in range(8)] for i in range(2)]
        Wd = [[A.alloc([1024], BF16, f"Wd{i}_{c}") for c in range(4)] for i in range(2)]
        xr = [A.alloc([4, 1024], BF16, f"xr{i}") for i in range(2)]
        xsT = [A.alloc([8, 512], BF16, f"xsT{i}") for i in range(2)]
        hT = [A.alloc([4, 512], BF16, f"hT{i}") for i in range(2)]
        sil = [A.alloc([512], F32, f"sil{i}") for i in range(2)]
        ysb = [A.alloc([1024], F32, f"ysb{i}") for i in range(3)]
        wg2 = T(self.wg_d.rearrange("l e d f -> (l e d) f"), Buf("wg", dram=True))
        wu2 = T(self.wu_d.rearrange("l e d f -> (l e d) f"), Buf("wu", dram=True))
        wd2 = T(self.wd_d.rearrange("l e f d -> (l e f) d"), Buf("wd", dram=True))
        yc = 0
        for b in range(NB):
            i2 = b % 2
            for c in range(8):
                self.idma(Wg[i2][c], IXW[:, b, c:c + 1], wg2, scatter=False, bound=2 * N_EXP * 1024 - 1)
                self.idma(Wu[i2][c], IXW[:, b, c:c + 1], wu2, scatter=False, bound=2 * N_EXP * 1024 - 1)
            for c in range(4):
                self.idma(Wd[i2][c], IXD[:, b, c:c + 1], wd2, scatter=False, bound=2 * N_EXP * 512 - 1)
            self.dma(xr[i2], T(self.XS.ap[b * 512:(b + 1) * 512, :].rearrange("(s p) d -> p s d", p=128), self.XS.buf))
            for c2 in range(4):
                ps = self.ps()
                psb = T(ps.ap.bitcast(BF16), ps.buf)
                for j in range(2):
                    c = 2 * c2 + j
                    for s4 in range(4):
                        self.tr(psb[:, j * 512 + s4 * 128:j * 512 + (s4 + 1) * 128], xr[i2][:, s4, c * 128:(c + 1) * 128])
                self.copy(T(xsT[i2].ap[:, 2 * c2:2 * c2 + 2, :], xsT[i2].buf),
                          T(psb.ap.rearrange("p (j n) -> p j n", n=512), ps.buf), eng=("act" if c2 % 2 == 0 else "dve"))
            for fc in range(4):
                pg, pu = self.ps(), self.ps()
                for c in range(8):
                    self.mm(pg, Wg[i2][c][:, fc * 128:(fc + 1) * 128], xsT[i2][:, c, :], start=(c == 0), stop=(c == 7))
                for c in range(8):
                    self.mm(pu, Wu[i2][c][:, fc * 128:(fc + 1) * 128], xsT[i2][:, c, :], start=(c == 0), stop=(c == 7))
                sl_ = sil[fc % 2]
                self.act(sl_, pg, AF.Silu)
                self.tt(hT[i2][:, fc, :], sl_, pu, ALU.mult)
            for s4 in range(4):
                y_ = ysb[yc % 3]
                yc += 1
                for half in range(2):
                    ps = self.ps()
                    for fc in range(4):
                        self.mm(ps, hT[i2][:, fc, s4 * 128:(s4 + 1) * 128], Wd[i2][fc][:, half * 512:(half + 1) * 512], start=(fc == 0), stop=(fc == 3))
                    self.copy(y_[:, half * 512:(half + 1) * 512], ps, eng=("act" if half == 0 else "dve"))
                r0 = b * 512 + s4 * 128
                self.dma(self.YS[r0:r0 + 128, :], y_)
        ya = [A.alloc([1024], F32, f"ya{i}") for i in range(2)]
        yb = [A.alloc([1024], F32, f"yb{i}") for i in range(2)]
        st = [A.alloc([16], F32, f"lnst{i}") for i in range(2)]
        self.junk = A.alloc([1024], F32, "junk")
        LNR = A.alloc([2048], F32, "LNR")
        o_ = PP_OFF["ln2_g"][0]
        self.dma(LNR, self.pp_d[l * 128:(l + 1) * 128, o_:o_ + 2048])

        def fetch(gt):
            self.dma(xl[gt % 3], self.X1[gt * 128:(gt + 1) * 128, :])
            self.idma(ya[gt % 2], DST[:, gt, 0:1], self.YS, scatter=False, bound=nslot - 1)
            self.idma(yb[gt % 2], DST[:, gt, 1:2], self.YS, scatter=False, bound=nslot - 1)
        fetch(0)
        for gt in range(NTT):
            i2 = gt % 2
            x_ = xl[gt % 3]
            self.ts(x_, x_, float(ALPHA), ALU.mult)
            self.stt(x_, ya[i2], self.GT[:, gt, 0:1], x_, ALU.mult, ALU.add)
            self.memset(st[i2][:, 0:1], 0.0, eng="dve")
            self.stt(x_, yb[i2], self.GT[:, gt, 1:2], x_, ALU.mult, ALU.add, accum=st[i2][:, 0:1])
            if gt + 1 < NTT:
                fetch(gt + 1)
            self.layer_norm(x_, LNR[:, 0:1024], LNR[:, 1024:2048], st[i2], gb_eng="dve", presum=True)
            self.dma(self.y_d[gt * 128:(gt + 1) * 128, :], x_)
        self.S.barrier()
        A.release(m)

    def idma(self, dst, idx, src, scatter, bound):
        a = self._a
        self.bounds.add(bound)
        regs = self.bregs
        bound_key = bound
        bound = None
        if scatter:
            fn = lambda e: e.indirect_dma_start(out=a(dst), out_offset=bass.IndirectOffsetOnAxis(ap=a(idx), axis=0),
                                                in_=a(src), in_offset=None, bounds_check=regs[bound_key], oob_is_err=False)
        else:
            fn = lambda e: e.indirect_dma_start(out=a(dst), out_offset=None, in_=a(src),
                                                in_offset=bass.IndirectOffsetOnAxis(ap=a(idx), axis=0),
                                                bounds_check=regs[bound_key], oob_is_err=False)
        nb = min(self._fs(dst), self._fs(src)) * 128.0 * 4.0
        self.S.op("pool", fn, reads=self._bufs(src, idx), writes=self._bufs(dst), dma=True, cost=900.0, lat=nb / 300.0)

    def next_w(self):
        self.wi += 1
        return self.Wb[self.wi % 2]

    def build(self):
        A = self.A
        nc = self.nc
        ntok = self.nseq * SEQ
        NTT = self.nseq * NT
        self.NB = (2 * ntok + N_EXP * 511) // 512 + 1
        assert self.NB <= 48
        self.X1 = T(nc.dram_tensor("x1_scr", [ntok, D], F32).ap(), Buf("X1", dram=True))
        self.XS = T(nc.dram_tensor("xs_scr", [self.NB * 512, D], BF16).ap(), Buf("XS", dram=True))
        self.YS = T(nc.dram_tensor("ys_scr", [self.NB * 512, D], F32).ap(), Buf("YS", dram=True))
        self.CO = A.alloc([NCO], F32, "CO")
        self.PP = A.alloc([NPPS], F32, "PP")
        self.identb = A.alloc([128], BF16, "identb")
        self.triUb = A.alloc([128], BF16, "triUb")
        cst = A.alloc([4], F32, "cst")
        self.OH1 = A.alloc([NTT, 32], BF16, "OH1")
        self.OH2 = A.alloc([NTT, 32], BF16, "OH2")
        self.GT = A.alloc([NTT, 2], F32, "GT")
        self.xtmark = A.mark()
        self.XT = A.alloc([8, 2048], BF16, "XT")
        self.ytmark = A.mark()
        self.YT = A.alloc([8, 2048], BF16, "YT")
        self.dma(self.CO, self.co_d)
        self.copy(self.identb, self.co("ident"))
        self.copy(self.triUb, self.co("triU"))
        self.memset(cst[:, 0:1], RMS_EPS)
        self.memset(cst[:, 1:2], 1.0)
        self.memset(cst[:, 2:3], LN_EPS)
        self.epsr, self.oner, self.lnepsr = cst[:, 0:1], cst[:, 1:2], cst[:, 2:3]
        self.Wb = [A.alloc([8, 784], BF16, "Wb0"), A.alloc([8, 1040], BF16, "Wb1")]
        self.wi = 0
        base = A.mark()
        self.xbase = base
        for l in range(self.n_layers):
            self.dma(self.PP, self.pp_d[l * 128:(l + 1) * 128, 0:NPPS])
            src = self.x_d if l == 0 else self.y_d
            for s in range(self.nseq):
                A.release(base)
                self.make_XT(src, s)
                self.run_gens([(self.conv_mixer(l), 1, [0, 1, 2, 3]), (self.gla_mixer(l), 3, [4, 5, 6, 7])])
                self.S.barrier()
                A.release(base)
                gens = [(self.ssd_mixer(l), 1, [0, 1, 2, 3]), (self.diff_mixer(l), 2, [4, 5, 6, 7])]
                self.run_gens(gens)
                self.S.barrier()
                A.release(base)
                self.out_proj_ln1_route(s, l)
            A.release(self.xtmark)
            self.moe_sparse(l)
            A.release(base)


_PROG_CACHE = {}


def get_prog(nseq, n_layers, with_moe, stages="cgsd", dbg=False):
    key = (nseq, n_layers, with_moe, stages, dbg)
    if key not in _PROG_CACHE:
        _PROG_CACHE[key] = Prog(*key)
    return _PROG_CACHE[key]


def kernel(**inp):
    inp = {k: np.asarray(v) for k, v in inp.items()}
    n_cores = 8
    nseq = 2
    prog = get_prog(nseq, 2, True)
    x = np.ascontiguousarray(inp["x"], dtype=np.float32).reshape(16 * SEQ, D)
    pp = np.concatenate([host_pp(inp, l) for l in range(2)], axis=0)
    co = host_consts()
    rt = np.ascontiguousarray(np.concatenate([inp["router_g"], inp["router_e"].reshape(2, D, 32)], axis=2), dtype=np.float32)
    shared = {"w_in": inp["w_in"], "w_o": inp["w_o"], "pp": pp, "co": co, "rt": rt,
              "w_gate": inp["w_gate"], "w_up": inp["w_up"], "w_down": inp["w_down"]}
    in_maps = []
    for c in range(n_cores):
        mcore = dict(shared)
        mcore["x"] = x[c * nseq * SEQ:(c + 1) * nseq * SEQ]
        in_maps.append(mcore)
    res = run_bass_kernel_spmd(prog.nc, in_maps, core_ids=list(range(n_cores)))
    y = np.concatenate([res.results[c]["y"] for c in range(n_cores)], axis=0)
    return y.reshape(16, SEQ, D).astype(np.float32)
```

```python
import numpy as np
import ml_dtypes
import concourse.bass as bass
import concourse.mybir as mybir
from concourse.bass_utils import run_bass_kernel_spmd

F32 = mybir.dt.float32
BF16 = mybir.dt.bfloat16
I32 = mybir.dt.int32
U8 = mybir.dt.uint8
ALU = mybir.AluOpType
AF = mybir.ActivationFunctionType
AX = mybir.AxisListType
DTSIZE = {F32: 4, BF16: 2, I32: 4, U8: 1}


class Buf:
    __slots__ = ("name", "wr", "rd", "psum", "dram")

    def __init__(self, name="", dram=False):
        self.name = name
        self.wr = []
        self.rd = []
        self.psum = False
        self.dram = dram


class T:
    __slots__ = ("ap", "buf")

    def __init__(self, ap, buf):
        self.ap = ap
        self.buf = buf

    def __getitem__(self, k):
        return T(self.ap[k], self.buf)


class Op:
    __slots__ = ("eng", "fn", "deps", "sig", "isdma", "sem", "val", "prev_val", "cost", "odeps", "idx", "lat")

    def __init__(self, eng, fn, isdma):
        self.eng = eng
        self.fn = fn
        self.isdma = isdma
        self.cost = 300.0
        self.lat = 0.0
        self.odeps = []
        self.idx = 0
        self.deps = []
        self.sig = False
        self.sem = None
        self.val = 0
        self.prev_val = 0


ENGS = ("pe", "act", "dve", "pool", "sp")
N_DMA_SEMS = 32
EPOCH = 30000


class Sched:
    def __init__(self):
        self.streams = {e: [] for e in ENGS}
        self.all_ops = []
        self.dma_count = 0
        self.pending_dma = []

    @staticmethod
    def _acc(x):
        if not isinstance(x, T):
            return x, None
        if x.buf.psum:
            return x.buf, None
        try:
            ap = x.ap
            pairs = [(int(p[0]), int(p[1])) for p in ap.ap]
            off = int(ap.offset)
            esz = DTSIZE.get(ap.dtype, 4)
            if x.buf.dram:
                dims = pairs
                base = off
            else:
                pstep = pairs[0][0]
                dims = pairs[1:]
                base = off % pstep if pstep > 0 else off
            dims = [(s_, c_) for (s_, c_) in dims if c_ > 1 and s_ != 0]
            if not dims:
                return x.buf, [(base * esz, (base + 1) * esz)]
            dims.sort(key=lambda d: -abs(d[0]))
            run = 1
            if dims[-1][0] == 1:
                run = dims[-1][1]
                dims = dims[:-1]
                while dims and dims[-1][0] == run:
                    run *= dims[-1][1]
                    dims = dims[:-1]
            n = 1
            for _, c_ in dims:
                n *= c_
            if n > 96:
                hi = base + sum(abs(s_) * (c_ - 1) for s_, c_ in dims) + run
                return x.buf, [(base * esz, hi * esz)]
            starts = [base]
            for s_, c_ in dims:
                starts = [st + s_ * k for st in starts for k in range(c_)]
            ivs = sorted((st * esz, (st + run) * esz) for st in starts)
            return x.buf, ivs
        except Exception:
            return x.buf, None

    @staticmethod
    def _ovl(a, b):
        if a is None or b is None:
            return True
        i = j = 0
        while i < len(a) and j < len(b):
            if a[i][1] <= b[j][0]:
                i += 1
            elif b[j][1] <= a[i][0]:
                j += 1
            else:
                return True
        return False

    @staticmethod
    def _covers(a, b):
        if a is None:
            return True
        if b is None:
            return False
        i = 0
        for lo, hi in b:
            while i < len(a) and a[i][1] <= lo:
                i += 1
            if i >= len(a) or a[i][0] > lo or a[i][1] < hi:
                return False
        return True

    def op(self, eng, fn, reads=(), writes=(), dma=False, cost=300.0, lat=0.0):
        o = Op(eng, fn, dma)
        o.cost = cost
        o.lat = lat
        o.idx = len(self.all_ops)
        deps = set()
        racc = [self._acc(x) for x in reads]
        wacc = [self._acc(x) for x in writes]
        for b, iv in racc:
            for w, wiv in b.wr:
                if self._ovl(iv, wiv):
                    deps.add(w)
            if b.psum:
                for r, riv in b.rd:
                    if r.eng != eng:
                        deps.add(r)
        for b, iv in wacc:
            for r, riv in b.rd:
                if r is not o and self._ovl(iv, riv):
                    deps.add(r)
            for w, wiv in b.wr:
                if not self._ovl(iv, wiv):
                    continue
                if w.isdma and dma:
                    continue
                if w.isdma or dma or w.eng != eng:
                    deps.add(w)
                else:
                    o.odeps.append(w)
        for b, iv in wacc:
            b.rd = [(r, riv) for (r, riv) in b.rd if not self._covers(iv, riv)]
            b.wr = [(w, wiv) for (w, wiv) in b.wr if (w.isdma and dma) or not self._covers(iv, wiv)]
            if len(b.wr) > 200:
                for w, _ in b.wr:
                    if not (w.isdma and dma):
                        deps.add(w)
                b.wr = [(w, wiv) for (w, wiv) in b.wr if (w.isdma and dma)][-200:]
                iv = None
            b.wr.append((o, iv))
        for b, iv in racc:
            if len(b.rd) > 200:
                for r, _ in b.rd:
                    deps.add(r)
                b.rd = []
                iv = None
            b.rd.append((o, iv))
        for d in deps:
            if d is o:
                continue
            if d.eng == "pe" and eng == "pe" and not d.isdma and not dma:
                o.odeps.append(d)
                continue
            d.sig = True
            o.deps.append(d)
        self.streams[eng].append(o)
        self.all_ops.append(o)
        if dma:
            self.pending_dma.append(o)
        return o

    def barrier(self):
        lasts = []
        for e in ENGS:
            for o in reversed(self.streams[e]):
                if not o.isdma and o.fn is not None:
                    lasts.append(o)
                    break
        pend = list(self.pending_dma)
        self.pending_dma = []
        for e in ENGS:
            o = Op(e, None, False)
            o.idx = len(self.all_ops)
            for d in lasts + pend:
                d.sig = True
                o.deps.append(d)
            self.streams[e].append(o)
            self.all_ops.append(o)

    def reschedule(self):
        import heapq
        new_streams = {e: [] for e in ENGS}
        pos = {e: 0 for e in ENGS}
        done_ids = set()
        while True:
            seg = {}
            more = False
            for e in ENGS:
                st = self.streams[e]
                i = pos[e]
                j = i
                while j < len(st) and st[j].fn is not None:
                    j += 1
                seg[e] = st[i:j]
                if j < len(st):
                    more = True
            ops = [o for e in ENGS for o in seg[e]]
            inseg = set(id(o) for o in ops)
            succ = {id(o): [] for o in ops}
            nun = {}
            for o in ops:
                n = 0
                for d in list(o.deps) + list(o.odeps):
                    if id(d) in inseg:
                        succ[id(d)].append(o)
                        n += 1
                nun[id(o)] = n
            cp = {}
            for o in sorted(ops, key=lambda q: -q.idx):
                m_ = 0.0
                for s_ in succ[id(o)]:
                    v = cp[id(s_)]
                    if v > m_:
                        m_ = v
                cp[id(o)] = o.cost + o.lat + m_
            ready = {e: [] for e in ENGS}
            for o in ops:
                if nun[id(o)] == 0:
                    heapq.heappush(ready[o.eng], (-cp[id(o)] if CP_PRIORITY else o.idx, o.idx, o))
            free = {e: 0.0 for e in ENGS}
            comp = []
            now = 0.0
            nsched = 0
            fabric = 0.0
            while True:
                for e in ENGS:
                    if ready[e] and free[e] <= now:
                        _, _, o = heapq.heappop(ready[e])
                        new_streams[e].append(o)
                        nsched += 1
                        free[e] = now + o.cost
                        if o.isdma:
                            t0_ = max(now + o.cost, fabric)
                            fabric = t0_ + o.lat
                            heapq.heappush(comp, (fabric + 2000.0, o.idx, o))
                        else:
                            heapq.heappush(comp, (now + o.cost + o.lat, o.idx, o))
                nxt = [free[e] for e in ENGS if ready[e] and free[e] > now]
                if not comp and not nxt:
                    break
                tnext = min([comp[0][0]] if comp else []) if comp else None
                cand = nxt + ([comp[0][0]] if comp else [])
                now = min(cand)
                while comp and comp[0][0] <= now:
                    _, _, o = heapq.heappop(comp)
                    for s_ in succ[id(o)]:
                        nun[id(s_)] -= 1
                        if nun[id(s_)] == 0:
                            heapq.heappush(ready[s_.eng], (-cp[id(s_)] if CP_PRIORITY else s_.idx, s_.idx, s_))
            assert nsched == len(ops), (nsched, len(ops))
            for e in ENGS:
                pos[e] += len(seg[e])
                st = self.streams[e]
                if pos[e] < len(st):
                    new_streams[e].append(st[pos[e]])
                    pos[e] += 1
            if not more:
                break
        for e in ENGS:
            assert len(new_streams[e]) == len(self.streams[e]), (e, len(new_streams[e]), len(self.streams[e]))
        self.streams = new_streams

    def emit(self, nc):
        import contextlib
        stack = contextlib.ExitStack()
        with stack:
            counts = {e: 0 for e in ENGS}
            n_epochs = {e: 1 for e in ENGS}
            for o in self.all_ops:
                if o.isdma or not o.sig:
                    continue
                counts[o.eng] += 1
            for e in ENGS:
                n_epochs[e] = max(1, (counts[e] + EPOCH - 1) // EPOCH)
            esems = {e: [stack.enter_context(nc.semaphore(f"s_{e}{i}")) for i in range(n_epochs[e])] for e in ENGS}
            dsems = {e: [stack.enter_context(nc.semaphore(f"s_dma_{e}{i}")) for i in range(N_DMA_SEMS)] for e in ("sp", "pool", "act")}
            for e in ENGS:
                k = 0
                dcnt = 0
                for o in self.streams[e]:
                    if o.isdma:
                        o.sem = dsems[e][dcnt % N_DMA_SEMS]
                        o.prev_val = 16 * (dcnt // N_DMA_SEMS)
                        o.val = o.prev_val + 16
                        dcnt += 1
                    elif o.sig:
                        o.sem = esems[e][k // EPOCH]
                        o.val = (k % EPOCH) + 1
                        k += 1
            block = stack.enter_context(nc.Block())

            def run_stream(ename, eng):
                waited = {}
                pro = getattr(self, "prologue", {}).get(ename)
                if pro is not None:
                    pro(eng)
                for o in self.streams[ename]:
                    for d in o.deps:
                        key = id(d.sem)
                        if waited.get(key, 0) >= d.val:
                            continue
                        eng.wait_ge(d.sem, d.val)
                        waited[key] = d.val
                    if o.isdma and o.prev_val > 0:
                        key = id(o.sem)
                        if waited.get(key, 0) < o.prev_val:
                            eng.wait_ge(o.sem, o.prev_val)
                            waited[key] = o.prev_val
                    if o.fn is None:
                        continue
                    ins = o.fn(eng)
                    if o.isdma:
                        ins.then_inc(o.sem, 16)
                    elif o.sig:
                        ins.then_inc(o.sem, 1)

            @block.tensor
            def _(eng):
                run_stream("pe", eng)

            @block.scalar
            def _(eng):
                run_stream("act", eng)

            @block.vector
            def _(eng):
                run_stream("dve", eng)

            @block.gpsimd
            def _(eng):
                run_stream("pool", eng)

            @block.sync
            def _(eng):
                run_stream("sp", eng)


class Arena:
    def __init__(self, nc, nbytes):
        self.h = nc.alloc_sbuf_tensor("arena", [128, nbytes], U8)
        self.nbytes = nbytes
        self.off = 0

    def alloc(self, shape, dtype, name="", nparts=128):
        n = int(np.prod(shape)) * DTSIZE[dtype]
        off = (self.off + 31) // 32 * 32
        assert off + n <= self.nbytes, (name, off, n, self.nbytes)
        self.off = off + n
        ap = self.h[0:nparts, off:off + n].bitcast(dtype)
        if len(shape) == 2:
            ap = ap.rearrange("p (a b) -> p a b", b=shape[1])
        elif len(shape) == 3:
            ap = ap.rearrange("p (a b c) -> p a b c", b=shape[1], c=shape[2])
        return T(ap, Buf(name))

    def mark(self):
        return self.off

    def release(self, m):
        self.off = m


D = 1024
SEQ = 2048
NT = 16
NTG = 4
P_IN = 3348
ALPHA = 4 ** 0.25
LN_EPS = 1e-5
RMS_EPS = 1e-6
N_EXP = 32
RESCHEDULE = True
CP_PRIORITY = True

C_CONV = 0
C_GLA = 768
C_SSD = 1552
C_DIFF = 2580

PP_OFF = {}
_o = 0
for _n, _w in (("conv_w", 6), ("gla_b", 128), ("gla_g", 256), ("gla_wlr", 128), ("ssd_cw", 24), ("ssd_cb", 6),
               ("ssd_alog", 4), ("ssd_d", 256), ("ssd_dtb", 4), ("ssd_g", 256), ("diff_l", 128), ("diff_g", 256),
               ("ln1_g", 1024), ("ln1_b", 1024), ("ln2_g", 1024), ("ln2_b", 1024)):
    PP_OFF[_n] = (_o, _w)
    _o += _w
NPP = _o
NPPS = PP_OFF["ln1_g"][0]

CO_OFF = {}
_o = 0
for _n, _w in (("ident", 128), ("triU", 128), ("triUn16", 128), ("trisL", 128), ("ones", 128), ("hm", 4), ("triUs", 128), ("thr16", 16), ("bthr", 48), ("pc", 8)):
    CO_OFF[_n] = (_o, _w)
    _o += _w
NCO = _o


def host_consts():
    c = np.zeros((128, NCO), np.float32)
    p = np.arange(128)[:, None]
    j = np.arange(128)[None, :]
    c[:, CO_OFF["ident"][0]:CO_OFF["ident"][0] + 128] = (p == j)
    c[:, CO_OFF["triU"][0]:CO_OFF["triU"][0] + 128] = (p <= j)
    c[:, CO_OFF["triUn16"][0]:CO_OFF["triUn16"][0] + 128] = (p <= j) * np.float32(-1.0 / 16.0)
    c[:, CO_OFF["trisL"][0]:CO_OFF["trisL"][0] + 128] = (p > j)
    c[:, CO_OFF["ones"][0]:CO_OFF["ones"][0] + 128] = 1.0
    c[:, CO_OFF["hm"][0]:CO_OFF["hm"][0] + 4] = ((p // 32) == np.arange(4)[None, :])
    c[:, CO_OFF["triUs"][0]:CO_OFF["triUs"][0] + 128] = (p < j)
    c[:, CO_OFF["thr16"][0]:CO_OFF["thr16"][0] + 16] = 512.0 * np.arange(16)[None, :]
    c[:, CO_OFF["bthr"][0]:CO_OFF["bthr"][0] + 48] = 512.0 * np.arange(48)[None, :]
    c[:, CO_OFF["pc"][0]:CO_OFF["pc"][0] + 8] = 128.0 * np.arange(8)[None, :] + p
    return c


def host_pp(inp, l):
    pp = np.zeros((128, NPP), np.float32)

    def put(name, arr):
        o, w = PP_OFF[name]
        assert arr.shape == (128, w), (name, arr.shape)
        pp[:, o:o + w] = arr

    def row(v):
        return np.broadcast_to(np.asarray(v, np.float32)[None, :], (128, len(v)))

    cw = inp["conv_w"][l]
    put("conv_w", np.stack([cw[k, fc * 128:(fc + 1) * 128] for fc in range(2) for k in range(3)], axis=1))
    put("gla_b", row(inp["gla_b_lr"][l]))
    put("gla_g", row(np.tile(inp["gla_norm_g"][l], 4)))
    wl = np.zeros((128, 128), np.float32)
    wl[:16] = inp["gla_w_lr"][l]
    put("gla_wlr", wl)
    sw = inp["ssd_conv_w"][l]
    put("ssd_cw", np.stack([sw[k, c6 * 128:(c6 + 1) * 128] for c6 in range(6) for k in range(4)], axis=1))
    sb = inp["ssd_conv_b"][l]
    put("ssd_cb", np.stack([sb[c6 * 128:(c6 + 1) * 128] for c6 in range(6)], axis=1))
    put("ssd_alog", row(inp["ssd_a_log"][l]))
    put("ssd_d", row(np.repeat(inp["ssd_d"][l], 64)))
    put("ssd_dtb", row(inp["ssd_dt_bias"][l]))
    put("ssd_g", row(inp["ssd_norm_g"][l]))
    put("diff_l", row(np.concatenate([inp["diff_lq1"][l], inp["diff_lk1"][l], inp["diff_lq2"][l], inp["diff_lk2"][l]])))
    put("diff_g", row(np.tile(inp["diff_norm_g"][l], 4)))
    for n in ("ln1_g", "ln1_b", "ln2_g", "ln2_b"):
        put(n, row(inp[n][l]))
    return pp


class Prog:
    def __init__(self, nseq, n_layers, with_moe, stages="cgsd", dbg=False):
        self.nseq, self.n_layers, self.with_moe, self.stages, self.dbg = nseq, n_layers, with_moe, stages, dbg
        nc = self.nc = bass.Bass("TRN2", target_bir_lowering=False)
        ntok = nseq * SEQ
        dt = nc.dram_tensor
        self.x_d = dt("x", [ntok, D], F32, kind="ExternalInput").ap()
        self.win_d = dt("w_in", [2, D, P_IN], F32, kind="ExternalInput").ap()
        self.wo_d = dt("w_o", [2, D, D], F32, kind="ExternalInput").ap()
        self.pp_d = dt("pp", [2 * 128, NPP], F32, kind="ExternalInput").ap()
        self.co_d = dt("co", [128, NCO], F32, kind="ExternalInput").ap()
        if with_moe:
            self.rt_d = dt("rt", [2, D, 36], F32, kind="ExternalInput").ap()
            self.wg_d = dt("w_gate", [2, N_EXP, D, 512], F32, kind="ExternalInput").ap()
            self.wu_d = dt("w_up", [2, N_EXP, D, 512], F32, kind="ExternalInput").ap()
            self.wd_d = dt("w_down", [2, N_EXP, 512, D], F32, kind="ExternalInput").ap()
        self.y_d = dt("y", [ntok, D], F32, kind="ExternalOutput").ap()
        if dbg:
            self.dbg_d = dt("dbg", [ntok, D], F32, kind="ExternalOutput").ap()
        self.S = Sched()
        self.A = Arena(nc, 207 * 1024)
        self.banks = []
        for i in range(8):
            b = Buf(f"ps{i}")
            b.psum = True
            self.banks.append(T(nc.alloc_psum_tensor(f"ps{i}", [128, 512], F32)[:, :], b))
        self.pool = {"free": list(range(8)), "rr": 0}
        self.bounds = set()
        self.bregs = {}
        self.build()
        self.S.barrier()

        def pool_prologue(eng):
            for bv in sorted(self.bounds):
                r = nc.alloc_register(mybir.EngineType.Pool, f"bnd{bv}")
                eng.reg_mov(r, int(bv))
                self.bregs[bv] = r
        self.S.prologue = {"pool": pool_prologue}
        if RESCHEDULE:
            self.S.reschedule()
        self.S.emit(nc)

    def ps(self):
        pool = self.pool
        i = pool["free"][pool["rr"] % len(pool["free"])]
        pool["rr"] += 1
        return self.banks[i]

    def reserve(self):
        i = self.pool["free"].pop()
        return i, self.banks[i]

    def unreserve(self, i):
        self.pool["free"].append(i)

    def run_gens(self, specs):
        active = [[g, w, {"free": list(banks), "rr": 0}] for g, w, banks in specs]
        save = self.pool
        while active:
            for item in list(active):
                g, w, pool = item
                self.pool = pool
                for _ in range(w):
                    try:
                        next(g)
                    except StopIteration:
                        active.remove(item)
                        break
        self.pool = save

    @staticmethod
    def _a(x):
        return x.ap if isinstance(x, T) else x

    @staticmethod
    def _bufs(*xs):
        return [x for x in xs if isinstance(x, T)]

    @staticmethod
    def _fs(x):
        x = x.ap if isinstance(x, T) else x
        try:
            return float(x.free_size())
        except Exception:
            return 256.0

    def mm(self, out, lhsT, rhs, start=True, stop=True):
        a = self._a
        n = self._fs(rhs)
        c = max(64.0, n) / 2.4 + 40.0 + self._fs(lhsT) / 2.4 * 0.5
        if a(rhs).dtype == F32:
            c *= 4.0
        self.S.op("pe", lambda e: e.matmul(a(out), a(lhsT), a(rhs), start=start, stop=stop),
                  reads=self._bufs(lhsT, rhs), writes=self._bufs(out), cost=c, lat=0.0)

    def tr(self, out, in_):
        a = self._a
        self.S.op("pe", lambda e: e.transpose(a(out), a(in_), a(self.identb)),
                  reads=self._bufs(in_, self.identb), writes=self._bufs(out), cost=120.0, lat=0.0)

    def act(self, out, in_, func=None, bias=None, scale=None, accum=None, eng="act"):
        a = self._a
        kw = {}
        if bias is not None:
            kw["bias"] = a(bias)
        if scale is not None:
            kw["scale"] = a(scale)
        if accum is not None:
            kw["accum_out"] = a(accum)
        f = func if func is not None else AF.Copy
        self.S.op("act", lambda e: e.activation(a(out), a(in_), f, **kw),
                  reads=self._bufs(in_, bias, scale), writes=self._bufs(out, accum), cost=200.0 + 0.85 * self._fs(out), lat=0.0)

    def tt(self, out, in0, in1, op, eng="dve"):
        a = self._a
        self.S.op(eng, lambda e: e.tensor_tensor(a(out), a(in0), a(in1), op),
                  reads=self._bufs(in0, in1), writes=self._bufs(out), cost=self._vc(eng, out), lat=0.0)

    def ts(self, out, in0, s1, op0, s2=None, op1=None, eng="dve"):
        a = self._a
        if op1 is None:
            self.S.op(eng, lambda e: e.tensor_scalar(a(out), a(in0), a(s1), None, op0),
                      reads=self._bufs(in0, s1), writes=self._bufs(out), cost=self._vc(eng, out), lat=0.0)
        else:
            self.S.op(eng, lambda e: e.tensor_scalar(a(out), a(in0), a(s1), a(s2), op0, op1),
                      reads=self._bufs(in0, s1, s2), writes=self._bufs(out), cost=self._vc(eng, out), lat=0.0)

    def stt(self, out, in0, scalar, in1, op0, op1, accum=None):
        a = self._a
        if accum is None:
            self.S.op("dve", lambda e: e.scalar_tensor_tensor(a(out), a(in0), a(scalar), a(in1), op0, op1),
                      reads=self._bufs(in0, scalar, in1), writes=self._bufs(out), cost=self._vc("dve", out), lat=0.0)
        else:
            self.S.op("dve", lambda e: e.scalar_tensor_tensor(a(out), a(in0), a(scalar), a(in1), op0, op1, accum_out=a(accum)),
                      reads=self._bufs(in0, scalar, in1), writes=self._bufs(out, accum), cost=self._vc("dve", out) + 100.0, lat=0.0)

    def silu(self, out, x):
        self.act(out, x, AF.Exp, scale=-1.0)
        self.act(out, out, AF.Ln, bias=self.oner)
        self.act(out, out, AF.Exp, scale=-1.0)
        self.tt(out, out, x, ALU.mult)

    def _vc(self, eng, x):
        n = self._fs(x)
        return (400.0 + 6.0 * n) if eng == "pool" else (160.0 + 1.0 * n)

    def copy(self, out, in_, eng="dve"):
        a = self._a
        if eng == "act":
            self.S.op("act", lambda e: e.copy(a(out), a(in_)), reads=self._bufs(in_), writes=self._bufs(out),
                      cost=200.0 + 0.85 * self._fs(out), lat=0.0)
        else:
            self.S.op(eng, lambda e: e.tensor_copy(a(out), a(in_)), reads=self._bufs(in_), writes=self._bufs(out),
                      cost=self._vc(eng, out), lat=0.0)

    def memset(self, out, v, eng="pool"):
        a = self._a
        self.S.op(eng, lambda e: e.memset(a(out), v), writes=self._bufs(out), cost=self._vc(eng, out), lat=0.0)

    def reduce(self, out, in_, op=ALU.add):
        a = self._a
        self.S.op("dve", lambda e: e.tensor_reduce(a(out), a(in_), AX.X, op), reads=self._bufs(in_), writes=self._bufs(out),
                  cost=self._vc("dve", in_), lat=0.0)

    def recip(self, out, in_):
        a = self._a
        self.S.op("dve", lambda e: e.reciprocal(a(out), a(in_)), reads=self._bufs(in_), writes=self._bufs(out),
                  cost=200.0 + 7.0 * self._fs(out), lat=0.0)

    def dma(self, out, in_, q="sp"):
        a = self._a
        nb = self._fs(out) * 128.0 * 4.0
        self.S.op(q, lambda e: e.dma_start(out=a(out), in_=a(in_)), reads=self._bufs(in_), writes=self._bufs(out), dma=True,
                  cost=(1000.0 if q == "pool" else 150.0), lat=nb / 300.0)

    def co(self, name):
        o, w = CO_OFF[name]
        return self.CO[:, o:o + w]

    def ppv(self, name, a=0, b=None):
        o, w = PP_OFF[name]
        b = w if b is None else b
        return self.PP[:, o + a:o + b]

    def load_w(self, Wt, dram2d, c0, ncols, kchunks=8):
        src = dram2d[:, c0:c0 + ncols].rearrange("(c p) n -> p c n", p=128)
        self.dma(Wt[:, 0:kchunks, 0:ncols], src, q="pool")

    def proj_fm(self, ps, W, c0, ncol, tg):
        for c in range(8):
            self.mm(ps[0:ncol, :], W[:, c, c0:c0 + ncol], self.XT[:, c, tg * 512:(tg + 1) * 512], start=(c == 0), stop=(c == 7))

    def proj_tm(self, ps, W, c0, ncol, t):
        for c in range(8):
            self.mm(ps[:, 0:ncol], self.XT[:, c, t * 128:(t + 1) * 128], W[:, c, c0:c0 + ncol], start=(c == 0), stop=(c == 7))

    def make_XT(self, src, s_):
        A = self.A
        xf = [A.alloc([1024], F32, "xf0")] * 2
        xb = [A.alloc([1024], BF16, f"xb{i}") for i in range(2)]
        for t in range(NT):
            r0 = s_ * SEQ + t * 128
            self.dma(xf[t % 2], src[r0:r0 + 128, :])
            self.copy(xb[t % 2], xf[t % 2], eng="act")
            for hh in range(2):
                ps = self.ps()
                for k in range(4):
                    c = hh * 4 + k
                    self.mm(ps[:, k * 128:(k + 1) * 128], xb[t % 2][:, c * 128:(c + 1) * 128], self.identb)
                self.copy(T(self.XT.ap[:, hh * 4:hh * 4 + 4, t * 128:(t + 1) * 128], self.XT.buf),
                          T(ps.ap.rearrange("p (k j) -> p k j", j=128), ps.buf), eng=("act" if hh == 0 else "dve"))

    def norm_tr(self, o_sb, ng, grow, ytc0, t, tmp, extra=None, post=None):
        gs = 256 // ng
        sq, ss, ybf = tmp["sq"], tmp["ss"], tmp["ybf"]
        self.tt(sq, o_sb, o_sb, ALU.mult)
        self.reduce(ss[:, 0:ng], sq.ap.rearrange("p (g e) -> p g e", e=gs) if False else T(sq.ap.rearrange("p (g e) -> p g e", e=gs), sq.buf))
        self.act(ss[:, 0:ng], ss[:, 0:ng], AF.Ln, bias=self.epsr, scale=1.0 / gs)
        self.act(ss[:, 0:ng], ss[:, 0:ng], AF.Exp, scale=-0.5)
        o3 = T(o_sb.ap.rearrange("p (g e) -> p g e", e=gs), o_sb.buf)
        s3 = T(sq.ap.rearrange("p (g e) -> p g e", e=gs), sq.buf)
        rb = T(ss.ap[:, 0:ng].unsqueeze(2).to_broadcast([128, ng, gs]), ss.buf)
        self.tt(s3, o3, rb, ALU.mult)
        if extra is not None:
            self.tt(sq, sq, extra, ALU.mult)
        if post is not None:
            self.stt(ybf, sq, float(post), grow, ALU.mult, ALU.mult)
        else:
            self.tt(ybf, sq, grow, ALU.mult)
        ps = self.ps()
        for j in range(2):
            self.mm(ps[:, j * 128:(j + 1) * 128], ybf[:, j * 128:(j + 1) * 128], self.identb)
        self.copy(T(self.YT.ap[:, ytc0:ytc0 + 2, t * 128:(t + 1) * 128], self.YT.buf),
                  T(ps.ap[:, 0:256].rearrange("p (j k) -> p j k", k=128), ps.buf), eng="act")

    def conv_mixer(self, l):
        A = self.A
        W = self.Wb[1]
        self.load_w(W, self.win_d[l], C_CONV, 768)
        m = A.mark()
        cu = A.alloc([2050], F32, "cu")
        Bf = A.alloc([2048], BF16, "Bf")
        acc = A.alloc([2048], F32, "acc")
        tmp = [A.alloc([512], F32, "ctmp0")] * 2
        self.memset(cu[:, 0:2], 0.0)
        for fc in range(2):
            for tg in range(NTG):
                pu, pc, pb = self.ps(), self.ps(), self.ps()
                self.proj_fm(pu, W, fc * 128, 128, tg)
                self.proj_fm(pc, W, 512 + fc * 128, 128, tg)
                self.proj_fm(pb, W, 256 + fc * 128, 128, tg)
                tm = tmp[tg % 2]
                self.copy(tm, pu, eng="act")
                self.tt(cu[:, 2 + tg * 512:2 + (tg + 1) * 512], tm, pc, ALU.mult)
                self.copy(Bf[:, tg * 512:(tg + 1) * 512], pb, eng="act")
                yield
            w = [self.ppv("conv_w", fc * 3 + k, fc * 3 + k + 1) for k in range(3)]
            self.act(acc, cu[:, 0:2048], AF.Copy, scale=w[0])
            self.stt(acc, cu[:, 1:2049], w[1], acc, ALU.mult, ALU.add)
            self.stt(acc, cu[:, 2:2050], w[2], acc, ALU.mult, ALU.add)
            self.tt(self.YT[:, fc, :], acc, Bf, ALU.mult)
            yield

    def gla_mixer(self, l):
        A = self.A
        W = self.Wb[0]
        self.load_w(W, self.win_d[l], C_GLA, 784)
        m = A.mark()
        qf = A.alloc([2048], F32, "qf")
        kf = A.alloc([2048], F32, "kf")
        lrT = A.alloc([2048], F32, "lrT")
        cumT = A.alloc([2048], F32, "cumT")
        big = lrT
        qdm = [A.alloc([2048], BF16, f"qdm{h}") for h in range(4)]
        ki = A.alloc([2048], BF16, "ki")
        keT = A.alloc([2048], BF16, "keT")
        ke_tok = A.alloc([16, 128], BF16, "ke_tok")
        dec = A.alloc([16], F32, "dec")
        zb = [A.alloc([128], F32, "zb0")] * 2
        Sf = A.alloc([256], F32, "Sf")
        Sb = A.alloc([256], BF16, "Sb")
        vtok = [A.alloc([256], BF16, f"vtok{i}") for i in range(2)]
        sg = [A.alloc([256], F32, "sg0")] * 2
        attm = [A.alloc([4, 128], BF16, f"attm{i}") for i in range(2)]
        osb = [A.alloc([256], F32, "osb0")] * 2
        tmp = [dict(sq=A.alloc([256], F32, "sq0"), ss=A.alloc([4], F32, "ss0"), ybf=A.alloc([256], BF16, "ybf0"))] * 2
        for tg in range(NTG):
            p1, p2, p3 = self.ps(), self.ps(), self.ps()
            self.proj_fm(p1, W, 0, 128, tg)
            self.proj_fm(p2, W, 128, 128, tg)
            self.proj_fm(p3, W, 768, 16, tg)
            self.copy(qf[:, tg * 512:(tg + 1) * 512], p1, eng="act")
            self.copy(kf[:, tg * 512:(tg + 1) * 512], p2, eng="dve")
            self.copy(lrT[0:16, tg * 512:(tg + 1) * 512], p3[0:16, :], eng="act")
            yield
        wlr = self.ppv("gla_wlr")
        blr = self.ppv("gla_b")
        for tg in range(NTG):
            pci, pc = self.reserve()
            for k in range(4):
                t = tg * 4 + k
                pz = self.ps()
                self.mm(pz[:, 0:128], lrT[0:16, t * 128:(t + 1) * 128], wlr[0:16, :])
                z = zb[t % 2]
                self.tt(z, pz[:, 0:128], blr, ALU.add)
                self.act(z, z, AF.Exp, scale=-1.0)
                self.act(z, z, AF.Ln, bias=self.oner)
                self.mm(pc[:, k * 128:(k + 1) * 128], z, self.co("triUn16"))
            self.copy(cumT[:, tg * 512:(tg + 1) * 512], pc, eng="act")
            self.unreserve(pci)
            yield
        cl = T(cumT.ap.rearrange("p (t k) -> p t k", k=128)[:, :, 127], cumT.buf)
        self.act(dec, cl, AF.Exp)
        self.act(big, cumT, AF.Exp)
        self.tt(qf, qf, big, ALU.mult)
        for h in range(4):
            self.ts(qdm[h], qf, self.co("hm")[:, h:h + 1], ALU.mult, float(32 ** -0.5), ALU.mult)
        self.act(big, cumT, AF.Exp, scale=-1.0)
        self.tt(ki, kf, big, ALU.mult)
        for t in range(NT):
            sl = slice(t * 128, (t + 1) * 128)
            z = zb[t % 2]
            self.act(z, cumT[:, sl], AF.Exp, scale=-1.0, bias=cl[:, t:t + 1])
            self.tt(keT[:, sl], kf[:, sl], z, ALU.mult)
            pt = self.ps()
            self.mm(pt[:, 0:128], keT[:, sl], self.identb)
            self.copy(ke_tok[:, t, :], pt[:, 0:128], eng="act")
            if t % 2 == 1:
                yield
        self.memset(Sf, 0.0)
        self.memset(Sb, 0.0)
        triU3 = T(self.co("triU").ap.unsqueeze(1).to_broadcast([128, 4, 128]), self.CO.buf)
        for t in range(NT):
            sl = slice(t * 128, (t + 1) * 128)
            v, g_, am, ob = vtok[t % 2], sg[t % 2], attm[t % 2], osb[t % 2]
            pv = self.ps()
            self.proj_tm(pv, W, 256, 256, t)
            self.copy(v, pv[:, 0:256], eng="act")
            pg = self.ps()
            self.proj_tm(pg, W, 512, 256, t)
            self.silu(g_, pg[:, 0:256])
            yield
            pa = self.ps()
            for h in range(4):
                self.mm(pa[:, h * 128:(h + 1) * 128], ki[:, sl], qdm[h][:, sl])
            self.tt(am, T(pa.ap.rearrange("p (h i) -> p h i", i=128), pa.buf), triU3, ALU.mult)
            yield
            po = self.ps()
            for h in range(4):
                hs = slice(h * 64, (h + 1) * 64)
                self.mm(po[:, hs], qdm[h][:, sl], Sb[:, hs], start=True, stop=False)
                self.mm(po[:, hs], am[:, h, :], v[:, hs], start=False, stop=True)
            yield
            pd = self.ps()
            self.mm(pd[:, 0:256], ke_tok[:, t, :], v)
            self.stt(Sf, Sf, dec[:, t:t + 1], pd[:, 0:256], ALU.mult, ALU.add)
            self.copy(Sb, Sf, eng="act")
            self.copy(ob, po[:, 0:256], eng="act")
            yield
            self.norm_tr(ob, 4, self.ppv("gla_g"), 2, t, tmp[t % 2], extra=g_)
            yield

    def ssd_mixer(self, l):
        A = self.A
        W = self.Wb[1]
        self.load_w(W, self.win_d[l], C_SSD, 1028)
        m = A.mark()
        pre = [A.alloc([2051], F32, "pre0")] * 2
        acc = A.alloc([2048], F32, "sacc")
        cv = [A.alloc([2048], BF16, f"cv{i}") for i in range(6)]
        dtt = A.alloc([16, 4], F32, "dtt")
        dtA = A.alloc([16, 4], F32, "dtA")
        arow = A.alloc([4], F32, "arow")
        STf = A.alloc([256], F32, "STf")
        STb = A.alloc([256], BF16, "STb")
        xs_tok = [A.alloc([256], F32, f"xs_tok{i}") for i in range(2)]
        B_tok = [A.alloc([256], BF16, f"B_tok{i}") for i in range(2)]
        xdt = [A.alloc([256], F32, f"xdt{i}") for i in range(2)]
        xdtb = [A.alloc([256], BF16, f"xdtb{i}") for i in range(2)]
        xdd = [A.alloc([256], BF16, f"xdd{i}") for i in range(2)]
        cbm = [A.alloc([2, 128], F32, f"cbm{i}") for i in range(2)]
        lhsD = [A.alloc([128], F32, f"lhsD{i}") for i in range(2)]
        eD = [A.alloc([128], F32, f"eD{i}") for i in range(2)]
        MT = [A.alloc([128], BF16, f"MT{i}") for i in range(2)]
        ecum = [A.alloc([4], F32, f"ecum{i}") for i in range(2)]
        decs = [A.alloc([4], F32, f"decs{i}") for i in range(2)]
        t1 = [A.alloc([256], F32, f"t1{i}") for i in range(2)]
        t2 = [A.alloc([256], F32, "t2_0")] * 2
        sz = [A.alloc([256], F32, "sz_0")] * 2
        tmp = [dict(sq=A.alloc([256], F32, f"ssq{i}"), ss=A.alloc([4], F32, f"sss{i}"), ybf=A.alloc([256], BF16, f"sybf{i}")) for i in range(2)]
        self.memset(pre[0][:, 0:3], 0.0)
        for c6 in range(6):
            pr = pre[c6 % 2]
            for tg in range(NTG):
                ps = self.ps()
                self.proj_fm(ps, W, 256 + c6 * 128, 128, tg)
                self.copy(pr[:, 3 + tg * 512:3 + (tg + 1) * 512], ps, eng=("act" if tg % 2 == 0 else "dve"))
            w = [self.ppv("ssd_cw", c6 * 4 + k, c6 * 4 + k + 1) for k in range(4)]
            self.act(acc, pr[:, 0:2048], AF.Copy, scale=w[0])
            for k in range(1, 4):
                self.stt(acc, pr[:, k:k + 2048], w[k], acc, ALU.mult, ALU.add)
            self.ts(acc, acc, self.ppv("ssd_cb", c6, c6 + 1), ALU.add)
            scr = pr[:, 3:2051]
            self.act(scr, acc, AF.Exp, scale=-1.0)
            self.act(scr, scr, AF.Ln, bias=self.oner)
            self.act(scr, scr, AF.Exp, scale=-1.0)
            self.tt(cv[c6], acc, scr, ALU.mult)
            yield
        self.act(arow, self.ppv("ssd_alog"), AF.Exp)
        self.ts(arow, arow, -1.0, ALU.mult)
        for t in range(NT):
            ps = self.ps()
            self.proj_tm(ps, W, 1024, 4, t)
            self.tt(dtt[:, t, :], ps[:, 0:4], self.ppv("ssd_dtb"), ALU.add)
            if t % 4 == 3:
                yield
        self.act(dtt, dtt, AF.Exp)
        self.act(dtt, dtt, AF.Ln, bias=self.oner)
        self.tt(dtA, dtt, T(arow.ap.unsqueeze(1).to_broadcast([128, 16, 4]), arow.buf), ALU.mult)
        self.memset(STf, 0.0)
        self.memset(STb, 0.0)
        triU = self.co("triU")
        triU2 = T(triU.ap.unsqueeze(1).to_broadcast([128, 2, 128]), self.CO.buf)
        for t in range(NT):
            sl = slice(t * 128, (t + 1) * 128)
            i2 = t % 2
            ptx = self.ps()
            for j in range(2):
                self.mm(ptx[:, j * 128:(j + 1) * 128], cv[j][:, sl], self.identb)
                self.mm(ptx[:, 256 + j * 128:256 + (j + 1) * 128], cv[2 + j][:, sl], self.identb)
            self.copy(xs_tok[i2], ptx[:, 0:256], eng="act")
            self.copy(B_tok[i2], ptx[:, 256:512], eng="dve")
            x3 = T(xs_tok[i2].ap.rearrange("p (h e) -> p h e", e=64), xs_tok[i2].buf)
            dtb = T(dtt.ap[:, t, :].unsqueeze(2).to_broadcast([128, 4, 64]), dtt.buf)
            self.tt(T(xdt[i2].ap.rearrange("p (h e) -> p h e", e=64), xdt[i2].buf), x3, dtb, ALU.mult)
            self.copy(xdtb[i2], xdt[i2], eng="act")
            yield
            pcb = self.ps()
            for g in range(2):
                self.mm(pcb[:, g * 128:(g + 1) * 128], cv[2 + g][:, sl], cv[4 + g][:, sl])
            self.tt(cbm[i2], T(pcb.ap[:, 0:256].rearrange("p (g i) -> p g i", i=128), pcb.buf), triU2, ALU.mult)
            pcm = self.ps()
            self.mm(pcm[:, 0:4], triU, dtA[:, t, :])
            self.mm(pcm[:, 4:8], self.co("ones"), dtA[:, t, :])
            self.act(ecum[i2], pcm[:, 0:4], AF.Exp)
            self.act(decs[i2], pcm[:, 4:8], AF.Exp)
            yield
            pyi, py = self.reserve()
            pyoi, pyo = self.reserve()
            for hd in range(4):
                g = hd // 2
                hs = slice(hd * 64, (hd + 1) * 64)
                k2 = hd % 2
                self.ts(lhsD[k2], self.co("trisL"), dtA[:, t, hd:hd + 1], ALU.mult)
                pD = self.ps()
                self.mm(pD[:, 0:128], lhsD[k2], triU)
                self.act(eD[k2], pD[:, 0:128], AF.Exp)
                self.tt(MT[k2], eD[k2], cbm[i2][:, g, :], ALU.mult)
                self.mm(py[:, hs], MT[k2], xdtb[i2][:, hs])
                self.mm(pyo[:, hs], cv[4 + g][:, sl], STb[:, hs])
                self.ts(xdd[i2][:, hs], xdt[i2][:, hs], eD[k2][:, 127:128], ALU.mult)
                yield
            pst = self.ps()
            for g in range(2):
                gs = slice(g * 128, (g + 1) * 128)
                self.mm(pst[:, gs], B_tok[i2][:, gs], xdd[i2][:, gs])
            S3 = T(STf.ap.rearrange("p (h e) -> p h e", e=64), STf.buf)
            self.tt(S3, S3, T(decs[i2].ap.unsqueeze(2).to_broadcast([128, 4, 64]), decs[i2].buf), ALU.mult)
            self.tt(STf, STf, pst[:, 0:256], ALU.add)
            self.copy(STb, STf, eng="act")
            yield
            a3 = T(t1[i2].ap.rearrange("p (h e) -> p h e", e=64), t1[i2].buf)
            self.tt(a3, T(pyo.ap[:, 0:256].rearrange("p (h e) -> p h e", e=64), pyo.buf),
                    T(ecum[i2].ap.unsqueeze(2).to_broadcast([128, 4, 64]), ecum[i2].buf), ALU.mult)
            self.tt(t1[i2], t1[i2], py[:, 0:256], ALU.add)
            self.unreserve(pyoi)
            self.unreserve(pyi)
            self.tt(t2[i2], xs_tok[i2], self.ppv("ssd_d"), ALU.mult, eng="dve")
            self.tt(t1[i2], t1[i2], t2[i2], ALU.add)
            yield
            pz = self.ps()
            self.proj_tm(pz, W, 0, 256, t)
            self.silu(sz[i2], pz[:, 0:256])
            self.tt(t1[i2], t1[i2], sz[i2], ALU.mult)
            yield
            self.norm_tr(t1[i2], 2, self.ppv("ssd_g"), 4, t, tmp[i2])
            yield

    def diff_mixer(self, l):
        A = self.A
        W = self.Wb[0]
        self.load_w(W, self.win_d[l], C_DIFF, 768)
        m = A.mark()
        lam_init = 0.8 - 0.6 * float(np.exp(-0.3 * l))
        kT = A.alloc([2, 2048], BF16, "kT")
        vtok = A.alloc([16, 4, 65], BF16, "dvtok")
        Otok = [A.alloc([4, 256], F32, f"Otok{i}") for i in range(2)]
        qm = [[A.alloc([512], BF16, f"qm{c}{i}") for i in range(2)] for c in range(2)]
        pts = [A.alloc([512], BF16, f"pt{i}") for i in range(3)]
        lt = A.alloc([64], F32, "lt")
        lam = A.alloc([4], F32, "lam")
        rec = [A.alloc([2, 4], F32, f"rec{i}") for i in range(2)]
        o1 = [A.alloc([4, 64], F32, f"o1{i}") for i in range(2)]
        o2 = [A.alloc([4, 64], F32, f"o2{i}") for i in range(2)]
        tmp = [dict(sq=A.alloc([256], F32, f"dsq{i}"), ss=A.alloc([4], F32, f"dss{i}"), ybf=A.alloc([256], BF16, f"dybf{i}")) for i in range(2)]
        dl = self.ppv("diff_l")
        self.tt(lt[:, 0:32], dl[:, 0:32], dl[:, 32:64], ALU.mult)
        self.tt(lt[:, 32:64], dl[:, 64:96], dl[:, 96:128], ALU.mult)
        self.reduce(lam[:, 0:2], T(lt.ap.rearrange("p (a b) -> p a b", b=32), lt.buf))
        self.act(lam[:, 0:2], lam[:, 0:2], AF.Exp)
        self.tt(lam[:, 2:3], lam[:, 0:1], lam[:, 1:2], ALU.subtract)
        self.ts(lam[:, 3:4], lam[:, 2:3], float(lam_init), ALU.add, -1.0, ALU.mult)
        nlam = lam[:, 3:4]
        for kc in range(2):
            for tg in range(NTG):
                ps = self.ps()
                self.proj_fm(ps, W, 256 + kc * 128, 128, tg)
                self.copy(kT[:, kc, tg * 512:(tg + 1) * 512], ps, eng=("act" if tg % 2 else "dve"))
                yield
        self.memset(vtok, 1.0)
        for t in range(NT):
            ps = self.ps()
            self.proj_tm(ps, W, 512, 256, t)
            self.copy(T(vtok.ap[:, t, :, 0:64], vtok.buf), T(ps.ap[:, 0:256].rearrange("p (h e) -> p h e", e=64), ps.buf), eng="act")
            if t % 4 == 3:
                yield
        scale = float(32 ** -0.5)
        ptc = 0
        for qg in range(NTG):
            for h in range(4):
                qc = h // 2
                k2 = h % 2
                pq = self.ps()
                self.proj_fm(pq, W, qc * 128, 128, qg)
                for c in range(2):
                    b = (h % 2) * 2 + c
                    self.ts(qm[c][k2], pq, self.co("hm")[:, b:b + 1], ALU.mult, scale, ALU.mult)
                ib = [self.reserve(), self.reserve()]
                steps = [(c, jt) for c in range(2) for jt in range(4 * qg + 4)]

                def qk(c, jt):
                    n0 = max(0, jt - 4 * qg) * 128
                    ps = self.ps()
                    self.mm(ps[:, n0:512], kT[:, qc, jt * 128:(jt + 1) * 128], qm[c][k2][:, n0:512])
                    return ps
                cur = qk(*steps[0])
                for si, (c, jt) in enumerate(steps):
                    nxt = qk(*steps[si + 1]) if si + 1 < len(steps) else None
                    po = ib[c][1]
                    i0 = max(0, jt - 4 * qg)
                    n0 = i0 * 128
                    pt = pts[ptc % 3]
                    ptc += 1
                    self.act(pt[:, n0:512], cur[:, n0:512], AF.Exp)
                    if jt >= 4 * qg:
                        self.tt(pt[:, n0:n0 + 128], pt[:, n0:n0 + 128], self.triUb, ALU.mult, eng="dve")
                    for it in range(i0, 4):
                        self.mm(po[:, it * 65:(it + 1) * 65], pt[:, it * 128:(it + 1) * 128], vtok[:, jt, h, :],
                                start=(jt == 0 and it == 0), stop=(jt == 4 * qg + it))
                    cur = nxt
                    yield
                r = rec[k2]
                for c in range(2):
                    po = ib[c][1]
                    p3 = T(po.ap[:, 0:260].rearrange("p (i e) -> p i e", e=65), po.buf)
                    self.recip(r[:, c, :], p3[:, :, 64])
                    dst = o1[k2] if c == 0 else o2[k2]
                    self.tt(dst, p3[:, :, 0:64], T(r.ap[:, c, :].unsqueeze(2).to_broadcast([128, 4, 64]), r.buf), ALU.mult)
                self.unreserve(ib[1][0])
                self.unreserve(ib[0][0])
                self.stt(T(Otok[qg % 2].ap[:, :, h * 64:(h + 1) * 64], Otok[qg % 2].buf), o2[k2], nlam, o1[k2], ALU.mult, ALU.add)
                yield
            for k4 in range(4):
                t = 4 * qg + k4
                self.norm_tr(Otok[qg % 2][:, k4, :], 4, self.ppv("diff_g"), 6, t, tmp[t % 2], post=(1.0 - lam_init))
            yield

    def layer_norm(self, xt, grow, brow, st, gb_eng="dve", presum=False):
        junk = self.junk
        self.act(junk, xt, AF.Copy, accum=st[:, 0:1])
        self.act(junk, xt, AF.Square, accum=st[:, 1:2])
        self.ts(st[:, 2:3], st[:, 0:1], 1.0 / D, ALU.mult)
        self.tt(st[:, 3:4], st[:, 2:3], st[:, 2:3], ALU.mult)
        self.stt(st[:, 4:5], st[:, 1:2], 1.0 / D, st[:, 3:4], ALU.mult, ALU.subtract)
        self.act(st[:, 5:6], st[:, 4:5], AF.Ln, bias=self.lnepsr)
        self.act(st[:, 6:7], st[:, 5:6], AF.Exp, scale=-0.5)
        self.stt(st[:, 7:8], st[:, 2:3], -1.0, st[:, 6:7], ALU.mult, ALU.mult)
        self.act(xt, xt, AF.Identity, bias=st[:, 7:8], scale=st[:, 6:7])
        self.tt(xt, xt, grow, ALU.mult, eng=gb_eng)
        self.tt(xt, xt, brow, ALU.add, eng=gb_eng)

    def out_proj_ln1_route(self, s, l):
        A = self.A
        Wo = self.Wb[1]
        self.load_w(Wo, self.wo_d[l], 0, 1024)
        m = A.mark()
        RT = A.alloc([8, 36], BF16, "RT")
        self.dma(RT, self.rt_d[l].rearrange("(c p) n -> p c n", p=128), q="pool")
        xo = [A.alloc([1024], F32, f"xo{i}") for i in range(2)]
        xt_ = [A.alloc([1024], F32, f"xt{i}") for i in range(2)]
        xT = [A.alloc([8, 128], BF16, f"x1T{i}") for i in range(2)]
        xtb = [A.alloc([1024], BF16, f"xtb{i}") for i in range(2)]
        st = [A.alloc([16], F32, f"lnst{i}") for i in range(2)]
        LA = A.alloc([NT, 36], F32, "LA")
        gmx = A.alloc([NT], F32, "gmx")
        ohg = A.alloc([NT, 4], F32, "ohg")
        eg = A.alloc([NT, 4], F32, "eg")
        pg_ = A.alloc([NT], F32, "pg_")
        sel = A.alloc([NT, 4, 8], F32, "sel")
        el = A.alloc([NT, 8], F32, "el")
        el2 = A.alloc([NT, 8], F32, "el2")
        k1 = A.alloc([NT, 8], F32, "k1")
        k2 = A.alloc([NT, 8], F32, "k2")
        m1 = A.alloc([NT], F32, "m1")
        m2 = A.alloc([NT], F32, "m2")
        self.junk = A.alloc([1024], F32, "junk")
        LNR = A.alloc([2048], F32, "LNR")
        o_ = PP_OFF["ln1_g"][0]
        self.dma(LNR, self.pp_d[l * 128:(l + 1) * 128, o_:o_ + 2048])
        src = self.x_d if l == 0 else self.y_d
        ident = self.co("ident")
        for t in range(NT):
            gt = s * NT + t
            r0 = s * SEQ + t * 128
            i2 = t % 2
            self.dma(xo[i2], src[r0:r0 + 128, :])
            self.memset(st[i2][:, 8:10], 0.0, eng="dve")
            xt = xt_[i2]
            for half in range(2):
                ps = self.ps()
                for c in range(8):
                    self.mm(ps, self.YT[:, c, t * 128:(t + 1) * 128], Wo[:, c, half * 512:(half + 1) * 512], start=(c == 0), stop=(c == 7))
                self.stt(xt[:, half * 512:(half + 1) * 512], xo[i2][:, half * 512:(half + 1) * 512], float(ALPHA), ps, ALU.mult, ALU.add,
                         accum=st[i2][:, 8 + half:9 + half])
            self.tt(st[i2][:, 0:1], st[i2][:, 8:9], st[i2][:, 9:10], ALU.add)
            self.layer_norm(xt, LNR[:, 0:1024], LNR[:, 1024:2048], st[i2], presum=True)
            self.dma(self.X1[r0:r0 + 128, :], xt)
            self.copy(xtb[i2], xt, eng="act")
            for hh in range(2):
                ps = self.ps()
                for k in range(4):
                    c = hh * 4 + k
                    self.mm(ps[:, k * 128:(k + 1) * 128], xtb[i2][:, c * 128:(c + 1) * 128], self.identb)
                self.copy(T(xT[i2].ap[:, hh * 4:hh * 4 + 4, :], xT[i2].buf), T(ps.ap.rearrange("p (k j) -> p k j", j=128), ps.buf), eng="act")
            ps = self.ps()
            for c in range(8):
                self.mm(ps[:, 0:36], xT[i2][:, c, :], RT[:, c, :], start=(c == 0), stop=(c == 7))
            self.copy(LA[:, t, :], ps[:, 0:36], eng="act")
        def bc(x, shape, axis):
            return T(x.ap.unsqueeze(axis).to_broadcast(shape), x.buf)
        g0 = s * NT
        Lg = LA[:, :, 0:4]
        Le = T(LA.ap[:, :, 4:36].rearrange("p t (g j) -> p t g j", j=8), LA.buf)
        self.reduce(gmx, Lg, op=ALU.max)
        self.tt(ohg, Lg, bc(gmx, [128, NT, 4], 2), ALU.is_equal)
        self.tt(eg, Lg, bc(gmx, [128, NT, 4], 2), ALU.subtract)
        self.act(eg, eg, AF.Exp)
        self.reduce(pg_, eg)
        self.recip(pg_, pg_)
        self.tt(sel, Le, bc(ohg, [128, NT, 4, 8], 3), ALU.mult)
        self.reduce(el, T(sel.ap.rearrange("p t g j -> p t j g"), sel.buf))
        self.reduce(m1, el, op=ALU.max)
        self.tt(k1, el, bc(m1, [128, NT, 8], 2), ALU.is_equal)
        self.stt(el2, k1, -1e30, el, ALU.mult, ALU.add)
        self.reduce(m2, el2, op=ALU.max)
        self.tt(k2, el2, bc(m2, [128, NT, 8], 2), ALU.is_equal)
        self.tt(m2, m2, m1, ALU.subtract)
        self.act(m2, m2, AF.Exp)
        self.ts(m2, m2, 1.0, ALU.add)
        self.recip(m2, m2)
        self.tt(self.GT[:, g0:g0 + NT, 0], m2, pg_, ALU.mult)
        self.tt(self.GT[:, g0:g0 + NT, 1], pg_, self.GT[:, g0:g0 + NT, 0], ALU.subtract)
        for (OH, kk) in ((self.OH1, k1), (self.OH2, k2)):
            dst = T(OH.ap[:, g0:g0 + NT, :].rearrange("p t (g j) -> p t g j", j=8), OH.buf)
            self.tt(dst, bc(kk, [128, NT, 4, 8], 2), bc(ohg, [128, NT, 4, 8], 3), ALU.mult)
        self.S.barrier()
        A.release(m)

    def moe_sparse(self, l):
        A = self.A
        m = A.mark()
        NTT = self.nseq * NT
        NB = self.NB
        ones = self.co("ones")
        RK = A.alloc([NTT, 32], F32, "RK")
        At = [A.alloc([32], F32, f"At{i}") for i in range(2)]
        Acum = A.alloc([32], F32, "Acum")
        cnt = A.alloc([32], F32, "cnt")
        self.memset(Acum, 0.0)
        for gt in range(NTT):
            a = At[gt % 2]
            self.tt(a, self.OH1[:, gt, :], self.OH2[:, gt, :], ALU.add)
            ps = self.ps()
            self.mm(ps[:, 0:32], ones, Acum, start=True, stop=False)
            self.mm(ps[:, 0:32], self.co("triUs"), a, start=False, stop=True)
            self.copy(RK[:, gt, :], ps[:, 0:32], eng="act")
            self.tt(Acum, Acum, a, ALU.add)
        ps = self.ps()
        self.mm(ps[:, 0:32], ones, Acum)
        self.copy(cnt, ps[:, 0:32], eng="act")
        cmp = A.alloc([32, 16], F32, "cmp")
        pad = A.alloc([32], F32, "pad")
        sc = [A.alloc([32], F32, f"sc{i}") for i in range(2)]
        pstart = A.alloc([32], F32, "pstart")
        thr = self.co("thr16")
        self.tt(cmp, T(cnt.ap.unsqueeze(2).to_broadcast([128, 32, 16]), cnt.buf),
                T(thr.ap.unsqueeze(1).to_broadcast([128, 32, 16]), self.CO.buf), ALU.is_gt)
        self.reduce(pad, cmp)
        self.ts(pad, pad, 512.0, ALU.mult)
        cur = pad
        k = 0
        for sh in (1, 2, 4, 8, 16):
            nx = sc[k % 2]
            k += 1
            self.copy(nx[:, 0:sh], cur[:, 0:sh])
            self.tt(nx[:, sh:32], cur[:, sh:32], cur[:, 0:32 - sh], ALU.add)
            cur = nx
        pend = cur
        self.tt(pstart, pend, pad, ALU.subtract)
        big = A.alloc([NTT, 32], F32, "big")
        dstf = A.alloc([NTT, 2], F32, "dstf")
        DST = A.alloc([NTT, 2], I32, "DST")
        self.tt(RK, RK, T(pstart.ap.unsqueeze(1).to_broadcast([128, NTT, 32]), pstart.buf), ALU.add)
        self.tt(big, RK, self.OH1, ALU.mult)
        self.reduce(dstf[:, :, 0], big)
        self.tt(big, RK, self.OH2, ALU.mult)
        self.reduce(dstf[:, :, 1], big)
        self.copy(DST, dstf)
        cb = A.alloc([NB, 32], F32, "cb")
        be = A.alloc([NB], F32, "be")
        inv = A.alloc([NB], F32, "inv")
        ixf = A.alloc([NB, 8], F32, "ixf")
        IXW = A.alloc([NB, 8], I32, "IXW")
        IXD = A.alloc([NB, 4], I32, "IXD")
        bthr = self.co("bthr")[:, 0:NB]
        self.tt(cb, T(pend.ap.unsqueeze(1).to_broadcast([128, NB, 32]), pend.buf),
                T(bthr.ap.unsqueeze(2).to_broadcast([128, NB, 32]), self.CO.buf), ALU.is_le)
        self.reduce(be, cb)
        self.ts(inv, bthr, pend[:, 31:32], ALU.is_ge, 4.0e6, ALU.mult)
        pc = self.co("pc")
        self.stt(be, be, 1024.0, inv, ALU.mult, ALU.add)
        self.ts(be, be, float(l * N_EXP * 1024), ALU.add)
        self.tt(ixf, T(be.ap.unsqueeze(2).to_broadcast([128, NB, 8]), be.buf),
                T(pc.ap.unsqueeze(1).to_broadcast([128, NB, 8]), self.CO.buf), ALU.add)
        self.copy(IXW, ixf)
        self.stt(be, be, 0.5, inv, ALU.mult, ALU.add)
        self.tt(ixf[:, :, 0:4], T(be.ap.unsqueeze(2).to_broadcast([128, NB, 4]), be.buf),
                T(pc.ap[:, 0:4].unsqueeze(1).to_broadcast([128, NB, 4]), self.CO.buf), ALU.add)
        self.copy(IXD, ixf[:, :, 0:4])
        xl = [A.alloc([1024], F32, f"xl{i}") for i in range(3)]
        nslot = NB * 512
        for gt in range(NTT):
            x_ = xl[gt % 3]
            self.dma(x_, self.X1[gt * 128:(gt + 1) * 128, :])
            for k2 in range(2):
                self.idma(self.XS, DST[:, gt, k2:k2 + 1], x_, scatter=True, bound=nslot - 1)
        Wg = [[A.alloc([512], BF16, f"Wg{i}_{c}") for c in range(8)] for i in range(2)]
        Wu = [[A.alloc([512], BF16, f"Wu{i}_{c}") for c in range(8)] for i in range(2)]
        Wd = [[A.alloc([1024], BF16, f"Wd{i}_{c}") for c in range(4)] for i in range(2)]
        xr = [A.alloc([4, 1024], BF16, f"xr{i}") for i in range(2)]
        xsT = [A.alloc([8, 512], BF16, f"xsT{i}") for i in range(2)]
        hT = [A.alloc([4, 512], BF16, f"hT{i}") for i in range(2)]
        sil = [A.alloc([512], F32, f"sil{i}") for i in range(2)]
        ysb = [A.alloc([1024], F32, f"ysb{i}") for i in range(3)]
        wg2 = T(self.wg_d.rearrange("l e d f -> (l e d) f"), Buf("wg", dram=True))
        wu2 = T(self.wu_d.rearrange("l e d f -> (l e d) f"), Buf("wu", dram=True))
        wd2 = T(self.wd_d.rearrange("l e f d -> (l e f) d"), Buf("wd", dram=True))
        yc = 0
        for b in range(NB):
            i2 = b % 2
            for c in range(8):
                self.idma(Wg[i2][c], IXW[:, b, c:c + 1], wg2, scatter=False, bound=2 * N_EXP * 1024 - 1)
                self.idma(Wu[i2][c], IXW[:, b, c:c + 1], wu2, scatter=False, bound=2 * N_EXP * 1024 - 1)
            for c in range(4):
                self.idma(Wd[i2][c], IXD[:, b, c:c + 1], wd2, scatter=False, bound=2 * N_EXP * 512 - 1)
            self.dma(xr[i2], T(self.XS.ap[b * 512:(b + 1) * 512, :].rearrange("(s p) d -> p s d", p=128), self.XS.buf))
            for c2 in range(4):
                ps = self.ps()
                psb = T(ps.ap.bitcast(BF16), ps.buf)
                for j in range(2):
                    c = 2 * c2 + j
                    for s4 in range(4):
                        self.tr(psb[:, j * 512 + s4 * 128:j * 512 + (s4 + 1) * 128], xr[i2][:, s4, c * 128:(c + 1) * 128])
                self.copy(T(xsT[i2].ap[:, 2 * c2:2 * c2 + 2, :], xsT[i2].buf),
                          T(psb.ap.rearrange("p (j n) -> p j n", n=512), ps.buf), eng=("act" if c2 % 2 == 0 else "dve"))
            for fc in range(4):
                pg, pu = self.ps(), self.ps()
                for c in range(8):
                    self.mm(pg, Wg[i2][c][:, fc * 128:(fc + 1) * 128], xsT[i2][:, c, :], start=(c == 0), stop=(c == 7))
                for c in range(8):
                    self.mm(pu, Wu[i2][c][:, fc * 128:(fc + 1) * 128], xsT[i2][:, c, :], start=(c == 0), stop=(c == 7))
                sl_ = sil[fc % 2]
                self.act(sl_, pg, AF.Silu)
                self.tt(hT[i2][:, fc, :], sl_, pu, ALU.mult)
            for s4 in range(4):
                y_ = ysb[yc % 3]
                yc += 1
                for half in range(2):
                    ps = self.ps()
                    for fc in range(4):
                        self.mm(ps, hT[i2][:, fc, s4 * 128:(s4 + 1) * 128], Wd[i2][fc][:, half * 512:(half + 1) * 512], start=(fc == 0), stop=(fc == 3))
                    self.copy(y_[:, half * 512:(half + 1) * 512], ps, eng=("act" if half == 0 else "dve"))
                r0 = b * 512 + s4 * 128
                self.dma(self.YS[r0:r0 + 128, :], y_)
        ya = [A.alloc([1024], F32, f"ya{i}") for i in range(2)]
        yb = [A.alloc([1024], F32, f"yb{i}") for i in range(2)]
        st = [A.alloc([16], F32, f"lnst{i}") for i in range(2)]
        self.junk = A.alloc([1024], F32, "junk")
        LNR = A.alloc([2048], F32, "LNR")
        o_ = PP_OFF["ln2_g"][0]
        self.dma(LNR, self.pp_d[l * 128:(l + 1) * 128, o_:o_ + 2048])

        def fetch(gt):
            self.dma(xl[gt % 3], self.X1[gt * 128:(gt + 1) * 128, :])
            self.idma(ya[gt % 2], DST[:, gt, 0:1], self.YS, scatter=False, bound=nslot - 1)
            self.idma(yb[gt % 2], DST[:, gt, 1:2], self.YS, scatter=False, bound=nslot - 1)
        fetch(0)
        for gt in range(NTT):
            i2 = gt % 2
            x_ = xl[gt % 3]
            self.ts(x_, x_, float(ALPHA), ALU.mult)
            self.stt(x_, ya[i2], self.GT[:, gt, 0:1], x_, ALU.mult, ALU.add)
            self.memset(st[i2][:, 0:1], 0.0, eng="dve")
            self.stt(x_, yb[i2], self.GT[:, gt, 1:2], x_, ALU.mult, ALU.add, accum=st[i2][:, 0:1])
            if gt + 1 < NTT:
                fetch(gt + 1)
            self.layer_norm(x_, LNR[:, 0:1024], LNR[:, 1024:2048], st[i2], gb_eng="dve", presum=True)
            self.dma(self.y_d[gt * 128:(gt + 1) * 128, :], x_)
        self.S.barrier()
        A.release(m)

    def idma(self, dst, idx, src, scatter, bound):
        a = self._a
        self.bounds.add(bound)
        regs = self.bregs
        bound_key = bound
        bound = None
        if scatter:
            fn = lambda e: e.indirect_dma_start(out=a(dst), out_offset=bass.IndirectOffsetOnAxis(ap=a(idx), axis=0),
                                                in_=a(src), in_offset=None, bounds_check=regs[bound_key], oob_is_err=False)
        else:
            fn = lambda e: e.indirect_dma_start(out=a(dst), out_offset=None, in_=a(src),
                                                in_offset=bass.IndirectOffsetOnAxis(ap=a(idx), axis=0),
                                                bounds_check=regs[bound_key], oob_is_err=False)
        nb = min(self._fs(dst), self._fs(src)) * 128.0 * 4.0
        self.S.op("pool", fn, reads=self._bufs(src, idx), writes=self._bufs(dst), dma=True, cost=900.0, lat=nb / 300.0)

    def next_w(self):
        self.wi += 1
        return self.Wb[self.wi % 2]

    def build(self):
        A = self.A
        nc = self.nc
        ntok = self.nseq * SEQ
        NTT = self.nseq * NT
        self.NB = (2 * ntok + N_EXP * 511) // 512 + 1
        assert self.NB <= 48
        self.X1 = T(nc.dram_tensor("x1_scr", [ntok, D], F32).ap(), Buf("X1", dram=True))
        self.XS = T(nc.dram_tensor("xs_scr", [self.NB * 512, D], BF16).ap(), Buf("XS", dram=True))
        self.YS = T(nc.dram_tensor("ys_scr", [self.NB * 512, D], F32).ap(), Buf("YS", dram=True))
        self.CO = A.alloc([NCO], F32, "CO")
        self.PP = A.alloc([NPPS], F32, "PP")
        self.identb = A.alloc([128], BF16, "identb")
        self.triUb = A.alloc([128], BF16, "triUb")
        cst = A.alloc([4], F32, "cst")
        self.OH1 = A.alloc([NTT, 32], BF16, "OH1")
        self.OH2 = A.alloc([NTT, 32], BF16, "OH2")
        self.GT = A.alloc([NTT, 2], F32, "GT")
        self.xtmark = A.mark()
        self.XT = A.alloc([8, 2048], BF16, "XT")
        self.ytmark = A.mark()
        self.YT = A.alloc([8, 2048], BF16, "YT")
        self.dma(self.CO, self.co_d)
        self.copy(self.identb, self.co("ident"))
        self.copy(self.triUb, self.co("triU"))
        self.memset(cst[:, 0:1], RMS_EPS)
        self.memset(cst[:, 1:2], 1.0)
        self.memset(cst[:, 2:3], LN_EPS)
        self.epsr, self.oner, self.lnepsr = cst[:, 0:1], cst[:, 1:2], cst[:, 2:3]
        self.Wb = [A.alloc([8, 784], BF16, "Wb0"), A.alloc([8, 1040], BF16, "Wb1")]
        self.wi = 0
        base = A.mark()
        self.xbase = base
        for l in range(self.n_layers):
            self.dma(self.PP, self.pp_d[l * 128:(l + 1) * 128, 0:NPPS])
            src = self.x_d if l == 0 else self.y_d
            for s in range(self.nseq):
                A.release(base)
                self.make_XT(src, s)
                self.run_gens([(self.conv_mixer(l), 1, [0, 1, 2, 3]), (self.gla_mixer(l), 3, [4, 5, 6, 7])])
                self.S.barrier()
                A.release(base)
                gens = [(self.ssd_mixer(l), 1, [0, 1, 2, 3]), (self.diff_mixer(l), 2, [4, 5, 6, 7])]
                self.run_gens(gens)
                self.S.barrier()
                A.release(base)
                self.out_proj_ln1_route(s, l)
            A.release(self.xtmark)
            self.moe_sparse(l)
            A.release(base)


_PROG_CACHE = {}


def get_prog(nseq, n_layers, with_moe, stages="cgsd", dbg=False):
    key = (nseq, n_layers, with_moe, stages, dbg)
    if key not in _PROG_CACHE:
        _PROG_CACHE[key] = Prog(*key)
    return _PROG_CACHE[key]


def kernel(**inp):
    inp = {k: np.asarray(v) for k, v in inp.items()}
    n_cores = 8
    nseq = 2
    prog = get_prog(nseq, 2, True)
    x = np.ascontiguousarray(inp["x"], dtype=np.float32).reshape(16 * SEQ, D)
    pp = np.concatenate([host_pp(inp, l) for l in range(2)], axis=0)
    co = host_consts()
    rt = np.ascontiguousarray(np.concatenate([inp["router_g"], inp["router_e"].reshape(2, D, 32)], axis=2), dtype=np.float32)
    shared = {"w_in": inp["w_in"], "w_o": inp["w_o"], "pp": pp, "co": co, "rt": rt,
              "w_gate": inp["w_gate"], "w_up": inp["w_up"], "w_down": inp["w_down"]}
    in_maps = []
    for c in range(n_cores):
        mcore = dict(shared)
        mcore["x"] = x[c * nseq * SEQ:(c + 1) * nseq * SEQ]
        in_maps.append(mcore)
    res = run_bass_kernel_spmd(prog.nc, in_maps, core_ids=list(range(n_cores)))
    y = np.concatenate([res.results[c]["y"] for c in range(n_cores)], axis=0)
    return y.reshape(16, SEQ, D).astype(np.float32)
```

```python
import numpy as np
import ml_dtypes
import concourse.bass as bass
import concourse.mybir as mybir
from concourse.bass_utils import run_bass_kernel_spmd

F32 = mybir.dt.float32
BF16 = mybir.dt.bfloat16
I32 = mybir.dt.int32
U8 = mybir.dt.uint8
ALU = mybir.AluOpType
AF = mybir.ActivationFunctionType
AX = mybir.AxisListType
DTSIZE = {F32: 4, BF16: 2, I32: 4, U8: 1}


class Buf:
    __slots__ = ("name", "wr", "rd", "psum", "dram")

    def __init__(self, name="", dram=False):
        self.name = name
        self.wr = []
        self.rd = []
        self.psum = False
        self.dram = dram


class T:
    __slots__ = ("ap", "buf")

    def __init__(self, ap, buf):
        self.ap = ap
        self.buf = buf

    def __getitem__(self, k):
        return T(self.ap[k], self.buf)


class Op:
    __slots__ = ("eng", "fn", "deps", "sig", "isdma", "sem", "val", "prev_val", "cost", "odeps", "idx", "lat")

    def __init__(self, eng, fn, isdma):
        self.eng = eng
        self.fn = fn
        self.isdma = isdma
        self.cost = 300.0
        self.lat = 0.0
        self.odeps = []
        self.idx = 0
        self.deps = []
        self.sig = False
        self.sem = None
        self.val = 0
        self.prev_val = 0


ENGS = ("pe", "act", "dve", "pool", "sp")
N_DMA_SEMS = 32
EPOCH = 30000


class Sched:
    def __init__(self):
        self.streams = {e: [] for e in ENGS}
        self.all_ops = []
        self.dma_count = 0
        self.pending_dma = []

    @staticmethod
    def _acc(x):
        if not isinstance(x, T):
            return x, None
        if x.buf.psum:
            return x.buf, None
        try:
            ap = x.ap
            pairs = [(int(p[0]), int(p[1])) for p in ap.ap]
            off = int(ap.offset)
            esz = DTSIZE.get(ap.dtype, 4)
            if x.buf.dram:
                dims = pairs
                base = off
            else:
                pstep = pairs[0][0]
                dims = pairs[1:]
                base = off % pstep if pstep > 0 else off
            dims = [(s_, c_) for (s_, c_) in dims if c_ > 1 and s_ != 0]
            if not dims:
                return x.buf, [(base * esz, (base + 1) * esz)]
            dims.sort(key=lambda d: -abs(d[0]))
            run = 1
            if dims[-1][0] == 1:
                run = dims[-1][1]
                dims = dims[:-1]
                while dims and dims[-1][0] == run:
                    run *= dims[-1][1]
                    dims = dims[:-1]
            n = 1
            for _, c_ in dims:
                n *= c_
            if n > 96:
                hi = base + sum(abs(s_) * (c_ - 1) for s_, c_ in dims) + run
                return x.buf, [(base * esz, hi * esz)]
            starts = [base]
            for s_, c_ in dims:
                starts = [st + s_ * k for st in starts for k in range(c_)]
            ivs = sorted((st * esz, (st + run) * esz) for st in starts)
            return x.buf, ivs
        except Exception:
            return x.buf, None

    @staticmethod
    def _ovl(a, b):
        if a is None or b is None:
            return True
        i = j = 0
        while i < len(a) and j < len(b):
            if a[i][1] <= b[j][0]:
                i += 1
            elif b[j][1] <= a[i][0]:
                j += 1
            else:
                return True
        return False

    @staticmethod
    def _covers(a, b):
        if a is None:
            return True
        if b is None:
            return False
        i = 0
        for lo, hi in b:
            while i < len(a) and a[i][1] <= lo:
                i += 1
            if i >= len(a) or a[i][0] > lo or a[i][1] < hi:
                return False
        return True

    def op(self, eng, fn, reads=(), writes=(), dma=False, cost=300.0, lat=0.0):
        o = Op(eng, fn, dma)
        o.cost = cost
        o.lat = lat
        o.idx = len(self.all_ops)
        deps = set()
        racc = [self._acc(x) for x in reads]
        wacc = [self._acc(x) for x in writes]
        for b, iv in racc:
            for w, wiv in b.wr:
                if self._ovl(iv, wiv):
                    deps.add(w)
            if b.psum:
                for r, riv in b.rd:
                    if r.eng != eng:
                        deps.add(r)
        for b, iv in wacc:
            for r, riv in b.rd:
                if r is not o and self._ovl(iv, riv):
                    deps.add(r)
            for w, wiv in b.wr:
                if not self._ovl(iv, wiv):
                    continue
                if w.isdma and dma:
                    continue
                if w.isdma or dma or w.eng != eng:
                    deps.add(w)
                else:
                    o.odeps.append(w)
        for b, iv in wacc:
            b.rd = [(r, riv) for (r, riv) in b.rd if not self._covers(iv, riv)]
            b.wr = [(w, wiv) for (w, wiv) in b.wr if (w.isdma and dma) or not self._covers(iv, wiv)]
            if len(b.wr) > 200:
                for w, _ in b.wr:
                    if not (w.isdma and dma):
                        deps.add(w)
                b.wr = [(w, wiv) for (w, wiv) in b.wr if (w.isdma and dma)][-200:]
                iv = None
            b.wr.append((o, iv))
        for b, iv in racc:
            if len(b.rd) > 200:
                for r, _ in b.rd:
                    deps.add(r)
                b.rd = []
                iv = None
            b.rd.append((o, iv))
        for d in deps:
            if d is o:
                continue
            if d.eng == "pe" and eng == "pe" and not d.isdma and not dma:
                o.odeps.append(d)
                continue
            d.sig = True
            o.deps.append(d)
        self.streams[eng].append(o)
        self.all_ops.append(o)
        if dma:
            self.pending_dma.append(o)
        return o

    def barrier(self):
        lasts = []
        for e in ENGS:
            for o in reversed(self.streams[e]):
                if not o.isdma and o.fn is not None:
                    lasts.append(o)
                    break
        pend = list(self.pending_dma)
        self.pending_dma = []
        for e in ENGS:
            o = Op(e, None, False)
            o.idx = len(self.all_ops)
            for d in lasts + pend:
                d.sig = True
                o.deps.append(d)
            self.streams[e].append(o)
            self.all_ops.append(o)

    def reschedule(self):
        import heapq
        new_streams = {e: [] for e in ENGS}
        pos = {e: 0 for e in ENGS}
        done_ids = set()
        while True:
            seg = {}
            more = False
            for e in ENGS:
                st = self.streams[e]
                i = pos[e]
                j = i
                while j < len(st) and st[j].fn is not None:
                    j += 1
                seg[e] = st[i:j]
                if j < len(st):
                    more = True
            ops = [o for e in ENGS for o in seg[e]]
            inseg = set(id(o) for o in ops)
            succ = {id(o): [] for o in ops}
            nun = {}
            for o in ops:
                n = 0
                for d in list(o.deps) + list(o.odeps):
                    if id(d) in inseg:
                        succ[id(d)].append(o)
                        n += 1
                nun[id(o)] = n
            cp = {}
            for o in sorted(ops, key=lambda q: -q.idx):
                m_ = 0.0
                for s_ in succ[id(o)]:
                    v = cp[id(s_)]
                    if v > m_:
                        m_ = v
                cp[id(o)] = o.cost + o.lat + m_
            ready = {e: [] for e in ENGS}
            for o in ops:
                if nun[id(o)] == 0:
                    heapq.heappush(ready[o.eng], (-cp[id(o)] if CP_PRIORITY else o.idx, o.idx, o))
            free = {e: 0.0 for e in ENGS}
            comp = []
            now = 0.0
            nsched = 0
            fabric = 0.0
            while True:
                for e in ENGS:
                    if ready[e] and free[e] <= now:
                        _, _, o = heapq.heappop(ready[e])
                        new_streams[e].append(o)
                        nsched += 1
                        free[e] = now + o.cost
                        if o.isdma:
                            t0_ = max(now + o.cost, fabric)
                            fabric = t0_ + o.lat
                            heapq.heappush(comp, (fabric + 2000.0, o.idx, o))
                        else:
                            heapq.heappush(comp, (now + o.cost + o.lat, o.idx, o))
                nxt = [free[e] for e in ENGS if ready[e] and free[e] > now]
                if not comp and not nxt:
                    break
                tnext = min([comp[0][0]] if comp else []) if comp else None
                cand = nxt + ([comp[0][0]] if comp else [])
                now = min(cand)
                while comp and comp[0][0] <= now:
                    _, _, o = heapq.heappop(comp)
                    for s_ in succ[id(o)]:
                        nun[id(s_)] -= 1
                        if nun[id(s_)] == 0:
                            heapq.heappush(ready[s_.eng], (-cp[id(s_)] if CP_PRIORITY else s_.idx, s_.idx, s_))
            assert nsched == len(ops), (nsched, len(ops))
            for e in ENGS:
                pos[e] += len(seg[e])
                st = self.streams[e]
                if pos[e] < len(st):
                    new_streams[e].append(st[pos[e]])
                    pos[e] += 1
            if not more:
                break
        for e in ENGS:
            assert len(new_streams[e]) == len(self.streams[e]), (e, len(new_streams[e]), len(self.streams[e]))
        self.streams = new_streams

    def emit(self, nc):
        import contextlib
        stack = contextlib.ExitStack()
        with stack:
            counts = {e: 0 for e in ENGS}
            n_epochs = {e: 1 for e in ENGS}
            for o in self.all_ops:
                if o.isdma or not o.sig:
                    continue
                counts[o.eng] += 1
            for e in ENGS:
                n_epochs[e] = max(1, (counts[e] + EPOCH - 1) // EPOCH)
            esems = {e: [stack.enter_context(nc.semaphore(f"s_{e}{i}")) for i in range(n_epochs[e])] for e in ENGS}
            dsems = {e: [stack.enter_context(nc.semaphore(f"s_dma_{e}{i}")) for i in range(N_DMA_SEMS)] for e in ("sp", "pool", "act")}
            for e in ENGS:
                k = 0
                dcnt = 0
                for o in self.streams[e]:
                    if o.isdma:
                        o.sem = dsems[e][dcnt % N_DMA_SEMS]
                        o.prev_val = 16 * (dcnt // N_DMA_SEMS)
                        o.val = o.prev_val + 16
                        dcnt += 1
                    elif o.sig:
                        o.sem = esems[e][k // EPOCH]
                        o.val = (k % EPOCH) + 1
                        k += 1
            block = stack.enter_context(nc.Block())

            def run_stream(ename, eng):
                waited = {}
                pro = getattr(self, "prologue", {}).get(ename)
                if pro is not None:
                    pro(eng)
                for o in self.streams[ename]:
                    for d in o.deps:
                        key = id(d.sem)
                        if waited.get(key, 0) >= d.val:
                            continue
                        eng.wait_ge(d.sem, d.val)
                        waited[key] = d.val
                    if o.isdma and o.prev_val > 0:
                        key = id(o.sem)
                        if waited.get(key, 0) < o.prev_val:
                            eng.wait_ge(o.sem, o.prev_val)
                            waited[key] = o.prev_val
                    if o.fn is None:
                        continue
                    ins = o.fn(eng)
                    if o.isdma:
                        ins.then_inc(o.sem, 16)
                    elif o.sig:
                        ins.then_inc(o.sem, 1)

            @block.tensor
            def _(eng):
                run_stream("pe", eng)

            @block.scalar
            def _(eng):
                run_stream("act", eng)

            @block.vector
            def _(eng):
                run_stream("dve", eng)

            @block.gpsimd
            def _(eng):
                run_stream("pool", eng)

            @block.sync
            def _(eng):
                run_stream("sp", eng)


class Arena:
    def __init__(self, nc, nbytes):
        self.h = nc.alloc_sbuf_tensor("arena", [128, nbytes], U8)
        self.nbytes = nbytes
        self.off = 0

    def alloc(self, shape, dtype, name="", nparts=128):
        n = int(np.prod(shape)) * DTSIZE[dtype]
        off = (self.off + 31) // 32 * 32
        assert off + n <= self.nbytes, (name, off, n, self.nbytes)
        self.off = off + n
        ap = self.h[0:nparts, off:off + n].bitcast(dtype)
        if len(shape) == 2:
            ap = ap.rearrange("p (a b) -> p a b", b=shape[1])
        elif len(shape) == 3:
            ap = ap.rearrange("p (a b c) -> p a b c", b=shape[1], c=shape[2])
        return T(ap, Buf(name))

    def mark(self):
        return self.off

    def release(self, m):
        self.off = m


D = 1024
SEQ = 2048
NT = 16
NTG = 4
P_IN = 3348
ALPHA = 4 ** 0.25
LN_EPS = 1e-5
RMS_EPS = 1e-6
N_EXP = 32
RESCHEDULE = True
CP_PRIORITY = True

C_CONV = 0
C_GLA = 768
C_SSD = 1552
C_DIFF = 2580

PP_OFF = {}
_o = 0
for _n, _w in (("conv_w", 6), ("gla_b", 128), ("gla_g", 256), ("gla_wlr", 128), ("ssd_cw", 24), ("ssd_cb", 6),
               ("ssd_alog", 4), ("ssd_d", 256), ("ssd_dtb", 4), ("ssd_g", 256), ("diff_l", 128), ("diff_g", 256),
               ("ln1_g", 1024), ("ln1_b", 1024), ("ln2_g", 1024), ("ln2_b", 1024)):
    PP_OFF[_n] = (_o, _w)
    _o += _w
NPP = _o
NPPS = PP_OFF["ln1_g"][0]

CO_OFF = {}
_o = 0
for _n, _w in (("ident", 128), ("triU", 128), ("triUn16", 128), ("trisL", 128), ("ones", 128), ("hm", 4), ("triUs", 128), ("thr16", 16), ("bthr", 48), ("pc", 8)):
    CO_OFF[_n] = (_o, _w)
    _o += _w
NCO = _o


def host_consts():
    c = np.zeros((128, NCO), np.float32)
    p = np.arange(128)[:, None]
    j = np.arange(128)[None, :]
    c[:, CO_OFF["ident"][0]:CO_OFF["ident"][0] + 128] = (p == j)
    c[:, CO_OFF["triU"][0]:CO_OFF["triU"][0] + 128] = (p <= j)
    c[:, CO_OFF["triUn16"][0]:CO_OFF["triUn16"][0] + 128] = (p <= j) * np.float32(-1.0 / 16.0)
    c[:, CO_OFF["trisL"][0]:CO_OFF["trisL"][0] + 128] = (p > j)
    c[:, CO_OFF["ones"][0]:CO_OFF["ones"][0] + 128] = 1.0
    c[:, CO_OFF["hm"][0]:CO_OFF["hm"][0] + 4] = ((p // 32) == np.arange(4)[None, :])
    c[:, CO_OFF["triUs"][0]:CO_OFF["triUs"][0] + 128] = (p < j)
    c[:, CO_OFF["thr16"][0]:CO_OFF["thr16"][0] + 16] = 512.0 * np.arange(16)[None, :]
    c[:, CO_OFF["bthr"][0]:CO_OFF["bthr"][0] + 48] = 512.0 * np.arange(48)[None, :]
    c[:, CO_OFF["pc"][0]:CO_OFF["pc"][0] + 8] = 128.0 * np.arange(8)[None, :] + p
    return c


def host_pp(inp, l):
    pp = np.zeros((128, NPP), np.float32)

    def put(name, arr):
        o, w = PP_OFF[name]
        assert arr.shape == (128, w), (name, arr.shape)
        pp[:, o:o + w] = arr

    def row(v):
        return np.broadcast_to(np.asarray(v, np.float32)[None, :], (128, len(v)))

    cw = inp["conv_w"][l]
    put("conv_w", np.stack([cw[k, fc * 128:(fc + 1) * 128] for fc in range(2) for k in range(3)], axis=1))
    put("gla_b", row(inp["gla_b_lr"][l]))
    put("gla_g", row(np.tile(inp["gla_norm_g"][l], 4)))
    wl = np.zeros((128, 128), np.float32)
    wl[:16] = inp["gla_w_lr"][l]
    put("gla_wlr", wl)
    sw = inp["ssd_conv_w"][l]
    put("ssd_cw", np.stack([sw[k, c6 * 128:(c6 + 1) * 128] for c6 in range(6) for k in range(4)], axis=1))
    sb = inp["ssd_conv_b"][l]
    put("ssd_cb", np.stack([sb[c6 * 128:(c6 + 1) * 128] for c6 in range(6)], axis=1))
    put("ssd_alog", row(inp["ssd_a_log"][l]))
    put("ssd_d", row(np.repeat(inp["ssd_d"][l], 64)))
    put("ssd_dtb", row(inp["ssd_dt_bias"][l]))
    put("ssd_g", row(inp["ssd_norm_g"][l]))
    put("diff_l", row(np.concatenate([inp["diff_lq1"][l], inp["diff_lk1"][l], inp["diff_lq2"][l], inp["diff_lk2"][l]])))
    put("diff_g", row(np.tile(inp["diff_norm_g"][l], 4)))
    for n in ("ln1_g", "ln1_b", "ln2_g", "ln2_b"):
        put(n, row(inp[n][l]))
    return pp


class Prog:
    def __init__(self, nseq, n_layers, with_moe, stages="cgsd", dbg=False):
        self.nseq, self.n_layers, self.with_moe, self.stages, self.dbg = nseq, n_layers, with_moe, stages, dbg
        nc = self.nc = bass.Bass("TRN2", target_bir_lowering=False)
        ntok = nseq * SEQ
        dt = nc.dram_tensor
        self.x_d = dt("x", [ntok, D], F32, kind="ExternalInput").ap()
        self.win_d = dt("w_in", [2, D, P_IN], F32, kind="ExternalInput").ap()
        self.wo_d = dt("w_o", [2, D, D], F32, kind="ExternalInput").ap()
        self.pp_d = dt("pp", [2 * 128, NPP], F32, kind="ExternalInput").ap()
        self.co_d = dt("co", [128, NCO], F32, kind="ExternalInput").ap()
        if with_moe:
            self.rt_d = dt("rt", [2, D, 36], F32, kind="ExternalInput").ap()
            self.wg_d = dt("w_gate", [2, N_EXP, D, 512], F32, kind="ExternalInput").ap()
            self.wu_d = dt("w_up", [2, N_EXP, D, 512], F32, kind="ExternalInput").ap()
            self.wd_d = dt("w_down", [2, N_EXP, 512, D], F32, kind="ExternalInput").ap()
        self.y_d = dt("y", [ntok, D], F32, kind="ExternalOutput").ap()
        if dbg:
            self.dbg_d = dt("dbg", [ntok, D], F32, kind="ExternalOutput").ap()
        self.S = Sched()
        self.A = Arena(nc, 207 * 1024)
        self.banks = []
        for i in range(8):
            b = Buf(f"ps{i}")
            b.psum = True
            self.banks.append(T(nc.alloc_psum_tensor(f"ps{i}", [128, 512], F32)[:, :], b))
        self.pool = {"free": list(range(8)), "rr": 0}
        self.bounds = set()
        self.bregs = {}
        self.build()
        self.S.barrier()

        def pool_prologue(eng):
            for bv in sorted(self.bounds):
                r = nc.alloc_register(mybir.EngineType.Pool, f"bnd{bv}")
                eng.reg_mov(r, int(bv))
                self.bregs[bv] = r
        self.S.prologue = {"pool": pool_prologue}
        if RESCHEDULE:
            self.S.reschedule()
        self.S.emit(nc)

    def ps(self):
        pool = self.pool
        i = pool["free"][pool["rr"] % len(pool["free"])]
        pool["rr"] += 1
        return self.banks[i]

    def reserve(self):
        i = self.pool["free"].pop()
        return i, self.banks[i]

    def unreserve(self, i):
        self.pool["free"].append(i)

    def run_gens(self, specs):
        active = [[g, w, {"free": list(banks), "rr": 0}] for g, w, banks in specs]
        save = self.pool
        while active:
            for item in list(active):
                g, w, pool = item
                self.pool = pool
                for _ in range(w):
                    try:
                        next(g)
                    except StopIteration:
                        active.remove(item)
                        break
        self.pool = save

    @staticmethod
    def _a(x):
        return x.ap if isinstance(x, T) else x

    @staticmethod
    def _bufs(*xs):
        return [x for x in xs if isinstance(x, T)]

    @staticmethod
    def _fs(x):
        x = x.ap if isinstance(x, T) else x
        try:
            return float(x.free_size())
        except Exception:
            return 256.0

    def mm(self, out, lhsT, rhs, start=True, stop=True):
        a = self._a
        n = self._fs(rhs)
        c = max(64.0, n) / 2.4 + 40.0 + self._fs(lhsT) / 2.4 * 0.5
        if a(rhs).dtype == F32:
            c *= 4.0
        self.S.op("pe", lambda e: e.matmul(a(out), a(lhsT), a(rhs), start=start, stop=stop),
                  reads=self._bufs(lhsT, rhs), writes=self._bufs(out), cost=c, lat=0.0)

    def tr(self, out, in_):
        a = self._a
        self.S.op("pe", lambda e: e.transpose(a(out), a(in_), a(self.identb)),
                  reads=self._bufs(in_, self.identb), writes=self._bufs(out), cost=120.0, lat=0.0)

    def act(self, out, in_, func=None, bias=None, scale=None, accum=None, eng="act"):
        a = self._a
        kw = {}
        if bias is not None:
            kw["bias"] = a(bias)
        if scale is not None:
            kw["scale"] = a(scale)
        if accum is not None:
            kw["accum_out"] = a(accum)
        f = func if func is not None else AF.Copy
        self.S.op("act", lambda e: e.activation(a(out), a(in_), f, **kw),
                  reads=self._bufs(in_, bias, scale), writes=self._bufs(out, accum), cost=200.0 + 0.85 * self._fs(out), lat=0.0)

    def tt(self, out, in0, in1, op, eng="dve"):
        a = self._a
        self.S.op(eng, lambda e: e.tensor_tensor(a(out), a(in0), a(in1), op),
                  reads=self._bufs(in0, in1), writes=self._bufs(out), cost=self._vc(eng, out), lat=0.0)

    def ts(self, out, in0, s1, op0, s2=None, op1=None, eng="dve"):
        a = self._a
        if op1 is None:
            self.S.op(eng, lambda e: e.tensor_scalar(a(out), a(in0), a(s1), None, op0),
                      reads=self._bufs(in0, s1), writes=self._bufs(out), cost=self._vc(eng, out), lat=0.0)
        else:
            self.S.op(eng, lambda e: e.tensor_scalar(a(out), a(in0), a(s1), a(s2), op0, op1),
                      reads=self._bufs(in0, s1, s2), writes=self._bufs(out), cost=self._vc(eng, out), lat=0.0)

    def stt(self, out, in0, scalar, in1, op0, op1, accum=None):
        a = self._a
        if accum is None:
            self.S.op("dve", lambda e: e.scalar_tensor_tensor(a(out), a(in0), a(scalar), a(in1), op0, op1),
                      reads=self._bufs(in0, scalar, in1), writes=self._bufs(out), cost=self._vc("dve", out), lat=0.0)
        else:
            self.S.op("dve", lambda e: e.scalar_tensor_tensor(a(out), a(in0), a(scalar), a(in1), op0, op1, accum_out=a(accum)),
                      reads=self._bufs(in0, scalar, in1), writes=self._bufs(out, accum), cost=self._vc("dve", out) + 100.0, lat=0.0)

    def silu(self, out, x):
        self.act(out, x, AF.Exp, scale=-1.0)
        self.act(out, out, AF.Ln, bias=self.oner)
        self.act(out, out, AF.Exp, scale=-1.0)
        self.tt(out, out, x, ALU.mult)

    def _vc(self, eng, x):
        n = self._fs(x)
        return (400.0 + 6.0 * n) if eng == "pool" else (160.0 + 1.0 * n)

    def copy(self, out, in_, eng="dve"):
        a = self._a
        if eng == "act":
            self.S.op("act", lambda e: e.copy(a(out), a(in_)), reads=self._bufs(in_), writes=self._bufs(out),
                      cost=200.0 + 0.85 * self._fs(out), lat=0.0)
        else:
            self.S.op(eng, lambda e: e.tensor_copy(a(out), a(in_)), reads=self._bufs(in_), writes=self._bufs(out),
                      cost=self._vc(eng, out), lat=0.0)

    def memset(self, out, v, eng="pool"):
        a = self._a
        self.S.op(eng, lambda e: e.memset(a(out), v), writes=self._bufs(out), cost=self._vc(eng, out), lat=0.0)

    def reduce(self, out, in_, op=ALU.add):
        a = self._a
        self.S.op("dve", lambda e: e.tensor_reduce(a(out), a(in_), AX.X, op), reads=self._bufs(in_), writes=self._bufs(out),
                  cost=self._vc("dve", in_), lat=0.0)

    def recip(self, out, in_):
        a = self._a
        self.S.op("dve", lambda e: e.reciprocal(a(out), a(in_)), reads=self._bufs(in_), writes=self._bufs(out),
                  cost=200.0 + 7.0 * self._fs(out), lat=0.0)

    def dma(self, out, in_, q="sp"):
        a = self._a
        nb = self._fs(out) * 128.0 * 4.0
        self.S.op(q, lambda e: e.dma_start(out=a(out), in_=a(in_)), reads=self._bufs(in_), writes=self._bufs(out), dma=True,
                  cost=(1000.0 if q == "pool" else 150.0), lat=nb / 300.0)

    def co(self, name):
        o, w = CO_OFF[name]
        return self.CO[:, o:o + w]

    def ppv(self, name, a=0, b=None):
        o, w = PP_OFF[name]
        b = w if b is None else b
        return self.PP[:, o + a:o + b]

    def load_w(self, Wt, dram2d, c0, ncols, kchunks=8):
        src = dram2d[:, c0:c0 + ncols].rearrange("(c p) n -> p c n", p=128)
        self.dma(Wt[:, 0:kchunks, 0:ncols], src, q="pool")

    def proj_fm(self, ps, W, c0, ncol, tg):
        for c in range(8):
            self.mm(ps[0:ncol, :], W[:, c, c0:c0 + ncol], self.XT[:, c, tg * 512:(tg + 1) * 512], start=(c == 0), stop=(c == 7))

    def proj_tm(self, ps, W, c0, ncol, t):
        for c in range(8):
            self.mm(ps[:, 0:ncol], self.XT[:, c, t * 128:(t + 1) * 128], W[:, c, c0:c0 + ncol], start=(c == 0), stop=(c == 7))

    def make_XT(self, src, s_):
        A = self.A
        xf = [A.alloc([1024], F32, "xf0")] * 2
        xb = [A.alloc([1024], BF16, f"xb{i}") for i in range(2)]
        for t in range(NT):
            r0 = s_ * SEQ + t * 128
            self.dma(xf[t % 2], src[r0:r0 + 128, :])
            self.copy(xb[t % 2], xf[t % 2], eng="act")
            for hh in range(2):
                ps = self.ps()
                for k in range(4):
                    c = hh * 4 + k
                    self.mm(ps[:, k * 128:(k + 1) * 128], xb[t % 2][:, c * 128:(c + 1) * 128], self.identb)
                self.copy(T(self.XT.ap[:, hh * 4:hh * 4 + 4, t * 128:(t + 1) * 128], self.XT.buf),
                          T(ps.ap.rearrange("p (k j) -> p k j", j=128), ps.buf), eng=("act" if hh == 0 else "dve"))

    def norm_tr(self, o_sb, ng, grow, ytc0, t, tmp, extra=None, post=None):
        gs = 256 // ng
        sq, ss, ybf = tmp["sq"], tmp["ss"], tmp["ybf"]
        self.tt(sq, o_sb, o_sb, ALU.mult)
        self.reduce(ss[:, 0:ng], sq.ap.rearrange("p (g e) -> p g e", e=gs) if False else T(sq.ap.rearrange("p (g e) -> p g e", e=gs), sq.buf))
        self.act(ss[:, 0:ng], ss[:, 0:ng], AF.Ln, bias=self.epsr, scale=1.0 / gs)
        self.act(ss[:, 0:ng], ss[:, 0:ng], AF.Exp, scale=-0.5)
        o3 = T(o_sb.ap.rearrange("p (g e) -> p g e", e=gs), o_sb.buf)
        s3 = T(sq.ap.rearrange("p (g e) -> p g e", e=gs), sq.buf)
        rb = T(ss.ap[:, 0:ng].unsqueeze(2).to_broadcast([128, ng, gs]), ss.buf)
        self.tt(s3, o3, rb, ALU.mult)
        if extra is not None:
            self.tt(sq, sq, extra, ALU.mult)
        if post is not None:
            self.stt(ybf, sq, float(post), grow, ALU.mult, ALU.mult)
        else:
            self.tt(ybf, sq, grow, ALU.mult)
        ps = self.ps()
        for j in range(2):
            self.mm(ps[:, j * 128:(j + 1) * 128], ybf[:, j * 128:(j + 1) * 128], self.identb)
        self.copy(T(self.YT.ap[:, ytc0:ytc0 + 2, t * 128:(t + 1) * 128], self.YT.buf),
                  T(ps.ap[:, 0:256].rearrange("p (j k) -> p j k", k=128), ps.buf), eng="act")

    def conv_mixer(self, l):
        A = self.A
        W = self.Wb[1]
        self.load_w(W, self.win_d[l], C_CONV, 768)
        m = A.mark()
        cu = A.alloc([2050], F32, "cu")
        Bf = A.alloc([2048], BF16, "Bf")
        acc = A.alloc([2048], F32, "acc")
        tmp = [A.alloc([512], F32, "ctmp0")] * 2
        self.memset(cu[:, 0:2], 0.0)
        for fc in range(2):
            for tg in range(NTG):
                pu, pc, pb = self.ps(), self.ps(), self.ps()
                self.proj_fm(pu, W, fc * 128, 128, tg)
                self.proj_fm(pc, W, 512 + fc * 128, 128, tg)
                self.proj_fm(pb, W, 256 + fc * 128, 128, tg)
                tm = tmp[tg % 2]
                self.copy(tm, pu, eng="act")
                self.tt(cu[:, 2 + tg * 512:2 + (tg + 1) * 512], tm, pc, ALU.mult)
                self.copy(Bf[:, tg * 512:(tg + 1) * 512], pb, eng="act")
                yield
            w = [self.ppv("conv_w", fc * 3 + k, fc * 3 + k + 1) for k in range(3)]
            self.act(acc, cu[:, 0:2048], AF.Copy, scale=w[0])
            self.stt(acc, cu[:, 1:2049], w[1], acc, ALU.mult, ALU.add)
            self.stt(acc, cu[:, 2:2050], w[2], acc, ALU.mult, ALU.add)
            self.tt(self.YT[:, fc, :], acc, Bf, ALU.mult)
            yield

    def gla_mixer(self, l):
        A = self.A
        W = self.Wb[0]
        self.load_w(W, self.win_d[l], C_GLA, 784)
        m = A.mark()
        qf = A.alloc([2048], F32, "qf")
        kf = A.alloc([2048], F32, "kf")
        lrT = A.alloc([2048], F32, "lrT")
        cumT = A.alloc([2048], F32, "cumT")
        big = lrT
        qdm = [A.alloc([2048], BF16, f"qdm{h}") for h in range(4)]
        ki = A.alloc([2048], BF16, "ki")
        keT = A.alloc([2048], BF16, "keT")
        ke_tok = A.alloc([16, 128], BF16, "ke_tok")
        dec = A.alloc([16], F32, "dec")
        zb = [A.alloc([128], F32, "zb0")] * 2
        Sf = A.alloc([256], F32, "Sf")
        Sb = A.alloc([256], BF16, "Sb")
        vtok = [A.alloc([256], BF16, f"vtok{i}") for i in range(2)]
        sg = [A.alloc([256], F32, "sg0")] * 2
        attm = [A.alloc([4, 128], BF16, f"attm{i}") for i in range(2)]
        osb = [A.alloc([256], F32, "osb0")] * 2
        tmp = [dict(sq=A.alloc([256], F32, "sq0"), ss=A.alloc([4], F32, "ss0"), ybf=A.alloc([256], BF16, "ybf0"))] * 2
        for tg in range(NTG):
            p1, p2, p3 = self.ps(), self.ps(), self.ps()
            self.proj_fm(p1, W, 0, 128, tg)
            self.proj_fm(p2, W, 128, 128, tg)
            self.proj_fm(p3, W, 768, 16, tg)
            self.copy(qf[:, tg * 512:(tg + 1) * 512], p1, eng="act")
            self.copy(kf[:, tg * 512:(tg + 1) * 512], p2, eng="dve")
            self.copy(lrT[0:16, tg * 512:(tg + 1) * 512], p3[0:16, :], eng="act")
            yield
        wlr = self.ppv("gla_wlr")
        blr = self.ppv("gla_b")
        for tg in range(NTG):
            pci, pc = self.reserve()
            for k in range(4):
                t = tg * 4 + k
                pz = self.ps()
                self.mm(pz[:, 0:128], lrT[0:16, t * 128:(t + 1) * 128], wlr[0:16, :])
                z = zb[t % 2]
                self.tt(z, pz[:, 0:128], blr, ALU.add)
                self.act(z, z, AF.Exp, scale=-1.0)
                self.act(z, z, AF.Ln, bias=self.oner)
                self.mm(pc[:, k * 128:(k + 1) * 128], z, self.co("triUn16"))
            self.copy(cumT[:, tg * 512:(tg + 1) * 512], pc, eng="act")
            self.unreserve(pci)
            yield
        cl = T(cumT.ap.rearrange("p (t k) -> p t k", k=128)[:, :, 127], cumT.buf)
        self.act(dec, cl, AF.Exp)
        self.act(big, cumT, AF.Exp)
        self.tt(qf, qf, big, ALU.mult)
        for h in range(4):
            self.ts(qdm[h], qf, self.co("hm")[:, h:h + 1], ALU.mult, float(32 ** -0.5), ALU.mult)
        self.act(big, cumT, AF.Exp, scale=-1.0)
        self.tt(ki, kf, big, ALU.mult)
        for t in range(NT):
            sl = slice(t * 128, (t + 1) * 128)
            z = zb[t % 2]
            self.act(z, cumT[:, sl], AF.Exp, scale=-1.0, bias=cl[:, t:t + 1])
            self.tt(keT[:, sl], kf[:, sl], z, ALU.mult)
            pt = self.ps()
            self.mm(pt[:, 0:128], keT[:, sl], self.identb)
            self.copy(ke_tok[:, t, :], pt[:, 0:128], eng="act")
            if t % 2 == 1:
                yield
        self.memset(Sf, 0.0)
        self.memset(Sb, 0.0)
        triU3 = T(self.co("triU").ap.unsqueeze(1).to_broadcast([128, 4, 128]), self.CO.buf)
        for t in range(NT):
            sl = slice(t * 128, (t + 1) * 128)
            v, g_, am, ob = vtok[t % 2], sg[t % 2], attm[t % 2], osb[t % 2]
            pv = self.ps()
            self.proj_tm(pv, W, 256, 256, t)
            self.copy(v, pv[:, 0:256], eng="act")
            pg = self.ps()
            self.proj_tm(pg, W, 512, 256, t)
            self.silu(g_, pg[:, 0:256])
            yield
            pa = self.ps()
            for h in range(4):
                self.mm(pa[:, h * 128:(h + 1) * 128], ki[:, sl], qdm[h][:, sl])
            self.tt(am, T(pa.ap.rearrange("p (h i) -> p h i", i=128), pa.buf), triU3, ALU.mult)
            yield
            po = self.ps()
            for h in range(4):
                hs = slice(h * 64, (h + 1) * 64)
                self.mm(po[:, hs], qdm[h][:, sl], Sb[:, hs], start=True, stop=False)
                self.mm(po[:, hs], am[:, h, :], v[:, hs], start=False, stop=True)
            yield
            pd = self.ps()
            self.mm(pd[:, 0:256], ke_tok[:, t, :], v)
            self.stt(Sf, Sf, dec[:, t:t + 1], pd[:, 0:256], ALU.mult, ALU.add)
            self.copy(Sb, Sf, eng="act")
            self.copy(ob, po[:, 0:256], eng="act")
            yield
            self.norm_tr(ob, 4, self.ppv("gla_g"), 2, t, tmp[t % 2], extra=g_)
            yield

    def ssd_mixer(self, l):
        A = self.A
        W = self.Wb[1]
        self.load_w(W, self.win_d[l], C_SSD, 1028)
        m = A.mark()
        pre = [A.alloc([2051], F32, "pre0")] * 2
        acc = A.alloc([2048], F32, "sacc")
        cv = [A.alloc([2048], BF16, f"cv{i}") for i in range(6)]
        dtt = A.alloc([16, 4], F32, "dtt")
        dtA = A.alloc([16, 4], F32, "dtA")
        arow = A.alloc([4], F32, "arow")
        STf = A.alloc([256], F32, "STf")
        STb = A.alloc([256], BF16, "STb")
        xs_tok = [A.alloc([256], F32, f"xs_tok{i}") for i in range(2)]
        B_tok = [A.alloc([256], BF16, f"B_tok{i}") for i in range(2)]
        xdt = [A.alloc([256], F32, f"xdt{i}") for i in range(2)]
        xdtb = [A.alloc([256], BF16, f"xdtb{i}") for i in range(2)]
        xdd = [A.alloc([256], BF16, f"xdd{i}") for i in range(2)]
        cbm = [A.alloc([2, 128], F32, f"cbm{i}") for i in range(2)]
        lhsD = [A.alloc([128], F32, f"lhsD{i}") for i in range(2)]
        eD = [A.alloc([128], F32, f"eD{i}") for i in range(2)]
        MT = [A.alloc([128], BF16, f"MT{i}") for i in range(2)]
        ecum = [A.alloc([4], F32, f"ecum{i}") for i in range(2)]
        decs = [A.alloc([4], F32, f"decs{i}") for i in range(2)]
        t1 = [A.alloc([256], F32, f"t1{i}") for i in range(2)]
        t2 = [A.alloc([256], F32, "t2_0")] * 2
        sz = [A.alloc([256], F32, "sz_0")] * 2
        tmp = [dict(sq=A.alloc([256], F32, f"ssq{i}"), ss=A.alloc([4], F32, f"sss{i}"), ybf=A.alloc([256], BF16, f"sybf{i}")) for i in range(2)]
        self.memset(pre[0][:, 0:3], 0.0)
        for c6 in range(6):
            pr = pre[c6 % 2]
            for tg in range(NTG):
                ps = self.ps()
                self.proj_fm(ps, W, 256 + c6 * 128, 128, tg)
                self.copy(pr[:, 3 + tg * 512:3 + (tg + 1) * 512], ps, eng=("act" if tg % 2 == 0 else "dve"))
            w = [self.ppv("ssd_cw", c6 * 4 + k, c6 * 4 + k + 1) for k in range(4)]
            self.act(acc, pr[:, 0:2048], AF.Copy, scale=w[0])
            for k in range(1, 4):
                self.stt(acc, pr[:, k:k + 2048], w[k], acc, ALU.mult, ALU.add)
            self.ts(acc, acc, self.ppv("ssd_cb", c6, c6 + 1), ALU.add)
            scr = pr[:, 3:2051]
            self.act(scr, acc, AF.Exp, scale=-1.0)
            self.act(scr, scr, AF.Ln, bias=self.oner)
            self.act(scr, scr, AF.Exp, scale=-1.0)
            self.tt(cv[c6], acc, scr, ALU.mult)
            yield
        self.act(arow, self.ppv("ssd_alog"), AF.Exp)
        self.ts(arow, arow, -1.0, ALU.mult)
        for t in range(NT):
            ps = self.ps()
            self.proj_tm(ps, W, 1024, 4, t)
            self.tt(dtt[:, t, :], ps[:, 0:4], self.ppv("ssd_dtb"), ALU.add)
            if t % 4 == 3:
                yield
        self.act(dtt, dtt, AF.Exp)
        self.act(dtt, dtt, AF.Ln, bias=self.oner)
        self.tt(dtA, dtt, T(arow.ap.unsqueeze(1).to_broadcast([128, 16, 4]), arow.buf), ALU.mult)
        self.memset(STf, 0.0)
        self.memset(STb, 0.0)
        triU = self.co("triU")
        triU2 = T(triU.ap.unsqueeze(1).to_broadcast([128, 2, 128]), self.CO.buf)
        for t in range(NT):
            sl = slice(t * 128, (t + 1) * 128)
            i2 = t % 2
            ptx = self.ps()
            for j in range(2):
                self.mm(ptx[:, j * 128:(j + 1) * 128], cv[j][:, sl], self.identb)
                self.mm(ptx[:, 256 + j * 128:256 + (j + 1) * 128], cv[2 + j][:, sl], self.identb)
            self.copy(xs_tok[i2], ptx[:, 0:256], eng="act")
            self.copy(B_tok[i2], ptx[:, 256:512], eng="dve")
            x3 = T(xs_tok[i2].ap.rearrange("p (h e) -> p h e", e=64), xs_tok[i2].buf)
            dtb = T(dtt.ap[:, t, :].unsqueeze(2).to_broadcast([128, 4, 64]), dtt.buf)
            self.tt(T(xdt[i2].ap.rearrange("p (h e) -> p h e", e=64), xdt[i2].buf), x3, dtb, ALU.mult)
            self.copy(xdtb[i2], xdt[i2], eng="act")
            yield
            pcb = self.ps()
            for g in range(2):
                self.mm(pcb[:, g * 128:(g + 1) * 128], cv[2 + g][:, sl], cv[4 + g][:, sl])
            self.tt(cbm[i2], T(pcb.ap[:, 0:256].rearrange("p (g i) -> p g i", i=128), pcb.buf), triU2, ALU.mult)
            pcm = self.ps()
            self.mm(pcm[:, 0:4], triU, dtA[:, t, :])
            self.mm(pcm[:, 4:8], self.co("ones"), dtA[:, t, :])
            self.act(ecum[i2], pcm[:, 0:4], AF.Exp)
            self.act(decs[i2], pcm[:, 4:8], AF.Exp)
            yield
            pyi, py = self.reserve()
            pyoi, pyo = self.reserve()
            for hd in range(4):
                g = hd // 2
                hs = slice(hd * 64, (hd + 1) * 64)
                k2 = hd % 2
                self.ts(lhsD[k2], self.co("trisL"), dtA[:, t, hd:hd + 1], ALU.mult)
                pD = self.ps()
                self.mm(pD[:, 0:128], lhsD[k2], triU)
                self.act(eD[k2], pD[:, 0:128], AF.Exp)
                self.tt(MT[k2], eD[k2], cbm[i2][:, g, :], ALU.mult)
                self.mm(py[:, hs], MT[k2], xdtb[i2][:, hs])
                self.mm(pyo[:, hs], cv[4 + g][:, sl], STb[:, hs])
                self.ts(xdd[i2][:, hs], xdt[i2][:, hs], eD[k2][:, 127:128], ALU.mult)
                yield
            pst = self.ps()
            for g in range(2):
                gs = slice(g * 128, (g + 1) * 128)
                self.mm(pst[:, gs], B_tok[i2][:, gs], xdd[i2][:, gs])
            S3 = T(STf.ap.rearrange("p (h e) -> p h e", e=64), STf.buf)
            self.tt(S3, S3, T(decs[i2].ap.unsqueeze(2).to_broadcast([128, 4, 64]), decs[i2].buf), ALU.mult)
            self.tt(STf, STf, pst[:, 0:256], ALU.add)
            self.copy(STb, STf, eng="act")
            yield
            a3 = T(t1[i2].ap.rearrange("p (h e) -> p h e", e=64), t1[i2].buf)
            self.tt(a3, T(pyo.ap[:, 0:256].rearrange("p (h e) -> p h e", e=64), pyo.buf),
                    T(ecum[i2].ap.unsqueeze(2).to_broadcast([128, 4, 64]), ecum[i2].buf), ALU.mult)
            self.tt(t1[i2], t1[i2], py[:, 0:256], ALU.add)
            self.unreserve(pyoi)
            self.unreserve(pyi)
            self.tt(t2[i2], xs_tok[i2], self.ppv("ssd_d"), ALU.mult, eng="pool")
            self.tt(t1[i2], t1[i2], t2[i2], ALU.add)
            yield
            pz = self.ps()
            self.proj_tm(pz, W, 0, 256, t)
            self.silu(sz[i2], pz[:, 0:256])
            self.tt(t1[i2], t1[i2], sz[i2], ALU.mult)
            yield
            self.norm_tr(t1[i2], 2, self.ppv("ssd_g"), 4, t, tmp[i2])
            yield

    def diff_mixer(self, l):
        A = self.A
        W = self.Wb[0]
        self.load_w(W, self.win_d[l], C_DIFF, 768)
        m = A.mark()
        lam_init = 0.8 - 0.6 * float(np.exp(-0.3 * l))
        kT = A.alloc([2, 2048], BF16, "kT")
        vtok = A.alloc([16, 4, 65], BF16, "dvtok")
        Otok = [A.alloc([4, 256], F32, f"Otok{i}") for i in range(2)]
        qm = [[A.alloc([512], BF16, f"qm{c}{i}") for i in range(2)] for c in range(2)]
        pts = [A.alloc([512], BF16, f"pt{i}") for i in range(3)]
        lt = A.alloc([64], F32, "lt")
        lam = A.alloc([4], F32, "lam")
        rec = [A.alloc([2, 4], F32, f"rec{i}") for i in range(2)]
        o1 = [A.alloc([4, 64], F32, f"o1{i}") for i in range(2)]
        o2 = [A.alloc([4, 64], F32, f"o2{i}") for i in range(2)]
        tmp = [dict(sq=A.alloc([256], F32, f"dsq{i}"), ss=A.alloc([4], F32, f"dss{i}"), ybf=A.alloc([256], BF16, f"dybf{i}")) for i in range(2)]
        dl = self.ppv("diff_l")
        self.tt(lt[:, 0:32], dl[:, 0:32], dl[:, 32:64], ALU.mult)
        self.tt(lt[:, 32:64], dl[:, 64:96], dl[:, 96:128], ALU.mult)
        self.reduce(lam[:, 0:2], T(lt.ap.rearrange("p (a b) -> p a b", b=32), lt.buf))
        self.act(lam[:, 0:2], lam[:, 0:2], AF.Exp)
        self.tt(lam[:, 2:3], lam[:, 0:1], lam[:, 1:2], ALU.subtract)
        self.ts(lam[:, 3:4], lam[:, 2:3], float(lam_init), ALU.add, -1.0, ALU.mult)
        nlam = lam[:, 3:4]
        for kc in range(2):
            for tg in range(NTG):
                ps = self.ps()
                self.proj_fm(ps, W, 256 + kc * 128, 128, tg)
                self.copy(kT[:, kc, tg * 512:(tg + 1) * 512], ps, eng=("act" if tg % 2 else "dve"))
                yield
        self.memset(vtok, 1.0)
        for t in range(NT):
            ps = self.ps()
            self.proj_tm(ps, W, 512, 256, t)
            self.copy(T(vtok.ap[:, t, :, 0:64], vtok.buf), T(ps.ap[:, 0:256].rearrange("p (h e) -> p h e", e=64), ps.buf), eng="act")
            if t % 4 == 3:
                yield
        scale = float(32 ** -0.5)
        ptc = 0
        for qg in range(NTG):
            for h in range(4):
                qc = h // 2
                k2 = h % 2
                pq = self.ps()
                self.proj_fm(pq, W, qc * 128, 128, qg)
                for c in range(2):
                    b = (h % 2) * 2 + c
                    self.ts(qm[c][k2], pq, self.co("hm")[:, b:b + 1], ALU.mult, scale, ALU.mult)
                ib = [self.reserve(), self.reserve()]
                steps = [(c, jt) for c in range(2) for jt in range(4 * qg + 4)]

                def qk(c, jt):
                    n0 = max(0, jt - 4 * qg) * 128
                    ps = self.ps()
                    self.mm(ps[:, n0:512], kT[:, qc, jt * 128:(jt + 1) * 128], qm[c][k2][:, n0:512])
                    return ps
                cur = qk(*steps[0])
                for si, (c, jt) in enumerate(steps):
                    nxt = qk(*steps[si + 1]) if si + 1 < len(steps) else None
                    po = ib[c][1]
                    i0 = max(0, jt - 4 * qg)
                    n0 = i0 * 128
                    pt = pts[ptc % 3]
                    ptc += 1
                    self.act(pt[:, n0:512], cur[:, n0:512], AF.Exp)
                    if jt >= 4 * qg:
                        self.tt(pt[:, n0:n0 + 128], pt[:, n0:n0 + 128], self.triUb, ALU.mult, eng="dve")
                    for it in range(i0, 4):
                        self.mm(po[:, it * 65:(it + 1) * 65], pt[:, it * 128:(it + 1) * 128], vtok[:, jt, h, :],
                                start=(jt == 0 and it == 0), stop=(jt == 4 * qg + it))
                    cur = nxt
                    yield
                r = rec[k2]
                for c in range(2):
                    po = ib[c][1]
                    p3 = T(po.ap[:, 0:260].rearrange("p (i e) -> p i e", e=65), po.buf)
                    self.recip(r[:, c, :], p3[:, :, 64])
                    dst = o1[k2] if c == 0 else o2[k2]
                    self.tt(dst, p3[:, :, 0:64], T(r.ap[:, c, :].unsqueeze(2).to_broadcast([128, 4, 64]), r.buf), ALU.mult)
                self.unreserve(ib[1][0])
                self.unreserve(ib[0][0])
                self.stt(T(Otok[qg % 2].ap[:, :, h * 64:(h + 1) * 64], Otok[qg % 2].buf), o2[k2], nlam, o1[k2], ALU.mult, ALU.add)
                yield
            for k4 in range(4):
                t = 4 * qg + k4
                self.norm_tr(Otok[qg % 2][:, k4, :], 4, self.ppv("diff_g"), 6, t, tmp[t % 2], post=(1.0 - lam_init))
            yield

    def layer_norm(self, xt, grow, brow, st, gb_eng="dve", presum=False):
        junk = self.junk
        self.act(junk, xt, AF.Copy, accum=st[:, 0:1])
        self.act(junk, xt, AF.Square, accum=st[:, 1:2])
        self.ts(st[:, 2:3], st[:, 0:1], 1.0 / D, ALU.mult)
        self.tt(st[:, 3:4], st[:, 2:3], st[:, 2:3], ALU.mult)
        self.stt(st[:, 4:5], st[:, 1:2], 1.0 / D, st[:, 3:4], ALU.mult, ALU.subtract)
        self.act(st[:, 5:6], st[:, 4:5], AF.Ln, bias=self.lnepsr)
        self.act(st[:, 6:7], st[:, 5:6], AF.Exp, scale=-0.5)
        self.stt(st[:, 7:8], st[:, 2:3], -1.0, st[:, 6:7], ALU.mult, ALU.mult)
        self.act(xt, xt, AF.Identity, bias=st[:, 7:8], scale=st[:, 6:7])
        self.tt(xt, xt, grow, ALU.mult, eng=gb_eng)
        self.tt(xt, xt, brow, ALU.add, eng=gb_eng)

    def out_proj_ln1_route(self, s, l):
        A = self.A
        Wo = self.Wb[1]
        self.load_w(Wo, self.wo_d[l], 0, 1024)
        m = A.mark()
        RT = A.alloc([8, 36], BF16, "RT")
        self.dma(RT, self.rt_d[l].rearrange("(c p) n -> p c n", p=128), q="pool")
        xo = [A.alloc([1024], F32, f"xo{i}") for i in range(2)]
        xt_ = [A.alloc([1024], F32, f"xt{i}") for i in range(2)]
        xT = [A.alloc([8, 128], BF16, f"x1T{i}") for i in range(2)]
        xtb = [A.alloc([1024], BF16, f"xtb{i}") for i in range(2)]
        st = [A.alloc([16], F32, f"lnst{i}") for i in range(2)]
        LA = A.alloc([NT, 36], F32, "LA")
        gmx = A.alloc([NT], F32, "gmx")
        ohg = A.alloc([NT, 4], F32, "ohg")
        eg = A.alloc([NT, 4], F32, "eg")
        pg_ = A.alloc([NT], F32, "pg_")
        sel = A.alloc([NT, 4, 8], F32, "sel")
        el = A.alloc([NT, 8], F32, "el")
        el2 = A.alloc([NT, 8], F32, "el2")
        k1 = A.alloc([NT, 8], F32, "k1")
        k2 = A.alloc([NT, 8], F32, "k2")
        m1 = A.alloc([NT], F32, "m1")
        m2 = A.alloc([NT], F32, "m2")
        self.junk = A.alloc([1024], F32, "junk")
        LNR = A.alloc([2048], F32, "LNR")
        o_ = PP_OFF["ln1_g"][0]
        self.dma(LNR, self.pp_d[l * 128:(l + 1) * 128, o_:o_ + 2048])
        src = self.x_d if l == 0 else self.y_d
        ident = self.co("ident")
        for t in range(NT):
            gt = s * NT + t
            r0 = s * SEQ + t * 128
            i2 = t % 2
            self.dma(xo[i2], src[r0:r0 + 128, :])
            self.memset(st[i2][:, 8:10], 0.0, eng="dve")
            xt = xt_[i2]
            for half in range(2):
                ps = self.ps()
                for c in range(8):
                    self.mm(ps, self.YT[:, c, t * 128:(t + 1) * 128], Wo[:, c, half * 512:(half + 1) * 512], start=(c == 0), stop=(c == 7))
                self.stt(xt[:, half * 512:(half + 1) * 512], xo[i2][:, half * 512:(half + 1) * 512], float(ALPHA), ps, ALU.mult, ALU.add,
                         accum=st[i2][:, 8 + half:9 + half])
            self.tt(st[i2][:, 0:1], st[i2][:, 8:9], st[i2][:, 9:10], ALU.add)
            self.layer_norm(xt, LNR[:, 0:1024], LNR[:, 1024:2048], st[i2], presum=True)
            self.dma(self.X1[r0:r0 + 128, :], xt)
            self.copy(xtb[i2], xt, eng="act")
            for hh in range(2):
                ps = self.ps()
                for k in range(4):
                    c = hh * 4 + k
                    self.mm(ps[:, k * 128:(k + 1) * 128], xtb[i2][:, c * 128:(c + 1) * 128], self.identb)
                self.copy(T(xT[i2].ap[:, hh * 4:hh * 4 + 4, :], xT[i2].buf), T(ps.ap.rearrange("p (k j) -> p k j", j=128), ps.buf), eng="act")
            ps = self.ps()
            for c in range(8):
                self.mm(ps[:, 0:36], xT[i2][:, c, :], RT[:, c, :], start=(c == 0), stop=(c == 7))
            self.copy(LA[:, t, :], ps[:, 0:36], eng="act")
        def bc(x, shape, axis):
            return T(x.ap.unsqueeze(axis).to_broadcast(shape), x.buf)
        g0 = s * NT
        Lg = LA[:, :, 0:4]
        Le = T(LA.ap[:, :, 4:36].rearrange("p t (g j) -> p t g j", j=8), LA.buf)
        self.reduce(gmx, Lg, op=ALU.max)
        self.tt(ohg, Lg, bc(gmx, [128, NT, 4], 2), ALU.is_equal)
        self.tt(eg, Lg, bc(gmx, [128, NT, 4], 2), ALU.subtract)
        self.act(eg, eg, AF.Exp)
        self.reduce(pg_, eg)
        self.recip(pg_, pg_)
        self.tt(sel, Le, bc(ohg, [128, NT, 4, 8], 3), ALU.mult)
        self.reduce(el, T(sel.ap.rearrange("p t g j -> p t j g"), sel.buf))
        self.reduce(m1, el, op=ALU.max)
        self.tt(k1, el, bc(m1, [128, NT, 8], 2), ALU.is_equal)
        self.stt(el2, k1, -1e30, el, ALU.mult, ALU.add)
        self.reduce(m2, el2, op=ALU.max)
        self.tt(k2, el2, bc(m2, [128, NT, 8], 2), ALU.is_equal)
        self.tt(m2, m2, m1, ALU.subtract)
        self.act(m2, m2, AF.Exp)
        self.ts(m2, m2, 1.0, ALU.add)
        self.recip(m2, m2)
        self.tt(self.GT[:, g0:g0 + NT, 0], m2, pg_, ALU.mult)
        self.tt(self.GT[:, g0:g0 + NT, 1], pg_, self.GT[:, g0:g0 + NT, 0], ALU.subtract)
        for (OH, kk) in ((self.OH1, k1), (self.OH2, k2)):
            dst = T(OH.ap[:, g0:g0 + NT, :].rearrange("p t (g j) -> p t g j", j=8), OH.buf)
            self.tt(dst, bc(kk, [128, NT, 4, 8], 2), bc(ohg, [128, NT, 4, 8], 3), ALU.mult)
        self.S.barrier()
        A.release(m)

    def moe_sparse(self, l):
        A = self.A
        m = A.mark()
        NTT = self.nseq * NT
        NB = self.NB
        ones = self.co("ones")
        RK = A.alloc([NTT, 32], F32, "RK")
        At = [A.alloc([32], F32, f"At{i}") for i in range(2)]
        Acum = A.alloc([32], F32, "Acum")
        cnt = A.alloc([32], F32, "cnt")
        self.memset(Acum, 0.0)
        for gt in range(NTT):
            a = At[gt % 2]
            self.tt(a, self.OH1[:, gt, :], self.OH2[:, gt, :], ALU.add)
            ps = self.ps()
            self.mm(ps[:, 0:32], ones, Acum, start=True, stop=False)
            self.mm(ps[:, 0:32], self.co("triUs"), a, start=False, stop=True)
            self.copy(RK[:, gt, :], ps[:, 0:32], eng="act")
            self.tt(Acum, Acum, a, ALU.add)
        ps = self.ps()
        self.mm(ps[:, 0:32], ones, Acum)
        self.copy(cnt, ps[:, 0:32], eng="act")
        cmp = A.alloc([32, 16], F32, "cmp")
        pad = A.alloc([32], F32, "pad")
        sc = [A.alloc([32], F32, f"sc{i}") for i in range(2)]
        pstart = A.alloc([32], F32, "pstart")
        thr = self.co("thr16")
        self.tt(cmp, T(cnt.ap.unsqueeze(2).to_broadcast([128, 32, 16]), cnt.buf),
                T(thr.ap.unsqueeze(1).to_broadcast([128, 32, 16]), self.CO.buf), ALU.is_gt)
        self.reduce(pad, cmp)
        self.ts(pad, pad, 512.0, ALU.mult)
        cur = pad
        k = 0
        for sh in (1, 2, 4, 8, 16):
            nx = sc[k % 2]
            k += 1
            self.copy(nx[:, 0:sh], cur[:, 0:sh])
            self.tt(nx[:, sh:32], cur[:, sh:32], cur[:, 0:32 - sh], ALU.add)
            cur = nx
        pend = cur
        self.tt(pstart, pend, pad, ALU.subtract)
        big = A.alloc([NTT, 32], F32, "big")
        dstf = A.alloc([NTT, 2], F32, "dstf")
        DST = A.alloc([NTT, 2], I32, "DST")
        self.tt(RK, RK, T(pstart.ap.unsqueeze(1).to_broadcast([128, NTT, 32]), pstart.buf), ALU.add)
        self.tt(big, RK, self.OH1, ALU.mult)
        self.reduce(dstf[:, :, 0], big)
        self.tt(big, RK, self.OH2, ALU.mult)
        self.reduce(dstf[:, :, 1], big)
        self.copy(DST, dstf)
        cb = A.alloc([NB, 32], F32, "cb")
        be = A.alloc([NB], F32, "be")
        inv = A.alloc([NB], F32, "inv")
        ixf = A.alloc([NB, 8], F32, "ixf")
        IXW = A.alloc([NB, 8], I32, "IXW")
        IXD = A.alloc([NB, 4], I32, "IXD")
        bthr = self.co("bthr")[:, 0:NB]
        self.tt(cb, T(pend.ap.unsqueeze(1).to_broadcast([128, NB, 32]), pend.buf),
                T(bthr.ap.unsqueeze(2).to_broadcast([128, NB, 32]), self.CO.buf), ALU.is_le)
        self.reduce(be, cb)
        self.ts(inv, bthr, pend[:, 31:32], ALU.is_ge, 4.0e6, ALU.mult)
        pc = self.co("pc")
        self.stt(be, be, 1024.0, inv, ALU.mult, ALU.add)
        self.ts(be, be, float(l * N_EXP * 1024), ALU.add)
        self.tt(ixf, T(be.ap.unsqueeze(2).to_broadcast([128, NB, 8]), be.buf),
                T(pc.ap.unsqueeze(1).to_broadcast([128, NB, 8]), self.CO.buf), ALU.add)
        self.copy(IXW, ixf)
        self.stt(be, be, 0.5, inv, ALU.mult, ALU.add)
        self.tt(ixf[:, :, 0:4], T(be.ap.unsqueeze(2).to_broadcast([128, NB, 4]), be.buf),
                T(pc.ap[:, 0:4].unsqueeze(1).to_broadcast([128, NB, 4]), self.CO.buf), ALU.add)
        self.copy(IXD, ixf[:, :, 0:4])
        xl = [A.alloc([1024], F32, f"xl{i}") for i in range(3)]
        nslot = NB * 512
        for gt in range(NTT):
            x_ = xl[gt % 3]
            self.dma(x_, self.X1[gt * 128:(gt + 1) * 128, :])
            for k2 in range(2):
                self.idma(self.XS, DST[:, gt, k2:k2 + 1], x_, scatter=True, bound=nslot - 1)
        Wg = [[A.alloc([512], BF16, f"Wg{i}_{c}") for c in range(8)] for i in range(2)]
        Wu = [[A.alloc([512], BF16, f"Wu{i}_{c}") for c in range(8)] for i in range(2)]
        Wd = [[A.alloc([1024], BF16, f"Wd{i}_{c}") for c in range(4)] for i in range(2)]
        xr = [A.alloc([4, 1024], BF16, f"xr{i}") for i in range(2)]
        xsT = [A.alloc([8, 512], BF16, f"xsT{i}") for i in range(2)]
        hT = [A.alloc([4, 512], BF16, f"hT{i}") for i in range(2)]
        sil = [A.alloc([512], F32, f"sil{i}") for i in range(2)]
        ysb = [A.alloc([1024], F32, f"ysb{i}") for i in range(3)]
        wg2 = T(self.wg_d.rearrange("l e d f -> (l e d) f"), Buf("wg", dram=True))
        wu2 = T(self.wu_d.rearrange("l e d f -> (l e d) f"), Buf("wu", dram=True))
        wd2 = T(self.wd_d.rearrange("l e f d -> (l e f) d"), Buf("wd", dram=True))
        yc = 0
        for b in range(NB):
            i2 = b % 2
            for c in range(8):
                self.idma(Wg[i2][c], IXW[:, b, c:c + 1], wg2, scatter=False, bound=2 * N_EXP * 1024 - 1)
                self.idma(Wu[i2][c], IXW[:, b, c:c + 1], wu2, scatter=False, bound=2 * N_EXP * 1024 - 1)
            for c in range(4):
                self.idma(Wd[i2][c], IXD[:, b, c:c + 1], wd2, scatter=False, bound=2 * N_EXP * 512 - 1)
            self.dma(xr[i2], T(self.XS.ap[b * 512:(b + 1) * 512, :].rearrange("(s p) d -> p s d", p=128), self.XS.buf))
            for c2 in range(4):
                ps = self.ps()
                psb = T(ps.ap.bitcast(BF16), ps.buf)
                for j in range(2):
                    c = 2 * c2 + j
                    for s4 in range(4):
                        self.tr(psb[:, j * 512 + s4 * 128:j * 512 + (s4 + 1) * 128], xr[i2][:, s4, c * 128:(c + 1) * 128])
                self.copy(T(xsT[i2].ap[:, 2 * c2:2 * c2 + 2, :], xsT[i2].buf),
                          T(psb.ap.rearrange("p (j n) -> p j n", n=512), ps.buf), eng=("act" if c2 % 2 == 0 else "dve"))
            for fc in range(4):
                pg, pu = self.ps(), self.ps()
                for c in range(8):
                    self.mm(pg, Wg[i2][c][:, fc * 128:(fc + 1) * 128], xsT[i2][:, c, :], start=(c == 0), stop=(c == 7))
                for c in range(8):
                    self.mm(pu, Wu[i2][c][:, fc * 128:(fc + 1) * 128], xsT[i2][:, c, :], start=(c == 0), stop=(c == 7))
                sl_ = sil[fc % 2]
                self.act(sl_, pg, AF.Silu)
                self.tt(hT[i2][:, fc, :], sl_, pu, ALU.mult)
            for s4 in range(4):
                y_ = ysb[yc % 3]
                yc += 1
                for half in range(2):
                    ps = self.ps()
                    for fc in range(4):
                        self.mm(ps, hT[i2][:, fc, s4 * 128:(s4 + 1) * 128], Wd[i2][fc][:, half * 512:(half + 1) * 512], start=(fc == 0), stop=(fc == 3))
                    self.copy(y_[:, half * 512:(half + 1) * 512], ps, eng=("act" if half == 0 else "dve"))
                r0 = b * 512 + s4 * 128
                self.dma(self.YS[r0:r0 + 128, :], y_)
        ya = [A.alloc([1024], F32, f"ya{i}") for i in range(2)]
        yb = [A.alloc([1024], F32, f"yb{i}") for i in range(2)]
        st = [A.alloc([16], F32, f"lnst{i}") for i in range(2)]
        self.junk = A.alloc([1024], F32, "junk")
        LNR = A.alloc([2048], F32, "LNR")
        o_ = PP_OFF["ln2_g"][0]
        self.dma(LNR, self.pp_d[l * 128:(l + 1) * 128, o_:o_ + 2048])

        def fetch(gt):
            self.dma(xl[gt % 3], self.X1[gt * 128:(gt + 1) * 128, :])
            self.idma(ya[gt % 2], DST[:, gt, 0:1], self.YS, scatter=False, bound=nslot - 1)
            self.idma(yb[gt % 2], DST[:, gt, 1:2], self.YS, scatter=False, bound=nslot - 1)
        fetch(0)
        for gt in range(NTT):
            i2 = gt % 2
            x_ = xl[gt % 3]
            self.act(x_, x_, AF.Copy, scale=float(ALPHA))
            self.stt(x_, ya[i2], self.GT[:, gt, 0:1], x_, ALU.mult, ALU.add)
            self.memset(st[i2][:, 0:1], 0.0, eng="dve")
            self.stt(x_, yb[i2], self.GT[:, gt, 1:2], x_, ALU.mult, ALU.add, accum=st[i2][:, 0:1])
            if gt + 1 < NTT:
                fetch(gt + 1)
            self.layer_norm(x_, LNR[:, 0:1024], LNR[:, 1024:2048], st[i2], gb_eng="dve", presum=True)
            self.dma(self.y_d[gt * 128:(gt + 1) * 128, :], x_)
        self.S.barrier()
        A.release(m)

    def idma(self, dst, idx, src, scatter, bound):
        a = self._a
        self.bounds.add(bound)
        regs = self.bregs
        bound_key = bound
        bound = None
        if scatter:
            fn = lambda e: e.indirect_dma_start(out=a(dst), out_offset=bass.IndirectOffsetOnAxis(ap=a(idx), axis=0),
                                                in_=a(src), in_offset=None, bounds_check=regs[bound_key], oob_is_err=False)
        else:
            fn = lambda e: e.indirect_dma_start(out=a(dst), out_offset=None, in_=a(src),
                                                in_offset=bass.IndirectOffsetOnAxis(ap=a(idx), axis=0),
                                                bounds_check=regs[bound_key], oob_is_err=False)
        nb = min(self._fs(dst), self._fs(src)) * 128.0 * 4.0
        self.S.op("pool", fn, reads=self._bufs(src, idx), writes=self._bufs(dst), dma=True, cost=900.0, lat=nb / 300.0)

    def next_w(self):
        self.wi += 1
        return self.Wb[self.wi % 2]

    def build(self):
        A = self.A
        nc = self.nc
        ntok = self.nseq * SEQ
        NTT = self.nseq * NT
        self.NB = (2 * ntok + N_EXP * 511) // 512 + 1
        assert self.NB <= 48
        self.X1 = T(nc.dram_tensor("x1_scr", [ntok, D], F32).ap(), Buf("X1", dram=True))
        self.XS = T(nc.dram_tensor("xs_scr", [self.NB * 512, D], BF16).ap(), Buf("XS", dram=True))
        self.YS = T(nc.dram_tensor("ys_scr", [self.NB * 512, D], F32).ap(), Buf("YS", dram=True))
        self.CO = A.alloc([NCO], F32, "CO")
        self.PP = A.alloc([NPPS], F32, "PP")
        self.identb = A.alloc([128], BF16, "identb")
        self.triUb = A.alloc([128], BF16, "triUb")
        cst = A.alloc([4], F32, "cst")
        self.OH1 = A.alloc([NTT, 32], BF16, "OH1")
        self.OH2 = A.alloc([NTT, 32], BF16, "OH2")
        self.GT = A.alloc([NTT, 2], F32, "GT")
        self.xtmark = A.mark()
        self.XT = A.alloc([8, 2048], BF16, "XT")
        self.ytmark = A.mark()
        self.YT = A.alloc([8, 2048], BF16, "YT")
        self.dma(self.CO, self.co_d)
        self.copy(self.identb, self.co("ident"))
        self.copy(self.triUb, self.co("triU"))
        self.memset(cst[:, 0:1], RMS_EPS)
        self.memset(cst[:, 1:2], 1.0)
        self.memset(cst[:, 2:3], LN_EPS)
        self.epsr, self.oner, self.lnepsr = cst[:, 0:1], cst[:, 1:2], cst[:, 2:3]
        self.Wb = [A.alloc([8, 784], BF16, "Wb0"), A.alloc([8, 1040], BF16, "Wb1")]
        self.wi = 0
        base = A.mark()
        self.xbase = base
        for l in range(self.n_layers):
            self.dma(self.PP, self.pp_d[l * 128:(l + 1) * 128, 0:NPPS])
            src = self.x_d if l == 0 else self.y_d
            for s in range(self.nseq):
                A.release(base)
                self.make_XT(src, s)
                self.run_gens([(self.conv_mixer(l), 1, [0, 1, 2, 3]), (self.gla_mixer(l), 3, [4, 5, 6, 7])])
                self.S.barrier()
                A.release(base)
                gens = [(self.ssd_mixer(l), 1, [0, 1, 2, 3]), (self.diff_mixer(l), 2, [4, 5, 6, 7])]
                self.run_gens(gens)
                self.S.barrier()
                A.release(base)
                self.out_proj_ln1_route(s, l)
            A.release(self.xtmark)
            self.moe_sparse(l)
            A.release(base)


_PROG_CACHE = {}


def get_prog(nseq, n_layers, with_moe, stages="cgsd", dbg=False):
    key = (nseq, n_layers, with_moe, stages, dbg)
    if key not in _PROG_CACHE:
        _PROG_CACHE[key] = Prog(*key)
    return _PROG_CACHE[key]


def kernel(**inp):
    inp = {k: np.asarray(v) for k, v in inp.items()}
    n_cores = 8
    nseq = 2
    prog = get_prog(nseq, 2, True)
    x = np.ascontiguousarray(inp["x"], dtype=np.float32).reshape(16 * SEQ, D)
    pp = np.concatenate([host_pp(inp, l) for l in range(2)], axis=0)
    co = host_consts()
    rt = np.ascontiguousarray(np.concatenate([inp["router_g"], inp["router_e"].reshape(2, D, 32)], axis=2), dtype=np.float32)
    shared = {"w_in": inp["w_in"], "w_o": inp["w_o"], "pp": pp, "co": co, "rt": rt,
              "w_gate": inp["w_gate"], "w_up": inp["w_up"], "w_down": inp["w_down"]}
    in_maps = []
    for c in range(n_cores):
        mcore = dict(shared)
        mcore["x"] = x[c * nseq * SEQ:(c + 1) * nseq * SEQ]
        in_maps.append(mcore)
    res = run_bass_kernel_spmd(prog.nc, in_maps, core_ids=list(range(n_cores)))
    y = np.concatenate([res.results[c]["y"] for c in range(n_cores)], axis=0)
    return y.reshape(16, SEQ, D).astype(np.float32)
```
